# Optimizing a Trainium2 kernel written in Bass

```python
import jax, jax.numpy as jnp
from jax import lax
import numpy as np

D_MODEL = 2048
BATCH = 4
SEQ = 4096
DEPTH = 1

HEAD_DIM = 128
N_HEADS_MIX = D_MODEL // HEAD_DIM
N_HEADS_ATTN = 3 * N_HEADS_MIX // 4
N_HEADS_SGU = N_HEADS_MIX - N_HEADS_ATTN
D_ATTN = N_HEADS_ATTN * HEAD_DIM
D_SGU = N_HEADS_SGU * HEAD_DIM
D_IN = 3 * D_ATTN + 2 * D_SGU
DILATION_PATTERNS = ((128, 1), (512, 4), (2048, 16))
ROPE_THETA = 10000.0
SGU_CHUNK = 128
PEER_HEADS = 8
PEER_NKEYS = 128
PEER_EXPERTS = PEER_NKEYS * PEER_NKEYS
PEER_DKEY = 256
PEER_TOPK = 16
PEER_TOKEN_BLOCK = 128
N_MOD = 6
RMS_EPS = 1e-6
LN_EPS = 1e-5

kernel_name = "hybrid_dilated_attn_sgu_peer_block"


def rms_norm(t, g):
    t32 = t.astype(jnp.float32)
    t32 = t32 * lax.rsqrt(jnp.mean(t32 * t32, axis=-1, keepdims=True) + RMS_EPS)
    return t32.astype(t.dtype) * g


def rope(t, positions):
    half = t.shape[-1] // 2
    inv = ROPE_THETA ** (-jnp.arange(half, dtype=jnp.float32) / half)
    ang = positions.astype(jnp.float32)[..., None] * inv
    cos = jnp.cos(ang)[:, :, None, :]
    sin = jnp.sin(ang)[:, :, None, :]
    t32 = t.astype(jnp.float32)
    t1, t2 = t32[..., :half], t32[..., half:]
    return jnp.concatenate([t1 * cos - t2 * sin, t2 * cos + t1 * sin], axis=-1).astype(t.dtype)


def dilated_window_attention(q, k, v, window, dilation):
    B, S, H, E = q.shape
    steps = window // dilation
    L = S // dilation
    nb = -(-L // steps)
    Lp = nb * steps

    def to_classes(t):
        t = t.reshape(B, L, dilation, H, E).transpose(0, 2, 1, 3, 4)
        return jnp.pad(t, ((0, 0), (0, 0), (0, Lp - L), (0, 0), (0, 0)))

    def with_prev(t):
        tb = t.reshape(B, dilation, nb, steps, H, E)
        prev = jnp.pad(tb, ((0, 0), (0, 0), (1, 0), (0, 0), (0, 0), (0, 0)))[:, :, :-1]
        return jnp.concatenate([prev, tb], axis=3)

    qb = to_classes(q).reshape(B, dilation, nb, steps, H, E)
    kb = with_prev(to_classes(k))
    vb = with_prev(to_classes(v))
    s = jnp.einsum('bdnqhe,bdnkhe->bdnhqk', qb, kb).astype(jnp.float32)
    qi = jnp.arange(steps)[:, None]
    km = jnp.arange(2 * steps)[None, :]
    band = (km >= qi) & (km <= qi + steps)
    valid = band[None] & ((jnp.arange(nb)[:, None, None] > 0) | (km[None] >= steps))
    s = jnp.where(valid[None, None, :, None], s, -jnp.inf)
    lse = jax.nn.logsumexp(s, axis=-1)
    p = jnp.exp(s - lse[..., None])
    o = jnp.einsum('bdnhqk,bdnkhe->bdnqhe', p.astype(v.dtype), vb)
    o = o.reshape(B, dilation, Lp, H, E)[:, :, :L].transpose(0, 2, 1, 3, 4).reshape(B, S, H, E)
    lse = lse.transpose(0, 1, 2, 4, 3).reshape(B, dilation, Lp, H)[:, :, :L]
    lse = lse.transpose(0, 2, 1, 3).reshape(B, S, H)
    return o, lse


def causal_chunk_sgu(u, v, w_s, b_s, ln_g, ln_b):
    B, S, Hs, E = u.shape
    n = S // SGU_CHUNK
    v32 = v.astype(jnp.float32)
    mu = jnp.mean(v32, axis=-1, keepdims=True)
    var = jnp.mean(jnp.square(v32 - mu), axis=-1, keepdims=True)
    vn = ((v32 - mu) * lax.rsqrt(var + LN_EPS)).astype(v.dtype) * ln_g + ln_b
    vc = vn.reshape(B, n, SGU_CHUNK, Hs, E)
    causal = jnp.tril(jnp.ones((SGU_CHUNK, SGU_CHUNK), dtype=bool))
    ws = jnp.where(causal[None], w_s, jnp.zeros((), w_s.dtype))
    mixed = jnp.einsum('hij,bnjhe->bnihe', ws, vc) + b_s.T[None, None, :, :, None]
    return u * mixed.reshape(B, S, Hs, E)


def peer_ffn(h, w_q, sub_keys, u_emb, v_emb):
    B, S, D = h.shape
    T = B * S
    K = PEER_TOPK
    x = h.reshape(T, D)
    q = (x @ w_q).reshape(T, PEER_HEADS, 2, PEER_DKEY // 2)
    s = jnp.einsum('thpc,hpkc->thpk', q, sub_keys).astype(jnp.float32)
    s_top, i_top = lax.top_k(s, K)
    cand_s = s_top[:, :, 0, :, None] + s_top[:, :, 1, None, :]
    cand_i = i_top[:, :, 0, :, None] * PEER_NKEYS + i_top[:, :, 1, None, :]
    best_s, pos = lax.top_k(cand_s.reshape(T, PEER_HEADS, K * K), K)
    idx = jnp.take_along_axis(cand_i.reshape(T, PEER_HEADS, K * K), pos, axis=-1)
    g = jax.nn.softmax(best_s, axis=-1).astype(h.dtype)
    nblk = T // PEER_TOKEN_BLOCK
    HK = PEER_HEADS * K

    def expert_block(args):
        xb, ib, gb = args
        ue = jnp.take(u_emb, ib, axis=0)
        a = jax.nn.gelu(jnp.einsum('td,tnd->tn', xb, ue), approximate=False)
        ve = jnp.take(v_emb, ib, axis=0)
        return jnp.einsum('tn,tnd->td', gb * a, ve)

    out = lax.map(expert_block, (x.reshape(nblk, PEER_TOKEN_BLOCK, D),
                                 idx.reshape(nblk, PEER_TOKEN_BLOCK, HK),
                                 g.reshape(nblk, PEER_TOKEN_BLOCK, HK)))
    return out.reshape(B, S, D)


def setup_inputs(seed: int = 0) -> dict:
    key = jax.random.key(seed)
    ks = jax.random.split(key, 24)
    f32 = jnp.float32
    D = D_MODEL
    nrm = lambda k, shape, scale: jax.random.normal(k, shape, f32) * scale
    x = jax.random.normal(ks[0], (BATCH, SEQ, D), f32)
    c = jax.random.normal(ks[1], (BATCH, D), f32)
    offset = jax.random.randint(ks[2], (BATCH, 1), 0, 1024, dtype=jnp.int32)
    positions = offset + jnp.arange(SEQ, dtype=jnp.int32)[None, :]
    return {
        "x": x,
        "c": c,
        "positions": positions,
        "w_ada": nrm(ks[3], (DEPTH, D, N_MOD * D), 0.5 * D ** -0.5),
        "b_ada": nrm(ks[4], (DEPTH, N_MOD * D), 0.02),
        "g_norm1": 1.0 + nrm(ks[5], (DEPTH, D), 0.05),
        "w_in": nrm(ks[6], (DEPTH, D, D_IN), D ** -0.5),
        "g_attn_out": 1.0 + nrm(ks[7], (DEPTH, D_ATTN), 0.05),
        "g_sgu_out": 1.0 + nrm(ks[8], (DEPTH, D_SGU), 0.05),
        "sgu_w": nrm(ks[9], (DEPTH, N_HEADS_SGU, SGU_CHUNK, SGU_CHUNK), SGU_CHUNK ** -0.5),
        "sgu_b": 1.0 + nrm(ks[10], (DEPTH, N_HEADS_SGU, SGU_CHUNK), 0.1),
        "sgu_ln_g": 1.0 + nrm(ks[11], (DEPTH, N_HEADS_SGU, HEAD_DIM), 0.05),
        "sgu_ln_b": nrm(ks[12], (DEPTH, N_HEADS_SGU, HEAD_DIM), 0.02),
        "w_out": nrm(ks[13], (DEPTH, D, D), D ** -0.5),
        "g_norm2": 1.0 + nrm(ks[14], (DEPTH, D), 0.05),
        "peer_w_q": nrm(ks[15], (DEPTH, D, PEER_HEADS * PEER_DKEY), D ** -0.5),
        "peer_sub_keys": nrm(ks[16], (DEPTH, PEER_HEADS, 2, PEER_NKEYS, PEER_DKEY // 2), (PEER_DKEY // 2) ** -0.5),
        "peer_u": nrm(ks[17], (DEPTH, PEER_EXPERTS, D), D ** -0.5),
        "peer_v": nrm(ks[18], (DEPTH, PEER_EXPERTS, D), 0.5),
        "g_final": 1.0 + nrm(ks[19], (D,), 0.05),
    }


def reference(x, c, positions, w_ada, b_ada, g_norm1, w_in, g_attn_out, g_sgu_out,
              sgu_w, sgu_b, sgu_ln_g, sgu_ln_b, w_out, g_norm2, peer_w_q, peer_sub_keys,
              peer_u, peer_v, g_final):
    B, S, D = x.shape
    E = HEAD_DIM
    h = x
    cond = jax.nn.silu(c)
    splits = np.cumsum([D_ATTN, D_ATTN, D_ATTN, D_SGU]).tolist()
    for l in range(DEPTH):
        mod = (cond @ w_ada[l] + b_ada[l]).reshape(B, N_MOD, D)
        sh1, sc1, gt1, sh2, sc2, gt2 = [mod[:, i, None, :] for i in range(N_MOD)]

        hn = rms_norm(h, g_norm1[l]) * (1.0 + sc1) + sh1
        proj = hn @ w_in[l]
        qa, ka, va, us, vs = jnp.split(proj, splits, axis=-1)
        qa = rope(qa.reshape(B, S, N_HEADS_ATTN, E), positions) * (E ** -0.5)
        ka = rope(ka.reshape(B, S, N_HEADS_ATTN, E), positions)
        va = va.reshape(B, S, N_HEADS_ATTN, E)
        outs, lses = [], []
        for window, dilation in DILATION_PATTERNS:
            o_i, lse_i = dilated_window_attention(qa, ka, va, window, dilation)
            outs.append(o_i)
            lses.append(lse_i)
        wts = jax.nn.softmax(jnp.stack(lses, axis=0), axis=0)
        attn = sum(wts[i][..., None] * outs[i].astype(jnp.float32) for i in range(len(outs)))
        attn = attn.astype(x.dtype).reshape(B, S, D_ATTN)

        us = jax.nn.gelu(us, approximate=False).reshape(B, S, N_HEADS_SGU, E)
        vs = jax.nn.gelu(vs, approximate=False).reshape(B, S, N_HEADS_SGU, E)
        sgu = causal_chunk_sgu(us, vs, sgu_w[l], sgu_b[l], sgu_ln_g[l], sgu_ln_b[l]).reshape(B, S, D_SGU)

        mixed = jnp.concatenate([rms_norm(attn, g_attn_out[l]), rms_norm(sgu, g_sgu_out[l])], axis=-1)
        h = h + gt1 * (mixed @ w_out[l])

        hn2 = rms_norm(h, g_norm2[l]) * (1.0 + sc2) + sh2
        h = h + gt2 * peer_ffn(hn2, peer_w_q[l], peer_sub_keys[l], peer_u[l], peer_v[l])
    return rms_norm(h, g_final)
```

```python
import numpy as np
import ml_dtypes
from contextlib import ExitStack
import concourse.bass as bass
import concourse.mybir as mybir
from concourse.bass_utils import run_bass_kernel_spmd

F32 = mybir.dt.float32
BF16 = mybir.dt.bfloat16
U32 = mybir.dt.uint32
I32 = mybir.dt.int32
AF = mybir.ActivationFunctionType
ALU = mybir.AluOpType
AX = mybir.AxisListType

D = 2048
NT = 2048
NCTX = 2048
DIN = 5632
ENG = ('pe', 'act', 'dve', 'pool', 'sp')
QG = 2
NW = 16 + QG


class LazySem:
    def __init__(self, name):
        self.name = name
        self.real = None


class Buf:
    def __init__(self, name=''):
        self.w = None
        self.r = {}
        self.name = name
        self.sems = {}
        self.psum = False


def PSB():
    b = Buf('psum')
    b.psum = True
    return b


class KB:
    EP = 16000

    def __init__(self, nc, es):
        self.nc = nc
        self.es = es
        self.streams = {e: [] for e in ENG}
        self.cnt = {e: 0 for e in ENG}
        self.esem = {e: [] for e in ENG}
        self.known = {e: {} for e in ENG}
        self.dcount = {}
        self.dsems = []
        self.nsem = 0
        self.enabled = True

    def _newsem(self, name):
        self.nsem += 1
        return self.es.enter_context(self.nc.semaphore(name))

    def dsem(self, name):
        return LazySem(name)

    def _real(self, ls):
        if ls.real is None:
            sm = self._newsem('d%d_%s' % (self.nsem, ls.name))
            self.dcount[id(sm)] = 0
            self.dsems.append(sm)
            ls.real = sm
        return ls.real

    def buf_sem(self, buf, kind):
        if kind not in buf.sems:
            buf.sems[kind] = LazySem(kind + '_' + (buf.name or 'b'))
        return buf.sems[kind]

    def _eev(self, e):
        c = self.cnt[e]
        ep = (c - 1) // self.EP
        while len(self.esem[e]) <= ep:
            self.esem[e].append(self._newsem('e_%s_%d' % (e, len(self.esem[e]))))
        return (self.esem[e][ep], (c - 1) % self.EP + 1)

    def _deps(self, e, reads, writes):
        evs = []
        for b in reads:
            if b.w is not None:
                evs.append(b.w)
            if b.psum:
                evs.extend(ev for ev in b.r.values() if ev[3] != e)
        for b in writes:
            if b.w is not None:
                evs.append(b.w)
            evs.extend(b.r.values())
        waits = []
        own = self.esem[e]
        k = self.known[e]
        for (sm, val, isd, _eng) in evs:
            if e == 'pe' and any(sm is x for x in own):
                continue
            if isd:
                val = 16 * self.dcount[id(sm)]
            if k.get(id(sm), 0) < val:
                k[id(sm)] = val
                waits.append((sm, val))
        return waits

    def _book(self, ev, reads, writes):
        for b in reads:
            b.r[id(ev[0])] = ev
        for b in writes:
            b.w = ev
            b.r = {}

    def op(self, e, fn, reads=(), writes=()):
        if not self.enabled:
            return
        waits = self._deps(e, reads, writes)
        self.cnt[e] += 1
        sm, val = self._eev(e)
        self.streams[e].append((waits, fn, (sm, 1)))
        self._book((sm, val, False, e), reads, writes)

    def dma(self, q, fn, sem, reads=(), writes=()):
        if not self.enabled:
            return
        sem = self._real(sem)
        waits = self._deps(q, reads, writes)
        self.dcount[id(sem)] += 1
        self.streams[q].append((waits, fn, (sem, 16)))
        self._book((sem, 16 * self.dcount[id(sem)], True, q), reads, writes)

    def barrier(self):
        if not self.enabled:
            return
        for e in ENG:
            waits = []
            k = self.known[e]
            for e2 in ENG:
                if self.cnt[e2] > 0 and not (e == 'pe' and e2 == 'pe'):
                    sm, val = self._eev(e2)
                    if k.get(id(sm), 0) < val:
                        k[id(sm)] = val
                        waits.append((sm, val))
            for sm in self.dsems:
                val = 16 * self.dcount[id(sm)]
                if val > 0 and k.get(id(sm), 0) < val:
                    k[id(sm)] = val
                    waits.append((sm, val))
            if waits:
                self.streams[e].append((waits, None, None))

    def emit(self, blk):
        emap = {'pe': blk.tensor, 'act': blk.scalar, 'dve': blk.vector, 'pool': blk.gpsimd, 'sp': blk.sync}
        for e in ENG:
            def body(eng, e=e):
                for waits, fn, inc in self.streams[e]:
                    for sm, val in waits:
                        eng.wait_ge(sm, val)
                    if fn is not None:
                        fn(eng).then_inc(inc[0], inc[1])
            emap[e](body)


def build(stop=99, dbg=False):
    nc = bass.Bass("TRN2", target_bir_lowering=False)
    SK = "ExternalOutput" if dbg else "Internal"

    def din(name, shape, dt=F32):
        return nc.dram_tensor(name, list(shape), dt, kind="ExternalInput").ap()

    xall = din("xall", [NCTX + NT, D])
    condT_d = din("condT", [128, 16])
    pos_d = din("pos", [1, NCTX + NT], I32)
    flag_d = din("flag", [128, 1])
    w_ada = din("w_ada", [D, 6 * D])
    b_adaT = din("b_adaT", [128, 96])
    gn1T_d = din("gn1T", [128, 16])
    gn2T_d = din("gn2T", [128, 16])
    w_in = din("w_in", [D, DIN])
    w_out = din("w_out", [D, D])
    gmixT_d = din("gmixT", [128, 16])
    sgu_wT_d = din("sgu_wT", [128, 4, 128])
    sgu_bT_d = din("sgu_bT", [128, 4])
    lng_d = din("lng", [1, 512])
    lnb_d = din("lnb", [1, 512])
    w_q = din("w_q", [D, D])
    skT_d = din("skT", [128, 16, 128])
    peer_u = din("peer_u", [16384, D])
    peer_v = din("peer_v", [16384, D])
    gfin_d = din("gfin", [1, D])
    inv2_d = din("inv2", [128, 1])
    sgn_d = din("sgn", [128, 1])
    maskW_d = din("maskW", [128, 17 * 128], BF16)
    perm_d = din("perm", [128, 128], BF16)
    identf_d = din("identf", [128, 128])
    iota_d = din("iota16", [128, 16])
    tril_d = din("trilT", [128, 128])
    out_d = nc.dram_tensor("out", [NT, D], F32, kind="ExternalOutput").ap()

    dbgmod = nc.dram_tensor("dbgmod", [128, 128], F32, kind=SK).ap()
    dbgidx = nc.dram_tensor("dbgidx", [128, 2048], I32, kind=SK).ap()
    dbgg = nc.dram_tensor("dbgg", [128, 2048], F32, kind=SK).ap()
    KTs = nc.dram_tensor("KTs", [12, 128, NCTX + NT], BF16, kind=SK).ap()
    QTs = nc.dram_tensor("QTs", [12, 128, NT], BF16, kind=SK).ap()
    Vs = nc.dram_tensor("Vs", [NCTX + NT, 12 * 129], BF16, kind=SK).ap()
    UVs = nc.dram_tensor("UVs", [NT, 1024], F32, kind=SK).ap()
    MTs = nc.dram_tensor("MTs", [16, 128, 2048], BF16, kind=SK).ap()
    H1s = nc.dram_tensor("H1s", [NT, D], F32, kind=SK).ap()
    HN2s = nc.dram_tensor("HN2s", [NT, D], BF16, kind=SK).ap()
    HN2Ts = nc.dram_tensor("HN2Ts", [16, 128, 2048], BF16, kind=SK).ap()
    UV16 = nc.dram_tensor("UV16", [16384, 4096], BF16).ap()

    with ExitStack() as es:
        kb = KB(nc, es)

        uniq = [0]

        def alloc(st, name, shape, dt):
            uniq[0] += 1
            return st.enter_context(nc.sbuf_tensor("s%d_%s" % (uniq[0], name), list(shape), dt))

        def palloc(st, name, shape, dt):
            uniq[0] += 1
            return st.enter_context(nc.psum_tensor("p%d_%s" % (uniq[0], name), list(shape), dt))

        qrr = [0]

        def ldq():
            qrr[0] += 1
            return 'sp' if qrr[0] % 2 else 'act'

        def load(dst_ap, src_ap, buf, sem, q=None, reads=()):
            kb.dma(q or 'sp', lambda e: e.dma_start(out=dst_ap, in_=src_ap), kb.buf_sem(buf, 'l'), reads=(), writes=[buf])

        def store(dst_ap, src_ap, srcbuf, dstbuf, sem, q=None):
            kb.dma(q or 'sp', lambda e: e.dma_start(out=dst_ap, in_=src_ap), kb.buf_sem(srcbuf, 's'), reads=[srcbuf], writes=())

        cast_sem = kb.dsem('cast')
        cast_todo = []
        for cch in range(16):
            rows = slice(cch * 1024, (cch + 1) * 1024)
            cast_todo.append((UV16[rows, 0:2048], peer_u[rows, :]))
            cast_todo.append((UV16[rows, 2048:4096], peer_v[rows, :]))

        def emit_cast(k):
            for _ in range(k):
                if cast_todo:
                    o_ap, i_ap = cast_todo.pop(0)
                    kb.dma('pool', lambda e, o_ap=o_ap, i_ap=i_ap: e.dma_start(out=o_ap, in_=i_ap), cast_sem)

        def flush_cast():
            emit_cast(len(cast_todo))

        G = es
        identf = alloc(G, "identf", [128, 128], F32)
        identb = alloc(G, "identb", [128, 128], BF16)
        onesf = alloc(G, "onesf", [128, 128], F32)
        perm = alloc(G, "perm", [128, 128], BF16)
        flag = alloc(G, "flag", [128, 1], F32)
        onec = alloc(G, "onec", [128, 1], F32)
        modT = alloc(G, "modT", [128, 96], F32)
        gs1T = alloc(G, "gs1T", [128, 16], F32)
        gs2T = alloc(G, "gs2T", [128, 16], F32)
        epsc = alloc(G, "epsc", [128, 1], F32)
        epsl = alloc(G, "epsl", [128, 1], F32)
        bC = Buf('const')
        bMod = Buf('mod')
        sC = kb.dsem('const')
        load(identf[:], identf_d, bC, sC)
        load(perm[:], perm_d, bC, sC)
        load(flag[:], flag_d, bC, sC)
        kb.op('dve', lambda e: e.tensor_copy(out=identb[:], in_=identf[:]), reads=[bC], writes=[bC])
        kb.op('dve', lambda e: e.memset(onesf[:], 1.0), writes=[bC])
        kb.op('dve', lambda e: e.memset(onec[:], 1.0), writes=[bC])
        kb.op('dve', lambda e: e.memset(epsc[:, 0:1], 1e-6), writes=[bC])
        kb.op('dve', lambda e: e.memset(epsl[:, 0:1], 1e-5), writes=[bC])

        def rstd_from_ss(ss, rstd, bss, brs, n, epscol=0):
            kb.op('act', lambda e: e.activation(out=rstd, in_=ss, func=AF.Sqrt, bias=epsc[:, 0:1], scale=1.0 / n),
                  reads=[bss, bC], writes=[brs])
            kb.op('dve', lambda e: e.reciprocal(out=rstd, in_=rstd), reads=[brs], writes=[brs])

        def make_bc(st, dst, dstbuf, srcT, srcbuf, ps4, psbuf, tmpd, tmpbuf):
            for dc in range(16):
                kb.op('dve', lambda e, dc=dc: e.tensor_scalar(out=tmpd[:], in0=identf[:], scalar1=srcT[:, dc:dc + 1], scalar2=None, op0=ALU.mult),
                      reads=[bC, srcbuf], writes=[tmpbuf])
                kb.op('pe', lambda e, dc=dc: e.matmul(ps4[dc // 4][:, (dc % 4) * 128:(dc % 4 + 1) * 128], lhsT=onesf[:], rhs=tmpd[:], start=True, stop=True),
                      reads=[bC, tmpbuf], writes=[psbuf])
            for b4 in range(4):
                kb.op('act', lambda e, b4=b4: e.copy(out=dst[:, b4 * 512:(b4 + 1) * 512], in_=ps4[b4][:]), reads=[psbuf], writes=[dstbuf])

        with ExitStack() as P1:
            condT = alloc(P1, "condT", [128, 16], F32)
            badaT = alloc(P1, "badaT", [128, 96], F32)
            gnT = alloc(P1, "gnT", [128, 32], F32)
            wst = [alloc(P1, "wst%d" % i, [128, 16, 512], F32) for i in range(3)]
            psmod = palloc(P1, "psmod", [128, 512], F32)
            bcond = Buf(); bw = [Buf(), Buf(), Buf()]; bps = PSB()
            sw = [kb.dsem('wada0'), kb.dsem('wada1')]
            load(condT[:], condT_d, bcond, sC)
            load(badaT[:], b_adaT, bcond, sC)
            load(gnT[:, 0:16], gn1T_d, bcond, sC)
            load(gnT[:, 16:32], gn2T_d, bcond, sC)
            kb.op('act', lambda e: e.activation(out=condT[:], in_=condT[:], func=AF.Silu), reads=[bcond], writes=[bcond])
            wsb = [alloc(P1, "wsb%d" % i, [128, 16, 512], BF16) for i in range(2)]
            condb = alloc(P1, "condb", [128, 16], BF16)
            bwsb = [Buf(), Buf()]
            kb.op('dve', lambda e: e.tensor_copy(out=condb[:], in_=condT[:]), reads=[bcond], writes=[bcond])
            wv = w_ada.rearrange("(kc p) f -> p kc f", p=128)
            for blk in range(24):
                i3 = blk % 3
                i = blk % 2
                load(wst[i3][:], wv[:, :, blk * 512:(blk + 1) * 512], bw[i3], None, q=ldq())
                kb.op('dve' if i == 0 else 'pool', lambda e, i=i, i3=i3: e.tensor_copy(out=wsb[i][:], in_=wst[i3][:]), reads=[bw[i3]], writes=[bwsb[i]])
                for j in range(4):
                    col = blk * 4 + j
                    for kc in range(16):
                        kb.op('pe', lambda e, i=i, j=j, kc=kc, col=col: e.matmul(
                            psmod[:, col:col + 1], lhsT=wsb[i][:, kc, j * 128:(j + 1) * 128], rhs=condb[:, kc:kc + 1],
                            start=(kc == 0), stop=(kc == 15)), reads=[bwsb[i], bcond], writes=[bps])
            kb.op('dve', lambda e: e.tensor_tensor(out=modT[:], in0=psmod[:, 0:96], in1=badaT[:], op=ALU.add), reads=[bps, bcond], writes=[bMod])
            kb.op('dve', lambda e: e.scalar_tensor_tensor(out=gs1T[:], in0=modT[:, 16:32], scalar=1.0, in1=gnT[:, 0:16], op0=ALU.add, op1=ALU.mult),
                  reads=[bMod, bcond], writes=[bMod])
            kb.op('dve', lambda e: e.scalar_tensor_tensor(out=gs2T[:], in0=modT[:, 64:80], scalar=1.0, in1=gnT[:, 16:32], op0=ALU.add, op1=ALU.mult),
                  reads=[bMod, bcond], writes=[bMod])
            if dbg:
                sdb = kb.dsem('dbg')
                bdb = Buf()
                store(dbgmod[:, 0:96], modT[:], bMod, bdb, sdb)
                store(dbgmod[:, 96:112], gs1T[:], bMod, bdb, sdb)
                store(dbgmod[:, 112:128], gs2T[:], bMod, bdb, sdb)
            kb.barrier()
        if stop <= 1:
            kb.enabled = False
        sh1T = modT[:, 0:16]
        gt1T = modT[:, 32:48]
        sh2T = modT[:, 48:64]
        gt2T = modT[:, 80:96]

        with ExitStack() as P2:
            ropetab = alloc(P2, "ropetab", [128, 2, 4096], F32)
            sinT = ropetab[:, 0, :]
            cosT = ropetab[:, 1, :]
            bTab = Buf(); bhnT = Buf()
            with ExitStack() as P2r:
                posi = alloc(P2r, "posi", [128, 4096], I32)
                y = alloc(P2r, "ry", [128, 4096], F32)
                y2 = alloc(P2r, "ry2", [128, 2, 4096], F32)
                yi = alloc(P2r, "ryi", [128, 2, 4096], I32)
                m = alloc(P2r, "rm", [128, 2, 4096], F32)
                inv2 = alloc(P2r, "inv2", [128, 1], F32)
                sgn = alloc(P2r, "sgn", [128, 1], F32)
                bR = Buf()
                load(posi[:], pos_d.to_broadcast([128, 4096]), bR, sC)
                load(inv2[:], inv2_d, bR, sC)
                load(sgn[:], sgn_d, bR, sC)
                kb.op('dve', lambda e: e.tensor_copy(out=y[:], in_=posi[:]), reads=[bR], writes=[bR])
                kb.op('dve', lambda e: e.tensor_scalar(out=y2[:, 0, :], in0=y[:], scalar1=inv2[:, 0:1], scalar2=None, op0=ALU.mult), reads=[bR], writes=[bR])
                kb.op('dve', lambda e: e.tensor_scalar(out=y2[:, 1, :], in0=y[:], scalar1=inv2[:, 0:1], scalar2=0.25, op0=ALU.mult, op1=ALU.add), reads=[bR], writes=[bR])
                kb.op('dve', lambda e: e.tensor_copy(out=yi[:], in_=y2[:]), reads=[bR], writes=[bR])
                kb.op('dve', lambda e: e.tensor_copy(out=m[:], in_=yi[:]), reads=[bR], writes=[bR])
                kb.op('dve', lambda e: e.tensor_tensor(out=y2[:], in0=y2[:], in1=m[:], op=ALU.subtract), reads=[bR], writes=[bR])
                kb.op('dve', lambda e: e.tensor_scalar(out=m[:], in0=y2[:], scalar1=0.5, scalar2=None, op0=ALU.is_gt), reads=[bR], writes=[bR])
                kb.op('dve', lambda e: e.tensor_tensor(out=y2[:], in0=y2[:], in1=m[:], op=ALU.subtract), reads=[bR], writes=[bR])
                kb.op('dve', lambda e: e.tensor_scalar(out=m[:], in0=y2[:], scalar1=-0.5, scalar2=None, op0=ALU.is_lt), reads=[bR], writes=[bR])
                kb.op('dve', lambda e: e.tensor_tensor(out=y2[:], in0=y2[:], in1=m[:], op=ALU.add), reads=[bR], writes=[bR])
                kb.op('act', lambda e: e.activation(out=ropetab[:], in_=y2[:], func=AF.Sin, scale=2.0 * np.pi * (1.0 - 1e-6)),
                      reads=[bR], writes=[bTab])
                kb.op('dve', lambda e: e.tensor_scalar(out=ropetab[:, 0, :], in0=ropetab[:, 0, :], scalar1=sgn[:, 0:1], scalar2=None, op0=ALU.mult), reads=[bTab, bR], writes=[bTab])
                kb.barrier()
            if stop <= 1.5:
                kb.enabled = False
            hnT = alloc(P2, "hnT", [128, 16, 2048], BF16)

            xv = xall.rearrange("(n p) d -> n p d", p=128)
            Vsv = Vs.rearrange("(n p) c -> n p c", p=128)
            UVv = UVs.rearrange("(n p) c -> n p c", p=128)
            bKTs = Buf(); bQTs = Buf(); bVs = Buf(); bUVs = Buf()
            wiv = w_in.rearrange("(kc p) f -> p kc f", p=128)
            def do_half(half):
                with ExitStack() as PA:
                    xst = [alloc(PA, "xst%d" % i, [128, 2048], F32) for i in range(2)]
                    xnb = [alloc(PA, "xnb%d" % i, [128, 2048], BF16) for i in range(2)]
                    junk = alloc(PA, "junkA", [128, 2048], BF16)
                    ss = [alloc(PA, "ssA%d" % i, [128, 2], F32) for i in range(2)]
                    pT = [palloc(PA, "pTA%d" % i, [128, 2048], BF16) for i in range(2)]
                    bx = [Buf(), Buf()]; bxn = [Buf(), Buf()]; bj = Buf(); bss = [Buf(), Buf()]; brs = [Buf(), Buf()]; bpT = [PSB(), PSB()]
                    sx = [kb.dsem('xA0_%d' % half), kb.dsem('xA1_%d' % half)]
                    for tt in range(16):
                        i = tt % 2
                        gt = half * 16 + tt
                        load(xst[i][:], xv[gt], bx[i], sx[i], q=ldq())
                        kb.op('act', lambda e, i=i: e.activation(out=junk[:], in_=xst[i][:], func=AF.Square, accum_out=ss[i][:, 0:1]),
                              reads=[bx[i]], writes=[bj, bss[i]])
                        rstd_from_ss(ss[i][:, 0:1], ss[i][:, 1:2], bss[i], brs[i], float(D))
                        kb.op('dve', lambda e, i=i: e.tensor_scalar(out=xnb[i][:], in0=xst[i][:], scalar1=ss[i][:, 1:2], scalar2=None, op0=ALU.mult),
                              reads=[bx[i], brs[i]], writes=[bxn[i]])
                        import os
                        KS = os.environ.get('KSKIP', '')
                        for dc in (range(16) if KS != 'a1' else ()):
                            kb.op('pe', lambda e, i=i, dc=dc: e.transpose(out=pT[i][:, dc * 128:(dc + 1) * 128], in_=xnb[i][:, dc * 128:(dc + 1) * 128], identity=identb[:]),
                                  reads=[bxn[i], bC], writes=[bpT[i]])
                        for dc in (range(16) if KS not in ('a1', 'a2') else ()):
                            if True:
                                kb.op('dve', lambda e, i=i, dc=dc, tt=tt: e.tensor_scalar(
                                    out=hnT[:, dc, tt * 128:(tt + 1) * 128], in0=pT[i][:, dc * 128:(dc + 1) * 128],
                                    scalar1=gs1T[:, dc:dc + 1], scalar2=sh1T[:, dc:dc + 1], op0=ALU.mult, op1=ALU.add),
                                    reads=[bpT[i], bMod], writes=[bhnT])
                            else:
                                kb.op('act', lambda e, i=i, dc=dc, tt=tt: e.activation(
                                    out=hnT[:, dc, tt * 128:(tt + 1) * 128], in_=pT[i][:, dc * 128:(dc + 1) * 128],
                                    func=AF.Identity, scale=(1.0 if KS == 'a4' else gs1T[:, dc:dc + 1]), bias=(0.0 if KS in ('a4', 'a5') else sh1T[:, dc:dc + 1])),
                                    reads=[bpT[i], bMod], writes=[bhnT])
                    kb.barrier()
                if stop <= 1.6 + 0.2 * half:
                    kb.enabled = False
                with ExitStack() as PB:
                    wf = [alloc(PB, "wf%d" % i, [128, 16, 256], F32) for i in range(2)]
                    wb = [alloc(PB, "wb%d" % i, [128, 16, 512], BF16) for i in range(2)]
                    qb = [alloc(PB, "qb%d" % i, [128, 512], BF16) for i in range(2)]
                    t1 = [alloc(PB, "t1%d" % i, [128, 512], F32) for i in range(2)]
                    t2 = [alloc(PB, "t2%d" % i, [128, 512], F32) for i in range(2)]
                    ro = [alloc(PB, "ro%d" % i, [128, 512], BF16) for i in range(2)]
                    vst = [alloc(PB, "vst%d" % i, [128, 4, 129], BF16) for i in range(2)]
                    uvst = [alloc(PB, "uvst%d" % i, [128, 512], F32) for i in range(2)]
                    pq = [palloc(PB, "pq%d" % i, [128, 512], F32) for i in range(2)]
                    psw = [palloc(PB, "psw%d" % i, [128, 512], F32) for i in range(2)]
                    pv = [palloc(PB, "pv%d" % i, [128, 512], F32) for i in range(2)]
                    bwf = [Buf(), Buf()]; bwb = [Buf(), Buf()]; bqb = [Buf(), Buf()]; bt1 = [Buf(), Buf()]; bt2 = [Buf(), Buf()]
                    bro = [Buf(), Buf()]; bvst = [Buf(), Buf()]; buv = [Buf(), Buf()]; bpq = [PSB(), PSB()]; bpsw = [PSB(), PSB()]; bpv = [PSB(), PSB()]
                    swf = [kb.dsem('wf0_%d' % half), kb.dsem('wf1_%d' % half)]
                    sro = [kb.dsem('ro0_%d' % half), kb.dsem('ro1_%d' % half)]
                    svs = [kb.dsem('vs0_%d' % half), kb.dsem('vs1_%d' % half)]
                    suv = [kb.dsem('uv0_%d' % half), kb.dsem('uv1_%d' % half)]
                    fl = flag if half == 0 else onec
                    for i in range(2):
                        kb.op('dve', lambda e, i=i: e.tensor_copy(out=vst[i][:, :, 128:129], in_=fl[:, 0:1].unsqueeze(1).to_broadcast([128, 4, 1])),
                              reads=[bC], writes=[bvst[i]])
                    blocks = [3, 4, 5, 6, 7, 8] if half == 0 else list(range(11))
                    cnt_fm = 0
                    cnt_tm = 0
                    for bi, blk in enumerate(blocks):
                        i = bi % 2
                        for hc in range(2):
                            load(wf[hc][:], wiv[:, :, blk * 512 + hc * 256:blk * 512 + (hc + 1) * 256], bwf[hc], swf[hc], q=ldq())
                            kb.op('pool', lambda e, i=i, hc=hc: e.tensor_copy(out=wb[i][:, :, hc * 256:(hc + 1) * 256], in_=wf[hc][:]),
                                  reads=[bwf[hc]], writes=[bwb[i]])
                        import os
                        KS = os.environ.get('KSKIP', '')
                        if KS == 'b1' or (KS == 'b2' and blk >= 6) or (KS == 'b3' and blk < 6):
                            continue
                        if blk < 6:
                            isq = blk < 3
                            for hh in range(4):
                                head = (blk % 3) * 4 + hh
                                for tg in range(4):
                                    c = cnt_fm % 2
                                    cnt_fm += 1
                                    for kc in range(16):
                                        kb.op('pe', lambda e, i=i, c=c, kc=kc, hh=hh, tg=tg: e.matmul(
                                            pq[c][:], lhsT=wb[i][:, kc, hh * 128:(hh + 1) * 128], rhs=hnT[:, kc, tg * 512:(tg + 1) * 512],
                                            start=(kc == 0), stop=(kc == 15)), reads=[bwb[i], bhnT], writes=[bpq[c]])
                                    tok0 = half * 2048 + tg * 512
                                    kb.op('act', lambda e, c=c: e.copy(out=qb[c][:], in_=pq[c][:]), reads=[bpq[c]], writes=[bqb[c]])
                                    kb.op('dve', lambda e, c=c, tok0=tok0: e.tensor_tensor(out=t1[c][:], in0=pq[c][:], in1=cosT[:, tok0:tok0 + 512], op=ALU.mult),
                                          reads=[bpq[c], bTab], writes=[bt1[c]])
                                    kb.op('pe', lambda e, c=c: e.matmul(psw[c][:], lhsT=perm[:], rhs=qb[c][:], start=True, stop=True),
                                          reads=[bC, bqb[c]], writes=[bpsw[c]])
                                    kb.op('dve', lambda e, c=c, tok0=tok0: e.tensor_tensor(out=t2[c][:], in0=psw[c][:], in1=sinT[:, tok0:tok0 + 512], op=ALU.mult),
                                          reads=[bpsw[c], bTab], writes=[bt2[c]])
                                    kb.op('pool', lambda e, c=c: e.tensor_tensor(out=ro[c][:], in0=t1[c][:], in1=t2[c][:], op=ALU.add),
                                          reads=[bt1[c], bt2[c]], writes=[bro[c]])
                                    if isq:
                                        store(QTs[head, :, tg * 512:(tg + 1) * 512], ro[c][:], bro[c], bQTs, sro[c], q='sp')
                                    else:
                                        store(KTs[head, :, tok0:tok0 + 512], ro[c][:], bro[c], bKTs, sro[c], q='sp')
                        else:
                            for tt in range(16):
                                c = cnt_tm % 2
                                cnt_tm += 1
                                for kc in range(16):
                                    kb.op('pe', lambda e, i=i, c=c, kc=kc, tt=tt: e.matmul(
                                        pv[c][:], lhsT=hnT[:, kc, tt * 128:(tt + 1) * 128], rhs=wb[i][:, kc, :],
                                        start=(kc == 0), stop=(kc == 15)), reads=[bwb[i], bhnT], writes=[bpv[c]])
                                gt = half * 16 + tt
                                if blk < 9:
                                    hb = blk - 6
                                    kb.op('act', lambda e, c=c: e.activation(out=vst[c][:, :, 0:128], in_=pv[c][:].rearrange("p (h e) -> p h e", h=4),
                                                                            func=AF.Identity, scale=fl[:, 0:1]),
                                          reads=[bpv[c], bC], writes=[bvst[c]])
                                    store(Vsv[gt][:, hb * 516:(hb + 1) * 516], vst[c][:].rearrange("p h e -> p (h e)"), bvst[c], bVs, svs[c], q='act')
                                else:
                                    kb.op('act', lambda e, c=c: e.activation(out=uvst[c][:], in_=pv[c][:], func=AF.Gelu), reads=[bpv[c]], writes=[buv[c]])
                                    store(UVv[tt][:, (blk - 9) * 512:(blk - 8) * 512], uvst[c][:], buv[c], bUVs, suv[c], q='act')
                    kb.barrier()

            do_half(0)
            do_half(1)
        if stop <= 2:
            kb.enabled = False

        bMTs = Buf()
        with ExitStack() as P3:
            KTc = [alloc(P3, "KTc%d" % i, [128, 4, NW * 128], BF16) for i in range(2)]
            Vc = [alloc(P3, "Vc%d" % i, [128, NW, 4 * 129], BF16) for i in range(2)]
            QTg = alloc(P3, "QTg", [128, 12, QG * 128], BF16)
            mixed = alloc(P3, "mixed", [128, QG, 2048], F32)
            maskW = alloc(P3, "maskW", [128, 17 * 128], BF16)
            Eb = [alloc(P3, "Eb%d" % i, [128, 512], BF16) for i in range(3)]
            Pb = [alloc(P3, "Pb%d" % i, [128, 512], BF16) for i in range(3)]
            rec = alloc(P3, "rec", [128, QG], F32)
            uvt = alloc(P3, "uvt", [128, 1024], F32)
            lng = alloc(P3, "lng", [128, 512], F32)
            lnb = alloc(P3, "lnb", [128, 512], F32)
            wsTf = alloc(P3, "wsTf", [128, 4, 128], F32)
            wsT = alloc(P3, "wsT", [128, 4, 128], BF16)
            tril = alloc(P3, "tril", [128, 128], F32)
            sbT = alloc(P3, "sbT", [128, 4], F32)
            bst = alloc(P3, "bst", [128, 4, 6], F32)
            mv = alloc(P3, "mv", [128, 4, 2], F32)
            lrs = alloc(P3, "lrs", [128, 4], F32)
            vn = alloc(P3, "vn", [128, 512], F32)
            vnb = alloc(P3, "vnb", [128, 512], BF16)
            junk3 = alloc(P3, "junk3", [128, 1536], BF16)
            ss3 = alloc(P3, "ss3", [128, 4], F32)
            mnb = alloc(P3, "mnb", [128, 2048], BF16)
            mnT = alloc(P3, "mnT", [128, 2048], BF16)
            pS = [palloc(P3, "pS%d" % i, [128, 512], F32) for i in range(3)]
            pO = [palloc(P3, "pO%d" % i, [128, 512], F32) for i in range(QG)]
            pM = palloc(P3, "pM", [128, 512], F32)
            pT3 = palloc(P3, "pT3", [128, 2048], BF16)
            b3c = Buf(); bKTc = [Buf(), Buf()]; bVc = [Buf(), Buf()]; bQTg = Buf(); bmixed = Buf()
            bEb = [Buf(), Buf(), Buf()]; bPb = [Buf(), Buf(), Buf()]; brec = Buf(); buvt = Buf()
            bpS = [PSB(), PSB(), PSB()]; bpO = [PSB() for _ in range(QG)]; bpM = PSB(); bpT3 = PSB()
            bvn = Buf(); bvnb = Buf(); bstat = Buf(); bj3 = Buf(); bss3 = Buf(); brs3 = Buf(); bmnb = Buf(); bmnT = Buf()
            s3c = kb.dsem('c3'); sK = kb.dsem('ktg'); sV = kb.dsem('vg'); sQ = kb.dsem('qtg'); sUV = kb.dsem('uvt'); sMT = kb.dsem('mnT')
            load(maskW[:], maskW_d, b3c, s3c)
            load(lng[:], lng_d.to_broadcast([128, 512]), b3c, s3c)
            load(lnb[:], lnb_d.to_broadcast([128, 512]), b3c, s3c)
            load(wsTf[:], sgu_wT_d, b3c, s3c)
            load(tril[:], tril_d, b3c, s3c)
            load(sbT[:], sgu_bT_d, b3c, s3c)
            for hh in range(4):
                kb.op('dve', lambda e, hh=hh: e.tensor_tensor(out=wsT[:, hh, :], in0=wsTf[:, hh, :], in1=tril[:], op=ALU.mult), reads=[b3c], writes=[b3c])
            KTv = KTs.rearrange("h e t -> e h t")
            QTv = QTs.rearrange("h e t -> e h t")
            Vwv = Vs.rearrange("(n p) c -> p n c", p=128)
            UVv = UVs.rearrange("(n p) c -> n p c", p=128)

            uvt2 = [uvt, alloc(P3, "uvtB", [128, 1024], F32)]
            vn2 = [vn, alloc(P3, "vnB", [128, 512], F32)]
            vnb2 = [vnb, alloc(P3, "vnbB", [128, 512], BF16)]
            mnb2 = [mnb, alloc(P3, "mnbB", [128, 2048], BF16)]
            ss32 = [ss3, alloc(P3, "ss3B", [128, 4], F32)]
            bst2 = [bst, alloc(P3, "bstB", [128, 4, 6], F32)]
            mv2 = [mv, alloc(P3, "mvB", [128, 4, 2], F32)]
            lrs2 = [lrs, alloc(P3, "lrsB", [128, 4], F32)]
            buvt2 = [buvt, Buf()]; bvn2 = [bvn, Buf()]; bvnb2 = [bvnb, Buf()]; bmnb2 = [bmnb, Buf()]; bss32 = [bss3, Buf()]; brs32 = [brs3, Buf()]; bstat2 = [bstat, Buf()]

            def tail_part(g, part):
                for j in range(QG):
                    tail_tile(g, part, j)

            def tail_tile(g, part, j):
                mx = mixed2[g % 2]
                bmx = bmixed2[g % 2]
                if True:
                    ot = g * QG + j
                    uvt_, vn_, vnb_, mnb_, ss_, bst_, mv_, lrs_ = uvt2[j], vn2[j], vnb2[j], mnb2[j], ss32[j], bst2[j], mv2[j], lrs2[j]
                    buvt_, bvn_, bvnb_, bmnb_, bss_, brs_, bstat_ = buvt2[j], bvn2[j], bvnb2[j], bmnb2[j], bss32[j], brs32[j], bstat2[j]
                    if part == 1:
                        load(uvt_[:], UVv[ot], buvt_, None, q='sp')
                        for hh in range(4):
                            kb.op('dve', lambda e, hh=hh: e.bn_stats(out=bst_[:, hh, :], in_=uvt_[:, 512 + hh * 128:512 + (hh + 1) * 128]), reads=[buvt_], writes=[bstat_])
                        for hh in range(4):
                            kb.op('dve', lambda e, hh=hh: e.bn_aggr(out=mv_[:, hh, :], in_=bst_[:, hh, :]), reads=[bstat_], writes=[bstat_])
                        kb.op('act', lambda e: e.activation(out=lrs_[:], in_=mv_[:, :, 1], func=AF.Sqrt, bias=epsl[:, 0:1], scale=1.0), reads=[bstat_, bC], writes=[bstat_])
                        kb.op('dve', lambda e: e.reciprocal(out=lrs_[:], in_=lrs_[:]), reads=[bstat_], writes=[bstat_])
                        for hh in range(4):
                            kb.op('dve', lambda e, hh=hh: e.tensor_scalar(out=vn_[:, hh * 128:(hh + 1) * 128], in0=uvt_[:, 512 + hh * 128:512 + (hh + 1) * 128],
                                                                        scalar1=mv_[:, hh, 0:1], scalar2=lrs_[:, hh:hh + 1], op0=ALU.subtract, op1=ALU.mult),
                                  reads=[buvt_, bstat_], writes=[bvn_])
                        kb.op('pool', lambda e: e.tensor_tensor(out=vn_[:], in0=vn_[:], in1=lng[:], op=ALU.mult), reads=[bvn_, b3c], writes=[bvn_])
                        kb.op('pool', lambda e: e.tensor_tensor(out=vnb_[:], in0=vn_[:], in1=lnb[:], op=ALU.add), reads=[bvn_, b3c], writes=[bvnb_])
                    elif part == 2:
                        for hh in range(4):
                            kb.op('pe', lambda e, hh=hh: e.matmul(pM[:, hh * 128:(hh + 1) * 128], lhsT=wsT[:, hh, :], rhs=vnb_[:, hh * 128:(hh + 1) * 128],
                                                                 start=True, stop=True), reads=[b3c, bvnb_], writes=[bpM])
                        for hh in range(4):
                            kb.op('dve', lambda e, hh=hh, j=j: e.scalar_tensor_tensor(
                                out=mx[:, j, 1536 + hh * 128:1536 + (hh + 1) * 128], in0=pM[:, hh * 128:(hh + 1) * 128], scalar=sbT[:, hh:hh + 1],
                                in1=uvt_[:, hh * 128:(hh + 1) * 128], op0=ALU.add, op1=ALU.mult), reads=[bpM, b3c, buvt_], writes=[bmx])
                        kb.op('act', lambda e, j=j: e.activation(out=junk3[:, 0:1536], in_=mx[:, j, 0:1536], func=AF.Square, accum_out=ss_[:, 0:1]),
                              reads=[bmx], writes=[bj3, bss_])
                        kb.op('act', lambda e, j=j: e.activation(out=junk3[:, 0:512], in_=mx[:, j, 1536:2048], func=AF.Square, accum_out=ss_[:, 1:2]),
                              reads=[bmx], writes=[bj3, bss_])
                        rstd_from_ss(ss_[:, 0:1], ss_[:, 2:3], bss_, brs_, 1536.0)
                        rstd_from_ss(ss_[:, 1:2], ss_[:, 3:4], bss_, brs_, 512.0)
                        kb.op('dve', lambda e, j=j: e.tensor_scalar(out=mnb_[:, 0:1536], in0=mx[:, j, 0:1536], scalar1=ss_[:, 2:3], scalar2=None, op0=ALU.mult),
                              reads=[bmx, brs_], writes=[bmnb_])
                        kb.op('pool', lambda e, j=j: e.tensor_scalar(out=mnb_[:, 1536:2048], in0=mx[:, j, 1536:2048], scalar1=ss_[:, 3:4], scalar2=None, op0=ALU.mult),
                              reads=[bmx, brs_], writes=[bmnb_])
                    else:
                        for dc in range(16):
                            kb.op('pe', lambda e, dc=dc: e.transpose(out=pT3[:, dc * 128:(dc + 1) * 128], in_=mnb_[:, dc * 128:(dc + 1) * 128], identity=identb[:]),
                                  reads=[bmnb_, bC], writes=[bpT3])
                        kb.op('act', lambda e: e.copy(out=mnT[:, 0:1024], in_=pT3[:, 0:1024]), reads=[bpT3], writes=[bmnT])
                        kb.op('dve', lambda e: e.tensor_copy(out=mnT[:, 1024:2048], in_=pT3[:, 1024:2048]), reads=[bpT3], writes=[bmnT])
                        store(MTs[ot], mnT[:], bmnT, bMTs, None, q='sp')

            mixed2 = [mixed, alloc(P3, "mixedB", [128, QG, 2048], F32)]
            bmixed2 = [bmixed, Buf()]
            sc_exp = float(128 ** -0.5)
            for g in range(16 // QG):
                ws = g * QG
                def load_chunk(ci):
                    if ci >= 24:
                        return
                    g_, hc_ = divmod(ci, 3)
                    ws_ = g_ * QG
                    bb = ci % 2
                    load(KTc[bb][:], KTv[:, hc_ * 4:(hc_ + 1) * 4, ws_ * 128:(ws_ + NW) * 128], bKTc[bb], None, q='sp')
                    load(Vc[bb][:], Vwv[:, ws_:ws_ + NW, hc_ * 516:(hc_ + 1) * 516], bVc[bb], None, q='act')
                if g == 0:
                    load_chunk(0)
                load(QTg[:], QTv[:, :, ws * 128:(ws + QG) * 128], bQTg, sQ, q='sp', reads=[bQTs])
                for h in range(12):
                    ci = g * 3 + h // 4
                    cb = ci % 2
                    hl = h % 4
                    if h % 4 == 0:
                        load_chunk(ci + 1)
                    def kinfo(kt):
                        jlo = max(0, kt - 16); jhi = min(QG - 1, kt)
                        return jlo, jhi, (jhi - jlo + 1) * 128

                    def qkpair(kp, h=h, cb=cb, hl=hl):
                        c = kp % 3
                        for s2 in range(2):
                            kt = kp * 2 + s2
                            jlo, jhi, n = kinfo(kt)
                            kb.op('pe', lambda e, kt=kt, jlo=jlo, jhi=jhi, n=n, s2=s2, c=c: e.matmul(
                                pS[c][:, s2 * 256:s2 * 256 + n], lhsT=KTc[cb][:, hl, kt * 128:(kt + 1) * 128], rhs=QTg[:, h, jlo * 128:(jhi + 1) * 128],
                                start=True, stop=True), reads=[bKTc[cb], bQTg], writes=[bpS[c]])
                    qkpair(0)
                    qkpair(1)
                    for kp in range(NW // 2):
                        if kp + 2 < NW // 2:
                            qkpair(kp + 2)
                        c = kp % 3
                        n0 = kinfo(kp * 2)[2]; n1 = kinfo(kp * 2 + 1)[2]
                        if n0 == 256 and n1 == 256:
                            kb.op('act', lambda e, c=c: e.activation(out=Eb[c][:], in_=pS[c][:], func=AF.Exp, scale=sc_exp),
                                  reads=[bpS[c]], writes=[bEb[c]])
                        else:
                            for s2, nn in ((0, n0), (1, n1)):
                                kb.op('act', lambda e, c=c, s2=s2, nn=nn: e.activation(out=Eb[c][:, s2 * 256:s2 * 256 + nn], in_=pS[c][:, s2 * 256:s2 * 256 + nn],
                                                                                     func=AF.Exp, scale=sc_exp), reads=[bpS[c]], writes=[bEb[c]])
                        for s2 in range(2):
                            kt = kp * 2 + s2
                            jlo, jhi, n = kinfo(kt)
                            m0 = (16 + jlo - kt) * 128
                            kb.op('dve', lambda e, c=c, n=n, m0=m0, s2=s2: e.tensor_tensor(out=Pb[c][:, s2 * 256:s2 * 256 + n], in0=Eb[c][:, s2 * 256:s2 * 256 + n],
                                                                                         in1=maskW[:, m0:m0 + n], op=ALU.mult),
                                  reads=[bEb[c], b3c], writes=[bPb[c]])
                        for s2 in range(2):
                            kt = kp * 2 + s2
                            jlo, jhi, n = kinfo(kt)
                            for j in range(jlo, jhi + 1):
                                kb.op('pe', lambda e, c=c, j=j, jlo=jlo, kt=kt, h=h, s2=s2, cb=cb, hl=hl: e.matmul(
                                    pO[j][:, 0:129], lhsT=Pb[c][:, s2 * 256 + (j - jlo) * 128:s2 * 256 + (j - jlo + 1) * 128], rhs=Vc[cb][:, kt, hl * 129:(hl + 1) * 129],
                                    start=(kt == j), stop=(kt == 16 + j)), reads=[bPb[c], bVc[cb]], writes=[bpO[j]])
                    for j in range(QG):
                        kb.op('dve', lambda e, j=j: e.reciprocal(out=rec[:, j:j + 1], in_=pO[j][:, 128:129]), reads=[bpO[j]], writes=[brec])
                        kb.op('dve', lambda e, j=j, h=h, mxh=mixed2[g % 2]: e.tensor_scalar(out=mxh[:, j, h * 128:(h + 1) * 128], in0=pO[j][:, 0:128],
                                                                      scalar1=rec[:, j:j + 1], scalar2=None, op0=ALU.mult),
                              reads=[bpO[j], brec], writes=[bmixed2[g % 2]])
                    if g > 0 and h in (0, 1, 2):
                        tail_part(g - 1, h + 1)
                pass
            for part in (1, 2, 3):
                tail_part(16 // QG - 1, part)
            kb.barrier()
        if stop <= 3:
            kb.enabled = False

        bH1s = Buf(); bHN2s = Buf(); bHN2Ts = Buf()

        def load_weight_bf16(st, wdram, wdst, bwdst, scaleT, name):
            stg = [alloc(st, name + "stg%d" % i, [128, 2048], F32) for i in range(2)]
            bstg = [Buf(), Buf()]
            sst = [kb.dsem(name + 's0'), kb.dsem(name + 's1')]
            wv_ = wdram.rearrange("(kc p) f -> p kc f", p=128)
            for kc in range(16):
                i = kc % 2
                load(stg[i][:], wv_[:, kc, :], bstg[i], sst[i], q=ldq())
                if scaleT is None:
                    kb.op('pool', lambda e, i=i, kc=kc: e.tensor_copy(out=wdst[:, kc, :], in_=stg[i][:]), reads=[bstg[i]], writes=[bwdst])
                else:
                    kb.op('pool', lambda e, i=i, kc=kc: e.tensor_scalar(out=wdst[:, kc, :], in0=stg[i][:], scalar1=scaleT[:, kc:kc + 1],
                                                                       scalar2=None, op0=ALU.mult), reads=[bstg[i], bC], writes=[bwdst])

        with ExitStack() as P4:
            wo = alloc(P4, "wo", [128, 16, 2048], BF16)
            gmixT = alloc(P4, "gmixT", [128, 16], F32)
            gt1bc = alloc(P4, "gt1bc", [128, 2048], F32)
            gs2bc = alloc(P4, "gs2bc", [128, 2048], F32)
            sh2bc = alloc(P4, "sh2bc", [128, 2048], F32)
            dtmp = alloc(P4, "dtmp", [128, 128], F32)
            mnT4 = [alloc(P4, "mnT4%d" % i, [128, 16, 128], BF16) for i in range(2)]
            x4 = [alloc(P4, "x4%d" % i, [128, 2048], F32) for i in range(2)]
            tmp4 = alloc(P4, "tmp4", [128, 2048], F32)
            h14 = alloc(P4, "h14", [128, 2048], F32)
            junk4 = alloc(P4, "junk4", [128, 2048], BF16)
            ss4 = alloc(P4, "ss4", [128, 2], F32)
            hn2 = alloc(P4, "hn2", [128, 2048], BF16)
            hn2T = alloc(P4, "hn2T", [128, 2048], BF16)
            pW = [palloc(P4, "pW%d" % i, [128, 512], F32) for i in range(4)]
            pT4 = palloc(P4, "pT4", [128, 2048], BF16)
            bwo = Buf(); b4c = Buf(); bbc = Buf(); bdt = Buf(); bmn4 = [Buf(), Buf()]; bx4 = [Buf(), Buf()]; btmp4 = Buf(); bh14 = Buf()
            bj4 = Buf(); bss4 = Buf(); brs4 = Buf(); bhn2 = Buf(); bhn2T = Buf(); bpW = PSB(); bpT4 = PSB()
            s4c = kb.dsem('c4'); smn = [kb.dsem('mn40'), kb.dsem('mn41')]; sx4 = [kb.dsem('x40'), kb.dsem('x41')]
            sh1 = kb.dsem('h1st'); shn2 = kb.dsem('hn2st'); shn2T = kb.dsem('hn2Tst')
            load(gmixT[:], gmixT_d, bC, s4c)
            with ExitStack() as P4w:
                load_weight_bf16(P4w, w_out, wo, bwo, gmixT, "wo")
                make_bc(P4w, gt1bc, bbc, gt1T, bMod, pW, bpW, dtmp, bdt)
                make_bc(P4w, gs2bc, bbc, gs2T, bMod, pW, bpW, dtmp, bdt)
                make_bc(P4w, sh2bc, bbc, sh2T, bMod, pW, bpW, dtmp, bdt)
                kb.barrier()
            xov = xall.rearrange("(n p) d -> n p d", p=128)
            H1v = H1s.rearrange("(n p) d -> n p d", p=128)
            HN2v = HN2s.rearrange("(n p) d -> n p d", p=128)
            h14b = [h14, alloc(P4, "h14b", [128, 2048], F32)]
            ss4b = [ss4, alloc(P4, "ss4b", [128, 2], F32)]
            tmpB = alloc(P4, "tmpB4", [128, 2048], F32)
            bh14b = [bh14, Buf()]; bss4b = [bss4, Buf()]; brs4b = [brs4, Buf()]; btmpB = Buf()

            def stageA(ot):
                i = ot % 2
                load(mnT4[i][:], MTs[ot].rearrange("p (kc t) -> p kc t", kc=16), bmn4[i], None, q='sp')
                load(x4[i][:], xov[16 + ot], bx4[i], None, q='act')
                for nb in range(4):
                    for kc in range(16):
                        kb.op('pe', lambda e, i=i, nb=nb, kc=kc: e.matmul(pW[nb][:], lhsT=mnT4[i][:, kc, :], rhs=wo[:, kc, nb * 512:(nb + 1) * 512],
                                                                         start=(kc == 0), stop=(kc == 15)), reads=[bmn4[i], bwo], writes=[bpW])
                for nb in range(4):
                    kb.op('dve', lambda e, nb=nb: e.tensor_tensor(out=tmp4[:, nb * 512:(nb + 1) * 512], in0=pW[nb][:], in1=gt1bc[:, nb * 512:(nb + 1) * 512], op=ALU.mult),
                          reads=[bpW, bbc], writes=[btmp4])
                kb.op('pool', lambda e, i=i: e.tensor_tensor(out=h14b[i][:], in0=tmp4[:], in1=x4[i][:], op=ALU.add), reads=[btmp4, bx4[i]], writes=[bh14b[i]])
                store(H1v[ot], h14b[i][:], bh14b[i], bH1s, None, q='sp')
                kb.op('act', lambda e, i=i: e.activation(out=junk4[:], in_=h14b[i][:], func=AF.Square, accum_out=ss4b[i][:, 0:1]), reads=[bh14b[i]], writes=[bj4, bss4b[i]])
                rstd_from_ss(ss4b[i][:, 0:1], ss4b[i][:, 1:2], bss4b[i], brs4b[i], float(D))

            def stageB(ot):
                i = ot % 2
                kb.op('dve', lambda e, i=i: e.scalar_tensor_tensor(out=tmpB[:], in0=h14b[i][:], scalar=ss4b[i][:, 1:2], in1=gs2bc[:], op0=ALU.mult, op1=ALU.mult),
                      reads=[bh14b[i], brs4b[i], bbc], writes=[btmpB])
                kb.op('pool', lambda e: e.tensor_tensor(out=hn2[:], in0=tmpB[:], in1=sh2bc[:], op=ALU.add), reads=[btmpB, bbc], writes=[bhn2])
                store(HN2v[ot], hn2[:], bhn2, bHN2s, None, q='act')
                for dc in range(16):
                    kb.op('pe', lambda e, dc=dc: e.transpose(out=pT4[:, dc * 128:(dc + 1) * 128], in_=hn2[:, dc * 128:(dc + 1) * 128], identity=identb[:]),
                          reads=[bhn2, bC], writes=[bpT4])
                kb.op('act', lambda e: e.copy(out=hn2T[:, 0:1024], in_=pT4[:, 0:1024]), reads=[bpT4], writes=[bhn2T])
                kb.op('dve', lambda e: e.tensor_copy(out=hn2T[:, 1024:2048], in_=pT4[:, 1024:2048]), reads=[bpT4], writes=[bhn2T])
                store(HN2Ts[ot], hn2T[:], bhn2T, bHN2Ts, None, q='sp')

            stageA(0)
            for ot in range(16):
                if ot + 1 < 16:
                    stageA(ot + 1)
                stageB(ot)
            kb.barrier()
        if stop <= 4:
            kb.enabled = False

        with ExitStack() as P56:
            idxT = alloc(P56, "idxT", [128, 2048], I32)
            gT = alloc(P56, "gT", [128, 2048], F32)
            bidxT = Buf(); bgT = Buf()
            with ExitStack() as P5:
                wq = alloc(P5, "wq", [128, 16, 2048], BF16)
                skT = alloc(P5, "skT", [128, 16, 128], BF16)
                iota16 = alloc(P5, "iota16", [128, 16], F32)
                hg = alloc(P5, "hg", [128, 16, 512], BF16)
                qT = alloc(P5, "qT", [128, 16, 512], BF16)
                sc = alloc(P5, "sc", [128, 16, 128], F32)
                wk = alloc(P5, "wk", [128, 16, 128], F32)
                stv = alloc(P5, "stv", [128, 16, 16], F32)
                siv = alloc(P5, "siv", [128, 16, 16], U32)
                sif = alloc(P5, "sif", [128, 16, 16], F32)
                cand = alloc(P5, "cand", [128, 8, 256], F32)
                bs = alloc(P5, "bs", [128, 8, 16], F32)
                bp = alloc(P5, "bp", [128, 8, 16], U32)
                bpf = alloc(P5, "bpf", [128, 8, 16], F32)
                pi_ = alloc(P5, "pi_", [128, 8, 16], F32)
                pj_ = alloc(P5, "pj_", [128, 8, 16], F32)
                pii = alloc(P5, "pii", [128, 8, 16], I32)
                eq = alloc(P5, "eq", [128, 128, 16], F32)
                ea = alloc(P5, "ea", [128, 128], F32)
                ebb = alloc(P5, "ebb", [128, 128], F32)
                idxf = alloc(P5, "idxf", [128, 128], F32)
                gte = alloc(P5, "gte", [128, 8, 16], F32)
                gsum = alloc(P5, "gsum", [128, 8], F32)
                pQ = [palloc(P5, "pQ%d" % i, [128, 512], F32) for i in range(2)]
                pS5 = [palloc(P5, "pS5%d" % i, [128, 512], F32) for i in range(4)]
                pTr = palloc(P5, "pTr", [128, 512], F32)
                bwq = Buf(); b5c = Buf(); bhg = Buf(); bqT = Buf(); bsc = Buf(); bwk = Buf(); bst5 = Buf(); bcand = Buf(); bcwk = Buf()
                bbs = Buf(); bsel = Buf(); bidxf = Buf(); bgte = Buf(); bpQ = [PSB(), PSB()]; bpS5 = PSB(); bpTr = PSB()
                s5c = kb.dsem('c5'); shg = kb.dsem('hg')
                load(iota16[:], iota_d, b5c, s5c)
                with ExitStack() as P5w:
                    skTf = alloc(P5w, "skTf", [128, 16, 128], F32)
                    load(skTf[:], skT_d, b5c, s5c)
                    kb.op('dve', lambda e: e.tensor_copy(out=skT[:], in_=skTf[:]), reads=[b5c], writes=[b5c])
                    load_weight_bf16(P5w, w_q, wq, bwq, None, "wq")
                    kb.barrier()
                for g4 in range(4):
                    for jj in range(4):
                        load(hg[:, :, jj * 128:(jj + 1) * 128], HN2Ts[g4 * 4 + jj].rearrange("p (kc t) -> p kc t", kc=16), bhg, shg, q=ldq(), reads=[bHN2Ts])
                    emit_cast(32 if g4 == 0 else 0)
                    for hp in range(16):
                        c = hp % 2
                        for kc in range(16):
                            kb.op('pe', lambda e, c=c, kc=kc, hp=hp: e.matmul(pQ[c][:], lhsT=wq[:, kc, hp * 128:(hp + 1) * 128], rhs=hg[:, kc, :],
                                                                             start=(kc == 0), stop=(kc == 15)), reads=[bwq, bhg], writes=[bpQ[c]])
                        kb.op('act', lambda e, c=c, hp=hp: e.copy(out=qT[:, hp, :], in_=pQ[c][:]), reads=[bpQ[c]], writes=[bqT])
                    for jj in range(4):
                        ot = g4 * 4 + jj
                        for hp in range(16):
                            kb.op('pe', lambda e, hp=hp, jj=jj: e.matmul(pS5[hp // 4][:, (hp % 4) * 128:(hp % 4 + 1) * 128], lhsT=qT[:, hp, jj * 128:(jj + 1) * 128],
                                                                        rhs=skT[:, hp, :], start=True, stop=True), reads=[bqT, b5c], writes=[bpS5])
                        for b4 in range(4):
                            kb.op('act', lambda e, b4=b4: e.copy(out=sc[:, b4 * 4:(b4 + 1) * 4, :], in_=pS5[b4][:].rearrange("p (a k) -> p a k", a=4)),
                                  reads=[bpS5], writes=[bsc])
                        for hp in range(16):
                            kb.op('dve', lambda e, hp=hp: e.max(out=stv[:, hp, 0:8], in_=sc[:, hp, :]), reads=[bsc], writes=[bst5])
                        for hp in range(16):
                            kb.op('dve', lambda e, hp=hp: e.max_index(out=siv[:, hp, 0:8], in_max=stv[:, hp, 0:8], in_values=sc[:, hp, :]), reads=[bsc, bst5], writes=[bst5])
                        for hp in range(16):
                            kb.op('dve', lambda e, hp=hp: e.match_replace(out=wk[:, hp, :], in_to_replace=stv[:, hp, 0:8], in_values=sc[:, hp, :], imm_value=-1e30),
                                  reads=[bsc, bst5], writes=[bwk])
                        for hp in range(16):
                            kb.op('dve', lambda e, hp=hp: e.max(out=stv[:, hp, 8:16], in_=wk[:, hp, :]), reads=[bwk], writes=[bst5])
                        for hp in range(16):
                            kb.op('dve', lambda e, hp=hp: e.max_index(out=siv[:, hp, 8:16], in_max=stv[:, hp, 8:16], in_values=wk[:, hp, :]), reads=[bwk, bst5], writes=[bst5])
                        kb.op('dve', lambda e: e.tensor_copy(out=sif[:], in_=siv[:]), reads=[bst5], writes=[bst5])
                        stv4 = stv[:].rearrange("t (h p) k -> t h p k", p=2)
                        sif4 = sif[:].rearrange("t (h p) k -> t h p k", p=2)
                        for h in range(8):
                            kb.op('dve', lambda e, h=h: e.tensor_tensor(
                                out=cand[:, h, :].rearrange("t (i j) -> t i j", i=16),
                                in0=stv4[:, h, 0, :].unsqueeze(2).to_broadcast([128, 16, 16]),
                                in1=stv4[:, h, 1, :].unsqueeze(1).to_broadcast([128, 16, 16]), op=ALU.add), reads=[bst5], writes=[bcand])
                        for h in range(8):
                            kb.op('dve', lambda e, h=h: e.max(out=bs[:, h, 0:8], in_=cand[:, h, :]), reads=[bcand], writes=[bbs])
                        for h in range(8):
                            kb.op('dve', lambda e, h=h: e.max_index(out=bp[:, h, 0:8], in_max=bs[:, h, 0:8], in_values=cand[:, h, :]), reads=[bcand, bbs], writes=[bbs])
                        for h in range(8):
                            kb.op('dve', lambda e, h=h: e.match_replace(out=wk[:].rearrange("t a k -> t (a k)")[:, h * 256:(h + 1) * 256], in_to_replace=bs[:, h, 0:8], in_values=cand[:, h, :], imm_value=-1e30),
                                  reads=[bcand, bbs], writes=[bwk])
                        for h in range(8):
                            kb.op('dve', lambda e, h=h: e.max(out=bs[:, h, 8:16], in_=wk[:].rearrange("t a k -> t (a k)")[:, h * 256:(h + 1) * 256]), reads=[bwk], writes=[bbs])
                        for h in range(8):
                            kb.op('dve', lambda e, h=h: e.max_index(out=bp[:, h, 8:16], in_max=bs[:, h, 8:16], in_values=wk[:].rearrange("t a k -> t (a k)")[:, h * 256:(h + 1) * 256]), reads=[bwk, bbs], writes=[bbs])
                        kb.op('dve', lambda e: e.tensor_copy(out=bpf[:], in_=bp[:]), reads=[bbs], writes=[bsel])
                        kb.op('dve', lambda e: e.tensor_scalar(out=pj_[:], in0=bpf[:], scalar1=1.0 / 16.0, scalar2=None, op0=ALU.mult), reads=[bsel], writes=[bsel])
                        kb.op('dve', lambda e: e.tensor_copy(out=pii[:], in_=pj_[:]), reads=[bsel], writes=[bsel])
                        kb.op('dve', lambda e: e.tensor_copy(out=pi_[:], in_=pii[:]), reads=[bsel], writes=[bsel])
                        kb.op('dve', lambda e: e.tensor_tensor(out=pj_[:], in0=pi_[:], in1=pj_[:], op=ALU.is_gt), reads=[bsel], writes=[bsel])
                        kb.op('dve', lambda e: e.tensor_tensor(out=pi_[:], in0=pi_[:], in1=pj_[:], op=ALU.subtract), reads=[bsel], writes=[bsel])
                        kb.op('dve', lambda e: e.scalar_tensor_tensor(out=pj_[:], in0=pi_[:], scalar=-16.0, in1=bpf[:], op0=ALU.mult, op1=ALU.add), reads=[bsel], writes=[bsel])
                        for which, pp, dst in ((0, pi_, ea), (1, pj_, ebb)):
                            kb.op('dve', lambda e, pp=pp: e.tensor_tensor(
                                out=eq[:], in0=pp[:].rearrange("t h k -> t (h k)").unsqueeze(2).to_broadcast([128, 128, 16]),
                                in1=iota16[:].unsqueeze(1).to_broadcast([128, 128, 16]), op=ALU.is_equal), reads=[bsel, b5c], writes=[bsel])
                            for h in range(8):
                                kb.op('dve', lambda e, h=h, which=which: e.tensor_tensor(
                                    out=eq[:, h * 16:(h + 1) * 16, :], in0=eq[:, h * 16:(h + 1) * 16, :],
                                    in1=sif4[:, h, which, :].unsqueeze(1).to_broadcast([128, 16, 16]), op=ALU.mult), reads=[bsel, bst5], writes=[bsel])
                            kb.op('dve', lambda e, dst=dst: e.tensor_reduce(out=dst[:], in_=eq[:], axis=AX.X, op=ALU.add), reads=[bsel], writes=[bsel])
                        kb.op('dve', lambda e: e.scalar_tensor_tensor(out=idxf[:], in0=ea[:], scalar=128.0, in1=ebb[:], op0=ALU.mult, op1=ALU.add), reads=[bsel], writes=[bidxf])
                        kb.op('dve', lambda e: e.tensor_tensor(out=gte[:], in0=bs[:], in1=bs[:, :, 0:1].to_broadcast([128, 8, 16]), op=ALU.subtract), reads=[bbs], writes=[bgte])
                        kb.op('act', lambda e: e.activation(out=gte[:], in_=gte[:], func=AF.Exp), reads=[bgte], writes=[bgte])
                        kb.op('dve', lambda e: e.tensor_reduce(out=gsum[:], in_=gte[:], axis=AX.X, op=ALU.add), reads=[bgte], writes=[bgte])
                        kb.op('dve', lambda e: e.reciprocal(out=gsum[:], in_=gsum[:]), reads=[bgte], writes=[bgte])
                        kb.op('dve', lambda e: e.tensor_tensor(out=gte[:], in0=gte[:], in1=gsum[:].unsqueeze(2).to_broadcast([128, 8, 16]), op=ALU.mult), reads=[bgte], writes=[bgte])
                        kb.op('pe', lambda e: e.transpose(out=pTr[:, 0:128], in_=idxf[:], identity=identf[:]), reads=[bidxf, bC], writes=[bpTr])
                        kb.op('pe', lambda e: e.transpose(out=pTr[:, 128:256], in_=gte[:].rearrange("t h k -> t (h k)"), identity=identf[:]), reads=[bgte, bC], writes=[bpTr])
                        kb.op('dve', lambda e, ot=ot: e.tensor_copy(out=idxT[:, ot * 128:(ot + 1) * 128], in_=pTr[:, 0:128]), reads=[bpTr], writes=[bidxT])
                        kb.op('act', lambda e, ot=ot: e.copy(out=gT[:, ot * 128:(ot + 1) * 128], in_=pTr[:, 128:256]), reads=[bpTr], writes=[bgT])
                if dbg:
                    sdb5 = kb.dsem('dbg5')
                    bdb5 = Buf()
                    store(dbgidx, idxT[:], bidxT, bdb5, sdb5)
                    store(dbgg, gT[:], bgT, bdb5, sdb5)
                kb.barrier()
            if stop <= 5:
                kb.enabled = False

            flush_cast()
            kb.barrier()
            with ExitStack() as P6:
                NB = 6
                gb = [alloc(P6, "gb%d" % i, [128, 4096], BF16) for i in range(NB)]
                hn2t = [alloc(P6, "hn2t%d" % i, [128, 2048], BF16) for i in range(2)]
                junk6 = alloc(P6, "junk6", [128, 1024], BF16)
                NA = 4
                acc = [alloc(P6, "acc%d" % i, [128, 8], F32) for i in range(NA)]
                win = [alloc(P6, "win%d" % i, [128, 256], BF16) for i in range(2)]
                gt2bc = alloc(P6, "gt2bc", [128, 2048], F32)
                gfbc = alloc(P6, "gfbc", [128, 2048], F32)
                dtmp6 = alloc(P6, "dtmp6", [128, 128], F32)
                h16 = alloc(P6, "h16", [128, 2048], F32)
                tmp6 = alloc(P6, "tmp6", [128, 2048], F32)
                hf6 = alloc(P6, "hf6", [128, 2048], F32)
                o6 = alloc(P6, "o6", [128, 2048], F32)
                ss6 = alloc(P6, "ss6", [128, 2], F32)
                pXh = [palloc(P6, "pXh%d" % i, [128, 1024], F32) for i in range(2)]
                pOut = [palloc(P6, "pOut%d" % i, [128, 512], F32) for i in range(4)]
                bgb = [Buf() for _ in range(NB)]; bhn2t = [Buf(), Buf()]; bj6 = Buf(); bacc = [Buf() for _ in range(NA)]
                bwin = [Buf(), Buf()]; bbc6 = Buf(); bdt6 = Buf(); bh16 = Buf(); btmp6 = Buf(); bhf6 = Buf(); bo6 = Buf(); bss6 = Buf(); brs6 = Buf()
                bpXh = [PSB(), PSB()]; bpOut = PSB(); bOut = Buf(); bpXall = PSB()
                sgb = [kb.dsem('gb%d' % i) for i in range(NB)]
                ps4 = [pXh[0][:, 0:512], pXh[0][:, 512:1024], pXh[1][:, 0:512], pXh[1][:, 512:1024]]
                make_bc(P6, gt2bc, bbc6, gt2T, bMod, ps4, bpXall, dtmp6, bdt6)
                load(gfbc[:], gfin_d.to_broadcast([128, 2048]), bbc6, None)
                for i in range(2):
                    kb.op('dve', lambda e, i=i: e.memset(win[i][:], 0.0), writes=[bwin[i]])
                kb.barrier()
                HN2v = HN2s.rearrange("(n p) d -> n p d", p=128)
                H1v = H1s.rearrange("(n p) d -> n p d", p=128)
                Ov = out_d.rearrange("(n p) d -> n p d", p=128)
                loaded = set()

                def ensure_tile(ot):
                    if ot not in loaded and ot < 16:
                        loaded.add(ot)
                        load(hn2t[ot % 2][:], HN2v[ot], bhn2t[ot % 2], None, q='sp')

                def emit_xb(t):
                    ot, tt = divmod(t, 128)
                    ensure_tile(ot)
                    hi = ot % 2
                    for hf in range(2):
                        for bb in range(2):
                            b4 = hf * 2 + bb
                            kb.op('pe', lambda e, hf=hf, bb=bb, b4=b4, tt=tt, hi=hi: e.matmul(
                                pXh[hf][:, bb * 512:(bb + 1) * 512], lhsT=identb[:, tt:tt + 1].to_broadcast([128, 128]),
                                rhs=hn2t[hi][:, b4 * 512:(b4 + 1) * 512], start=True, stop=True),
                                reads=[bC, bhn2t[hi]], writes=[bpXh[hf]])

                def emit_gather(t):
                    u = t % NB
                    kb.dma('pool', lambda e, u=u, t=t: e.indirect_dma_start(
                        out=gb[u][:], out_offset=None, in_=UV16, in_offset=bass.IndirectOffsetOnAxis(ap=idxT[:, t:t + 1], axis=0)),
                        sgb[u], reads=[bidxT], writes=[bgb[u]])

                emit_gather(0)
                emit_xb(0)
                for t in range(2048):
                    ot, tt = divmod(t, 128)
                    u = t % NB
                    a4 = t % NA
                    a2 = t % 2
                    if t + 1 < 2048:
                        emit_gather(t + 1)
                    for hf in range(2):
                        kb.op('dve', lambda e, hf=hf, u=u, a4=a4: e.scalar_tensor_tensor(
                            out=junk6[:], in0=gb[u][:, hf * 1024:(hf + 1) * 1024], scalar=1.0, in1=pXh[hf][:], op0=ALU.mult, op1=ALU.mult,
                            accum_out=acc[a4][:, hf:hf + 1]), reads=[bgb[u], bpXh[hf]], writes=[bj6, bacc[a4]])
                    kb.op('act', lambda e, a4=a4: e.activation(out=acc[a4][:, 2:4], in_=acc[a4][:, 0:2], func=AF.Identity, accum_out=acc[a4][:, 4:5]),
                          reads=[bacc[a4]], writes=[bacc[a4]])
                    kb.op('act', lambda e, a4=a4: e.activation(out=acc[a4][:, 5:6], in_=acc[a4][:, 4:5], func=AF.Gelu), reads=[bacc[a4]], writes=[bacc[a4]])
                    kb.op('act', lambda e, a4=a4, a2=a2, t=t: e.activation(out=win[a2][:, 127:128], in_=acc[a4][:, 5:6], func=AF.Identity, scale=gT[:, t:t + 1]),
                          reads=[bacc[a4], bgT], writes=[bwin[a2]])
                    if t + 1 < 2048:
                        emit_xb(t + 1)
                    for b4 in range(4):
                        kb.op('pe', lambda e, b4=b4, a2=a2, tt=tt, u=u: e.matmul(pOut[b4][:], lhsT=win[a2][:, 127 - tt:255 - tt], rhs=gb[u][:, 2048 + b4 * 512:2048 + (b4 + 1) * 512],
                                                                                start=(tt == 0), stop=(tt == 127)), reads=[bwin[a2], bgb[u]], writes=[bpOut])
                    if tt == 127:
                        load(h16[:], H1v[ot], bh16, None, q='act')
                        for b4 in range(4):
                            kb.op('dve', lambda e, b4=b4: e.tensor_tensor(out=tmp6[:, b4 * 512:(b4 + 1) * 512], in0=pOut[b4][:], in1=gt2bc[:, b4 * 512:(b4 + 1) * 512], op=ALU.mult),
                                  reads=[bpOut, bbc6], writes=[btmp6])
                        kb.op('pool', lambda e: e.tensor_tensor(out=hf6[:], in0=tmp6[:], in1=h16[:], op=ALU.add), reads=[btmp6, bh16], writes=[bhf6])
                        kb.op('act', lambda e: e.activation(out=tmp6[:], in_=hf6[:], func=AF.Square, accum_out=ss6[:, 0:1]), reads=[bhf6], writes=[btmp6, bss6])
                        rstd_from_ss(ss6[:, 0:1], ss6[:, 1:2], bss6, brs6, float(D))
                        kb.op('dve', lambda e: e.scalar_tensor_tensor(out=o6[:], in0=hf6[:], scalar=ss6[:, 1:2], in1=gfbc[:], op0=ALU.mult, op1=ALU.mult),
                              reads=[bhf6, brs6, bbc6], writes=[bo6])
                        store(Ov[ot], o6[:], bo6, bOut, None, q='sp')
                kb.barrier()

        blk = es.enter_context(nc.Block())
        kb.emit(blk)
    return nc


_NC = None


def kernel(x, c, positions, w_ada, b_ada, g_norm1, w_in, g_attn_out, g_sgu_out, sgu_w, sgu_b, sgu_ln_g, sgu_ln_b,
           w_out, g_norm2, peer_w_q, peer_sub_keys, peer_u, peer_v, g_final, _prep_only=False):
    global _NC
    f32 = np.float32
    x = np.asarray(x, f32); c = np.asarray(c, f32); positions = np.asarray(positions, np.int32)

    def colT(v, n):
        return np.ascontiguousarray(np.asarray(v, f32).reshape(n, 128).T)

    half = 64
    inv = (10000.0 ** (-np.arange(half, dtype=np.float64) / half))
    inv2 = np.concatenate([inv, inv]) / (2.0 * np.pi)
    inv2 = inv2.astype(f32).reshape(128, 1)
    sgn = np.concatenate([-np.ones(64), np.ones(64)]).astype(f32).reshape(128, 1)
    k = np.arange(128)[:, None]
    cidx = np.arange(17 * 128)[None, :]
    delta = cidx - k
    cm = ((delta >= 0) & (delta <= 128)).astype(f32) + ((delta >= 0) & (delta % 4 == 0) & (delta <= 512)).astype(f32) \
        + ((delta >= 0) & (delta % 16 == 0) & (delta <= 2048)).astype(f32)
    maskW = cm.astype(ml_dtypes.bfloat16)
    perm = np.zeros((128, 128), f32)
    perm[(np.arange(128) + 64) % 128, np.arange(128)] = 1.0
    perm = perm.astype(ml_dtypes.bfloat16)
    identf = np.eye(128, dtype=f32)
    iota16 = np.tile(np.arange(16, dtype=f32)[None, :], (128, 1))
    trilT = (np.arange(128)[:, None] <= np.arange(128)[None, :]).astype(f32)

    shared = {
        "w_ada": np.ascontiguousarray(np.asarray(w_ada, f32)[0]),
        "b_adaT": colT(np.asarray(b_ada)[0], 96),
        "gn1T": colT(np.asarray(g_norm1)[0], 16),
        "gn2T": colT(np.asarray(g_norm2)[0], 16),
        "w_in": np.ascontiguousarray(np.asarray(w_in, f32)[0]),
        "w_out": np.ascontiguousarray(np.asarray(w_out, f32)[0]),
        "gmixT": colT(np.concatenate([np.asarray(g_attn_out)[0], np.asarray(g_sgu_out)[0]]), 16),
        "sgu_wT": np.ascontiguousarray(np.transpose(np.asarray(sgu_w, f32)[0], (2, 0, 1))),
        "sgu_bT": np.ascontiguousarray(np.asarray(sgu_b, f32)[0].T),
        "lng": np.ascontiguousarray(np.asarray(sgu_ln_g, f32)[0].reshape(1, 512)),
        "lnb": np.ascontiguousarray(np.asarray(sgu_ln_b, f32)[0].reshape(1, 512)),
        "w_q": np.ascontiguousarray(np.asarray(peer_w_q, f32)[0]),
        "skT": np.ascontiguousarray(np.transpose(np.asarray(peer_sub_keys, f32)[0].reshape(16, 128, 128), (2, 0, 1))),
        "peer_u": np.ascontiguousarray(np.asarray(peer_u, f32)[0]),
        "peer_v": np.ascontiguousarray(np.asarray(peer_v, f32)[0]),
        "gfin": np.ascontiguousarray(np.asarray(g_final, f32).reshape(1, D)),
        "inv2": inv2, "sgn": sgn, "maskW": maskW, "perm": perm, "identf": identf, "iota16": iota16, "trilT": trilT,
    }
    in_maps = []
    for core in range(8):
        b = core // 2
        hf = core % 2
        xa = np.zeros((NCTX + NT, D), f32)
        pa = np.zeros((1, NCTX + NT), np.int32)
        xa[NCTX:] = x[b, hf * NT:(hf + 1) * NT]
        pa[0, NCTX:] = positions[b, hf * NT:(hf + 1) * NT]
        if hf == 1:
            xa[:NCTX] = x[b, 0:NCTX]
            pa[0, :NCTX] = positions[b, 0:NCTX]
        m = dict(shared)
        m["xall"] = xa
        m["pos"] = pa
        m["condT"] = colT(c[b], 16)
        m["flag"] = np.full((128, 1), float(hf), f32)
        in_maps.append(m)
    if _prep_only:
        return in_maps
    if _NC is None:
        import os
        _NC = build(stop=float(os.environ.get('KSTOP', '99')))
    res = run_bass_kernel_spmd(_NC, in_maps, core_ids=list(range(8)))
    out = np.zeros((4, 4096, D), f32)
    for core in range(8):
        b = core // 2
        hf = core % 2
        out[b, hf * NT:(hf + 1) * NT] = np.asarray(res.results[core]["out"], f32)
    return out
```

```python
import numpy as np
import ml_dtypes
from contextlib import ExitStack
import concourse.bass as bass
import concourse.mybir as mybir
from concourse.bass_utils import run_bass_kernel_spmd

F32 = mybir.dt.float32
BF16 = mybir.dt.bfloat16
U32 = mybir.dt.uint32
I32 = mybir.dt.int32
AF = mybir.ActivationFunctionType
ALU = mybir.AluOpType
AX = mybir.AxisListType

D = 2048
NT = 2048
NCTX = 2048
DIN = 5632
ENG = ('pe', 'act', 'dve', 'pool', 'sp')
QG = 2
NW = 16 + QG


class LazySem:
    def __init__(self, name):
        self.name = name
        self.real = None


class Buf:
    def __init__(self, name=''):
        self.w = None
        self.r = {}
        self.name = name
        self.sems = {}
        self.psum = False


def PSB():
    b = Buf('psum')
    b.psum = True
    return b


class KB:
    EP = 16000

    def __init__(self, nc, es):
        self.nc = nc
        self.es = es
        self.streams = {e: [] for e in ENG}
        self.cnt = {e: 0 for e in ENG}
        self.esem = {e: [] for e in ENG}
        self.known = {e: {} for e in ENG}
        self.dcount = {}
        self.dsems = []
        self.nsem = 0
        self.enabled = True

    def _newsem(self, name):
        self.nsem += 1
        return self.es.enter_context(self.nc.semaphore(name))

    def dsem(self, name):
        return LazySem(name)

    def _real(self, ls):
        if ls.real is None:
            sm = self._newsem('d%d_%s' % (self.nsem, ls.name))
            self.dcount[id(sm)] = 0
            self.dsems.append(sm)
            ls.real = sm
        return ls.real

    def buf_sem(self, buf, kind):
        if kind not in buf.sems:
            buf.sems[kind] = LazySem(kind + '_' + (buf.name or 'b'))
        return buf.sems[kind]

    def _eev(self, e):
        c = self.cnt[e]
        ep = (c - 1) // self.EP
        while len(self.esem[e]) <= ep:
            self.esem[e].append(self._newsem('e_%s_%d' % (e, len(self.esem[e]))))
        return (self.esem[e][ep], (c - 1) % self.EP + 1)

    def _deps(self, e, reads, writes):
        evs = []
        for b in reads:
            if b.w is not None:
                evs.append(b.w)
            if b.psum:
                evs.extend(ev for ev in b.r.values() if ev[3] != e)
        for b in writes:
            if b.w is not None:
                evs.append(b.w)
            evs.extend(b.r.values())
        waits = []
        own = self.esem[e]
        k = self.known[e]
        for (sm, val, isd, _eng) in evs:
            if e == 'pe' and any(sm is x for x in own):
                continue
            if isd:
                val = 16 * self.dcount[id(sm)]
            if k.get(id(sm), 0) < val:
                k[id(sm)] = val
                waits.append((sm, val))
        return waits

    def _book(self, ev, reads, writes):
        for b in reads:
            b.r[id(ev[0])] = ev
        for b in writes:
            b.w = ev
            b.r = {}

    def op(self, e, fn, reads=(), writes=()):
        if not self.enabled:
            return
        waits = self._deps(e, reads, writes)
        self.cnt[e] += 1
        sm, val = self._eev(e)
        self.streams[e].append((waits, fn, (sm, 1)))
        self._book((sm, val, False, e), reads, writes)

    def dma(self, q, fn, sem, reads=(), writes=(), after=()):
        if not self.enabled:
            return
        sem = self._real(sem)
        waits = self._deps(q, list(reads) + list(after), writes)
        self.dcount[id(sem)] += 1
        self.streams[q].append((waits, fn, (sem, 16)))
        self._book((sem, 16 * self.dcount[id(sem)], True, q), reads, writes)

    def barrier(self):
        if not self.enabled:
            return
        for e in ENG:
            waits = []
            k = self.known[e]
            for e2 in ENG:
                if self.cnt[e2] > 0 and not (e == 'pe' and e2 == 'pe'):
                    sm, val = self._eev(e2)
                    if k.get(id(sm), 0) < val:
                        k[id(sm)] = val
                        waits.append((sm, val))
            for sm in self.dsems:
                val = 16 * self.dcount[id(sm)]
                if val > 0 and k.get(id(sm), 0) < val:
                    k[id(sm)] = val
                    waits.append((sm, val))
            if waits:
                self.streams[e].append((waits, None, None))

    def emit(self, blk):
        emap = {'pe': blk.tensor, 'act': blk.scalar, 'dve': blk.vector, 'pool': blk.gpsimd, 'sp': blk.sync}
        for e in ENG:
            def body(eng, e=e):
                for waits, fn, inc in self.streams[e]:
                    for sm, val in waits:
                        eng.wait_ge(sm, val)
                    if fn is not None:
                        fn(eng).then_inc(inc[0], inc[1])
            emap[e](body)


def build(stop=99, dbg=False):
    nc = bass.Bass("TRN2", target_bir_lowering=False)
    SK = "ExternalOutput" if dbg else "Internal"

    def din(name, shape, dt=F32):
        return nc.dram_tensor(name, list(shape), dt, kind="ExternalInput").ap()

    xall = din("xall", [NCTX + NT, D])
    condT_d = din("condT", [128, 16])
    pos_d = din("pos", [1, NCTX + NT], I32)
    flag_d = din("flag", [128, 1])
    w_ada = din("w_ada", [D, 6 * D])
    b_adaT = din("b_adaT", [128, 96])
    gn1T_d = din("gn1T", [128, 16])
    gn2T_d = din("gn2T", [128, 16])
    w_in = din("w_in", [D, DIN])
    w_out = din("w_out", [D, D])
    gmixT_d = din("gmixT", [128, 16])
    sgu_wT_d = din("sgu_wT", [128, 4, 128])
    sgu_bT_d = din("sgu_bT", [128, 4])
    lng_d = din("lng", [1, 512])
    lnb_d = din("lnb", [1, 512])
    w_q = din("w_q", [D, D])
    skT_d = din("skT", [128, 16, 128])
    peer_u = din("peer_u", [16384, D])
    peer_v = din("peer_v", [16384, D])
    gfin_d = din("gfin", [1, D])
    inv2_d = din("inv2", [128, 1])
    sgn_d = din("sgn", [128, 1])
    maskW_d = din("maskW", [128, 17 * 128], BF16)
    perm_d = din("perm", [128, 128], BF16)
    identf_d = din("identf", [128, 128])
    iota_d = din("iota16", [128, 16])
    tril_d = din("trilT", [128, 128])
    out_d = nc.dram_tensor("out", [NT, D], F32, kind="ExternalOutput").ap()

    dbgmod = nc.dram_tensor("dbgmod", [128, 128], F32, kind=SK).ap()
    dbgidx = nc.dram_tensor("dbgidx", [128, 2048], I32, kind=SK).ap()
    dbgg = nc.dram_tensor("dbgg", [128, 2048], F32, kind=SK).ap()
    KTs = nc.dram_tensor("KTs", [12, 128, NCTX + NT], BF16, kind=SK).ap()
    QTs = nc.dram_tensor("QTs", [12, 128, NT], BF16, kind=SK).ap()
    Vs = nc.dram_tensor("Vs", [NCTX + NT, 12 * 129], BF16, kind=SK).ap()
    UVs = nc.dram_tensor("UVs", [NT, 1024], F32, kind=SK).ap()
    MTs = nc.dram_tensor("MTs", [16, 128, 2048], BF16, kind=SK).ap()
    H1s = nc.dram_tensor("H1s", [NT, D], F32, kind=SK).ap()
    HN2s = nc.dram_tensor("HN2s", [NT, D], BF16, kind=SK).ap()
    HN2Ts = nc.dram_tensor("HN2Ts", [16, 128, 2048], BF16, kind=SK).ap()
    UV16 = nc.dram_tensor("UV16", [16384, 4096], BF16).ap()

    with ExitStack() as es:
        kb = KB(nc, es)

        uniq = [0]

        def alloc(st, name, shape, dt):
            uniq[0] += 1
            return st.enter_context(nc.sbuf_tensor("s%d_%s" % (uniq[0], name), list(shape), dt))

        def palloc(st, name, shape, dt):
            uniq[0] += 1
            return st.enter_context(nc.psum_tensor("p%d_%s" % (uniq[0], name), list(shape), dt))

        qrr = [0]

        def ldq():
            qrr[0] += 1
            return 'sp' if qrr[0] % 2 else 'act'

        def load(dst_ap, src_ap, buf, sem, q=None, reads=()):
            kb.dma(q or 'sp', lambda e: e.dma_start(out=dst_ap, in_=src_ap), kb.buf_sem(buf, 'l'), reads=(), writes=[buf])

        def store(dst_ap, src_ap, srcbuf, dstbuf, sem, q=None):
            kb.dma(q or 'sp', lambda e: e.dma_start(out=dst_ap, in_=src_ap), kb.buf_sem(srcbuf, 's'), reads=[srcbuf], writes=())

        cast_sem = kb.dsem('cast')
        cast_todo = []
        for cch in range(16):
            rows = slice(cch * 1024, (cch + 1) * 1024)
            cast_todo.append((UV16[rows, 0:2048], peer_u[rows, :]))
            cast_todo.append((UV16[rows, 2048:4096], peer_v[rows, :]))

        def emit_cast(k, after=()):
            for _ in range(k):
                if cast_todo:
                    o_ap, i_ap = cast_todo.pop(0)
                    kb.dma('pool', lambda e, o_ap=o_ap, i_ap=i_ap: e.dma_start(out=o_ap, in_=i_ap), cast_sem, after=after)

        def flush_cast():
            emit_cast(len(cast_todo))

        G = es
        identf = alloc(G, "identf", [128, 128], F32)
        identb = alloc(G, "identb", [128, 128], BF16)
        onesf = alloc(G, "onesf", [128, 128], F32)
        perm = alloc(G, "perm", [128, 128], BF16)
        flag = alloc(G, "flag", [128, 1], F32)
        onec = alloc(G, "onec", [128, 1], F32)
        modT = alloc(G, "modT", [128, 96], F32)
        gs1T = alloc(G, "gs1T", [128, 16], F32)
        gs2T = alloc(G, "gs2T", [128, 16], F32)
        epsc = alloc(G, "epsc", [128, 1], F32)
        epsl = alloc(G, "epsl", [128, 1], F32)
        bC = Buf('const')
        bMod = Buf('mod')
        sC = kb.dsem('const')
        load(identf[:], identf_d, bC, sC)
        load(perm[:], perm_d, bC, sC)
        load(flag[:], flag_d, bC, sC)
        kb.op('dve', lambda e: e.tensor_copy(out=identb[:], in_=identf[:]), reads=[bC], writes=[bC])
        kb.op('dve', lambda e: e.memset(onesf[:], 1.0), writes=[bC])
        kb.op('dve', lambda e: e.memset(onec[:], 1.0), writes=[bC])
        kb.op('dve', lambda e: e.memset(epsc[:, 0:1], 1e-6), writes=[bC])
        kb.op('dve', lambda e: e.memset(epsl[:, 0:1], 1e-5), writes=[bC])

        def rstd_from_ss(ss, rstd, bss, brs, n, epscol=0):
            kb.op('act', lambda e: e.activation(out=rstd, in_=ss, func=AF.Sqrt, bias=epsc[:, 0:1], scale=1.0 / n),
                  reads=[bss, bC], writes=[brs])
            kb.op('dve', lambda e: e.reciprocal(out=rstd, in_=rstd), reads=[brs], writes=[brs])

        def make_bc(st, dst, dstbuf, srcT, srcbuf, ps4, psbuf, tmpd, tmpbuf):
            for dc in range(16):
                kb.op('dve', lambda e, dc=dc: e.tensor_scalar(out=tmpd[:], in0=identf[:], scalar1=srcT[:, dc:dc + 1], scalar2=None, op0=ALU.mult),
                      reads=[bC, srcbuf], writes=[tmpbuf])
                kb.op('pe', lambda e, dc=dc: e.matmul(ps4[dc // 4][:, (dc % 4) * 128:(dc % 4 + 1) * 128], lhsT=onesf[:], rhs=tmpd[:], start=True, stop=True),
                      reads=[bC, tmpbuf], writes=[psbuf])
            for b4 in range(4):
                kb.op('act', lambda e, b4=b4: e.copy(out=dst[:, b4 * 512:(b4 + 1) * 512], in_=ps4[b4][:]), reads=[psbuf], writes=[dstbuf])

        with ExitStack() as P1:
            condT = alloc(P1, "condT", [128, 16], F32)
            badaT = alloc(P1, "badaT", [128, 96], F32)
            gnT = alloc(P1, "gnT", [128, 32], F32)
            wst = [alloc(P1, "wst%d" % i, [128, 16, 512], F32) for i in range(3)]
            psmod = palloc(P1, "psmod", [128, 512], F32)
            bcond = Buf(); bw = [Buf(), Buf(), Buf()]; bps = PSB()
            sw = [kb.dsem('wada0'), kb.dsem('wada1')]
            load(condT[:], condT_d, bcond, sC)
            load(badaT[:], b_adaT, bcond, sC)
            load(gnT[:, 0:16], gn1T_d, bcond, sC)
            load(gnT[:, 16:32], gn2T_d, bcond, sC)
            kb.op('act', lambda e: e.activation(out=condT[:], in_=condT[:], func=AF.Silu), reads=[bcond], writes=[bcond])
            wsb = [alloc(P1, "wsb%d" % i, [128, 16, 512], BF16) for i in range(2)]
            condb = alloc(P1, "condb", [128, 16], BF16)
            bwsb = [Buf(), Buf()]
            kb.op('dve', lambda e: e.tensor_copy(out=condb[:], in_=condT[:]), reads=[bcond], writes=[bcond])
            wv = w_ada.rearrange("(kc p) f -> p kc f", p=128)
            for blk in range(24):
                i3 = blk % 3
                i = blk % 2
                load(wst[i3][:], wv[:, :, blk * 512:(blk + 1) * 512], bw[i3], None, q=ldq())
                kb.op('dve' if i == 0 else 'pool', lambda e, i=i, i3=i3: e.tensor_copy(out=wsb[i][:], in_=wst[i3][:]), reads=[bw[i3]], writes=[bwsb[i]])
                for j in range(4):
                    col = blk * 4 + j
                    for kc in range(16):
                        kb.op('pe', lambda e, i=i, j=j, kc=kc, col=col: e.matmul(
                            psmod[:, col:col + 1], lhsT=wsb[i][:, kc, j * 128:(j + 1) * 128], rhs=condb[:, kc:kc + 1],
                            start=(kc == 0), stop=(kc == 15)), reads=[bwsb[i], bcond], writes=[bps])
            kb.op('dve', lambda e: e.tensor_tensor(out=modT[:], in0=psmod[:, 0:96], in1=badaT[:], op=ALU.add), reads=[bps, bcond], writes=[bMod])
            kb.op('dve', lambda e: e.scalar_tensor_tensor(out=gs1T[:], in0=modT[:, 16:32], scalar=1.0, in1=gnT[:, 0:16], op0=ALU.add, op1=ALU.mult),
                  reads=[bMod, bcond], writes=[bMod])
            kb.op('dve', lambda e: e.scalar_tensor_tensor(out=gs2T[:], in0=modT[:, 64:80], scalar=1.0, in1=gnT[:, 16:32], op0=ALU.add, op1=ALU.mult),
                  reads=[bMod, bcond], writes=[bMod])
            if dbg:
                sdb = kb.dsem('dbg')
                bdb = Buf()
                store(dbgmod[:, 0:96], modT[:], bMod, bdb, sdb)
                store(dbgmod[:, 96:112], gs1T[:], bMod, bdb, sdb)
                store(dbgmod[:, 112:128], gs2T[:], bMod, bdb, sdb)
            kb.barrier()
        if stop <= 1:
            kb.enabled = False
        sh1T = modT[:, 0:16]
        gt1T = modT[:, 32:48]
        sh2T = modT[:, 48:64]
        gt2T = modT[:, 80:96]

        with ExitStack() as P2:
            ropetab = alloc(P2, "ropetab", [128, 2, 4096], F32)
            sinT = ropetab[:, 0, :]
            cosT = ropetab[:, 1, :]
            bTab = Buf(); bhnT = Buf()
            with ExitStack() as P2r:
                posi = alloc(P2r, "posi", [128, 4096], I32)
                y = alloc(P2r, "ry", [128, 4096], F32)
                y2 = alloc(P2r, "ry2", [128, 2, 4096], F32)
                yi = alloc(P2r, "ryi", [128, 2, 4096], I32)
                m = alloc(P2r, "rm", [128, 2, 4096], F32)
                inv2 = alloc(P2r, "inv2", [128, 1], F32)
                sgn = alloc(P2r, "sgn", [128, 1], F32)
                bR = Buf()
                load(posi[:], pos_d.to_broadcast([128, 4096]), bR, sC)
                load(inv2[:], inv2_d, bR, sC)
                load(sgn[:], sgn_d, bR, sC)
                kb.op('dve', lambda e: e.tensor_copy(out=y[:], in_=posi[:]), reads=[bR], writes=[bR])
                kb.op('dve', lambda e: e.tensor_scalar(out=y2[:, 0, :], in0=y[:], scalar1=inv2[:, 0:1], scalar2=None, op0=ALU.mult), reads=[bR], writes=[bR])
                kb.op('dve', lambda e: e.tensor_scalar(out=y2[:, 1, :], in0=y[:], scalar1=inv2[:, 0:1], scalar2=0.25, op0=ALU.mult, op1=ALU.add), reads=[bR], writes=[bR])
                kb.op('dve', lambda e: e.tensor_copy(out=yi[:], in_=y2[:]), reads=[bR], writes=[bR])
                kb.op('dve', lambda e: e.tensor_copy(out=m[:], in_=yi[:]), reads=[bR], writes=[bR])
                kb.op('dve', lambda e: e.tensor_tensor(out=y2[:], in0=y2[:], in1=m[:], op=ALU.subtract), reads=[bR], writes=[bR])
                kb.op('dve', lambda e: e.tensor_scalar(out=m[:], in0=y2[:], scalar1=0.5, scalar2=None, op0=ALU.is_gt), reads=[bR], writes=[bR])
                kb.op('dve', lambda e: e.tensor_tensor(out=y2[:], in0=y2[:], in1=m[:], op=ALU.subtract), reads=[bR], writes=[bR])
                kb.op('dve', lambda e: e.tensor_scalar(out=m[:], in0=y2[:], scalar1=-0.5, scalar2=None, op0=ALU.is_lt), reads=[bR], writes=[bR])
                kb.op('dve', lambda e: e.tensor_tensor(out=y2[:], in0=y2[:], in1=m[:], op=ALU.add), reads=[bR], writes=[bR])
                kb.op('act', lambda e: e.activation(out=ropetab[:], in_=y2[:], func=AF.Sin, scale=2.0 * np.pi * (1.0 - 1e-6)),
                      reads=[bR], writes=[bTab])
                kb.op('dve', lambda e: e.tensor_scalar(out=ropetab[:, 0, :], in0=ropetab[:, 0, :], scalar1=sgn[:, 0:1], scalar2=None, op0=ALU.mult), reads=[bTab, bR], writes=[bTab])
                kb.barrier()
            if stop <= 1.5:
                kb.enabled = False
            hnT = alloc(P2, "hnT", [128, 16, 2048], BF16)

            xv = xall.rearrange("(n p) d -> n p d", p=128)
            Vsv = Vs.rearrange("(n p) c -> n p c", p=128)
            UVv = UVs.rearrange("(n p) c -> n p c", p=128)
            bKTs = Buf(); bQTs = Buf(); bVs = Buf(); bUVs = Buf()
            wiv = w_in.rearrange("(kc p) f -> p kc f", p=128)
            def do_half(half):
                with ExitStack() as PA:
                    xst = [alloc(PA, "xst%d" % i, [128, 2048], F32) for i in range(2)]
                    xnb = [alloc(PA, "xnb%d" % i, [128, 2048], BF16) for i in range(2)]
                    junk = alloc(PA, "junkA", [128, 2048], BF16)
                    ss = [alloc(PA, "ssA%d" % i, [128, 2], F32) for i in range(2)]
                    pT = [palloc(PA, "pTA%d" % i, [128, 2048], BF16) for i in range(2)]
                    bx = [Buf(), Buf()]; bxn = [Buf(), Buf()]; bj = Buf(); bss = [Buf(), Buf()]; brs = [Buf(), Buf()]; bpT = [PSB(), PSB()]
                    sx = [kb.dsem('xA0_%d' % half), kb.dsem('xA1_%d' % half)]
                    for tt in range(16):
                        i = tt % 2
                        gt = half * 16 + tt
                        load(xst[i][:], xv[gt], bx[i], sx[i], q=ldq())
                        kb.op('act', lambda e, i=i: e.activation(out=junk[:], in_=xst[i][:], func=AF.Square, accum_out=ss[i][:, 0:1]),
                              reads=[bx[i]], writes=[bj, bss[i]])
                        rstd_from_ss(ss[i][:, 0:1], ss[i][:, 1:2], bss[i], brs[i], float(D))
                        kb.op('dve', lambda e, i=i: e.tensor_scalar(out=xnb[i][:], in0=xst[i][:], scalar1=ss[i][:, 1:2], scalar2=None, op0=ALU.mult),
                              reads=[bx[i], brs[i]], writes=[bxn[i]])
                        import os
                        KS = os.environ.get('KSKIP', '')
                        for dc in (range(16) if KS != 'a1' else ()):
                            kb.op('pe', lambda e, i=i, dc=dc: e.transpose(out=pT[i][:, dc * 128:(dc + 1) * 128], in_=xnb[i][:, dc * 128:(dc + 1) * 128], identity=identb[:]),
                                  reads=[bxn[i], bC], writes=[bpT[i]])
                        for dc in (range(16) if KS not in ('a1', 'a2') else ()):
                            if True:
                                kb.op('dve', lambda e, i=i, dc=dc, tt=tt: e.tensor_scalar(
                                    out=hnT[:, dc, tt * 128:(tt + 1) * 128], in0=pT[i][:, dc * 128:(dc + 1) * 128],
                                    scalar1=gs1T[:, dc:dc + 1], scalar2=sh1T[:, dc:dc + 1], op0=ALU.mult, op1=ALU.add),
                                    reads=[bpT[i], bMod], writes=[bhnT])
                            else:
                                kb.op('act', lambda e, i=i, dc=dc, tt=tt: e.activation(
                                    out=hnT[:, dc, tt * 128:(tt + 1) * 128], in_=pT[i][:, dc * 128:(dc + 1) * 128],
                                    func=AF.Identity, scale=(1.0 if KS == 'a4' else gs1T[:, dc:dc + 1]), bias=(0.0 if KS in ('a4', 'a5') else sh1T[:, dc:dc + 1])),
                                    reads=[bpT[i], bMod], writes=[bhnT])
                    kb.barrier()
                if stop <= 1.6 + 0.2 * half:
                    kb.enabled = False
                with ExitStack() as PB:
                    wf = [alloc(PB, "wf%d" % i, [128, 16, 256], F32) for i in range(2)]
                    wb = [alloc(PB, "wb%d" % i, [128, 16, 512], BF16) for i in range(2)]
                    qb = [alloc(PB, "qb%d" % i, [128, 512], BF16) for i in range(2)]
                    t1 = [alloc(PB, "t1%d" % i, [128, 512], F32) for i in range(2)]
                    t2 = [alloc(PB, "t2%d" % i, [128, 512], F32) for i in range(2)]
                    ro = [alloc(PB, "ro%d" % i, [128, 512], BF16) for i in range(2)]
                    vst = [alloc(PB, "vst%d" % i, [128, 4, 129], BF16) for i in range(2)]
                    uvst = [alloc(PB, "uvst%d" % i, [128, 512], F32) for i in range(2)]
                    pq = [palloc(PB, "pq%d" % i, [128, 512], F32) for i in range(2)]
                    psw = [palloc(PB, "psw%d" % i, [128, 512], F32) for i in range(2)]
                    pv = [palloc(PB, "pv%d" % i, [128, 512], F32) for i in range(2)]
                    bwf = [Buf(), Buf()]; bwb = [Buf(), Buf()]; bqb = [Buf(), Buf()]; bt1 = [Buf(), Buf()]; bt2 = [Buf(), Buf()]
                    bro = [Buf(), Buf()]; bvst = [Buf(), Buf()]; buv = [Buf(), Buf()]; bpq = [PSB(), PSB()]; bpsw = [PSB(), PSB()]; bpv = [PSB(), PSB()]
                    swf = [kb.dsem('wf0_%d' % half), kb.dsem('wf1_%d' % half)]
                    sro = [kb.dsem('ro0_%d' % half), kb.dsem('ro1_%d' % half)]
                    svs = [kb.dsem('vs0_%d' % half), kb.dsem('vs1_%d' % half)]
                    suv = [kb.dsem('uv0_%d' % half), kb.dsem('uv1_%d' % half)]
                    fl = flag if half == 0 else onec
                    for i in range(2):
                        kb.op('dve', lambda e, i=i: e.tensor_copy(out=vst[i][:, :, 128:129], in_=fl[:, 0:1].unsqueeze(1).to_broadcast([128, 4, 1])),
                              reads=[bC], writes=[bvst[i]])
                    blocks = [3, 4, 5, 6, 7, 8] if half == 0 else list(range(11))
                    cnt_fm = 0
                    cnt_tm = 0
                    for bi, blk in enumerate(blocks):
                        i = bi % 2
                        for hc in range(2):
                            load(wf[hc][:], wiv[:, :, blk * 512 + hc * 256:blk * 512 + (hc + 1) * 256], bwf[hc], swf[hc], q=ldq())
                            kb.op('pool', lambda e, i=i, hc=hc: e.tensor_copy(out=wb[i][:, :, hc * 256:(hc + 1) * 256], in_=wf[hc][:]),
                                  reads=[bwf[hc]], writes=[bwb[i]])
                        import os
                        KS = os.environ.get('KSKIP', '')
                        if KS == 'b1' or (KS == 'b2' and blk >= 6) or (KS == 'b3' and blk < 6):
                            continue
                        if blk < 6:
                            isq = blk < 3
                            for hh in range(4):
                                head = (blk % 3) * 4 + hh
                                for tg in range(4):
                                    c = cnt_fm % 2
                                    cnt_fm += 1
                                    for kc in range(16):
                                        kb.op('pe', lambda e, i=i, c=c, kc=kc, hh=hh, tg=tg: e.matmul(
                                            pq[c][:], lhsT=wb[i][:, kc, hh * 128:(hh + 1) * 128], rhs=hnT[:, kc, tg * 512:(tg + 1) * 512],
                                            start=(kc == 0), stop=(kc == 15)), reads=[bwb[i], bhnT], writes=[bpq[c]])
                                    tok0 = half * 2048 + tg * 512
                                    kb.op('act', lambda e, c=c: e.copy(out=qb[c][:], in_=pq[c][:]), reads=[bpq[c]], writes=[bqb[c]])
                                    kb.op('dve', lambda e, c=c, tok0=tok0: e.tensor_tensor(out=t1[c][:], in0=pq[c][:], in1=cosT[:, tok0:tok0 + 512], op=ALU.mult),
                                          reads=[bpq[c], bTab], writes=[bt1[c]])
                                    kb.op('pe', lambda e, c=c: e.matmul(psw[c][:], lhsT=perm[:], rhs=qb[c][:], start=True, stop=True),
                                          reads=[bC, bqb[c]], writes=[bpsw[c]])
                                    kb.op('dve', lambda e, c=c, tok0=tok0: e.tensor_tensor(out=t2[c][:], in0=psw[c][:], in1=sinT[:, tok0:tok0 + 512], op=ALU.mult),
                                          reads=[bpsw[c], bTab], writes=[bt2[c]])
                                    kb.op('pool', lambda e, c=c: e.tensor_tensor(out=ro[c][:], in0=t1[c][:], in1=t2[c][:], op=ALU.add),
                                          reads=[bt1[c], bt2[c]], writes=[bro[c]])
                                    if isq:
                                        store(QTs[head, :, tg * 512:(tg + 1) * 512], ro[c][:], bro[c], bQTs, sro[c], q='sp')
                                    else:
                                        store(KTs[head, :, tok0:tok0 + 512], ro[c][:], bro[c], bKTs, sro[c], q='sp')
                        else:
                            for tt in range(16):
                                c = cnt_tm % 2
                                cnt_tm += 1
                                for kc in range(16):
                                    kb.op('pe', lambda e, i=i, c=c, kc=kc, tt=tt: e.matmul(
                                        pv[c][:], lhsT=hnT[:, kc, tt * 128:(tt + 1) * 128], rhs=wb[i][:, kc, :],
                                        start=(kc == 0), stop=(kc == 15)), reads=[bwb[i], bhnT], writes=[bpv[c]])
                                gt = half * 16 + tt
                                if blk < 9:
                                    hb = blk - 6
                                    kb.op('act', lambda e, c=c: e.activation(out=vst[c][:, :, 0:128], in_=pv[c][:].rearrange("p (h e) -> p h e", h=4),
                                                                            func=AF.Identity, scale=fl[:, 0:1]),
                                          reads=[bpv[c], bC], writes=[bvst[c]])
                                    store(Vsv[gt][:, hb * 516:(hb + 1) * 516], vst[c][:].rearrange("p h e -> p (h e)"), bvst[c], bVs, svs[c], q='act')
                                else:
                                    kb.op('act', lambda e, c=c: e.activation(out=uvst[c][:], in_=pv[c][:], func=AF.Gelu), reads=[bpv[c]], writes=[buv[c]])
                                    store(UVv[tt][:, (blk - 9) * 512:(blk - 8) * 512], uvst[c][:], buv[c], bUVs, suv[c], q='act')
                    kb.barrier()

            do_half(0)
            do_half(1)
        if stop <= 2:
            kb.enabled = False

        bMTs = Buf()
        with ExitStack() as P3:
            KTc = [alloc(P3, "KTc%d" % i, [128, 4, NW * 128], BF16) for i in range(2)]
            Vc = [alloc(P3, "Vc%d" % i, [128, NW, 4 * 129], BF16) for i in range(2)]
            QTg = alloc(P3, "QTg", [128, 12, QG * 128], BF16)
            mixed = alloc(P3, "mixed", [128, QG, 2048], F32)
            maskW = alloc(P3, "maskW", [128, 17 * 128], BF16)
            Eb = [alloc(P3, "Eb%d" % i, [128, 512], BF16) for i in range(3)]
            Pb = [alloc(P3, "Pb%d" % i, [128, 512], BF16) for i in range(3)]
            rec = alloc(P3, "rec", [128, QG], F32)
            uvt = alloc(P3, "uvt", [128, 1024], F32)
            lng = alloc(P3, "lng", [128, 512], F32)
            lnb = alloc(P3, "lnb", [128, 512], F32)
            wsTf = alloc(P3, "wsTf", [128, 4, 128], F32)
            wsT = alloc(P3, "wsT", [128, 4, 128], BF16)
            tril = alloc(P3, "tril", [128, 128], F32)
            sbT = alloc(P3, "sbT", [128, 4], F32)
            bst = alloc(P3, "bst", [128, 4, 6], F32)
            mv = alloc(P3, "mv", [128, 4, 2], F32)
            lrs = alloc(P3, "lrs", [128, 4], F32)
            vn = alloc(P3, "vn", [128, 512], F32)
            vnb = alloc(P3, "vnb", [128, 512], BF16)
            junk3 = alloc(P3, "junk3", [128, 1536], BF16)
            ss3 = alloc(P3, "ss3", [128, 4], F32)
            mnb = alloc(P3, "mnb", [128, 2048], BF16)
            mnT = alloc(P3, "mnT", [128, 2048], BF16)
            pS = [palloc(P3, "pS%d" % i, [128, 512], F32) for i in range(3)]
            pO = [palloc(P3, "pO%d" % i, [128, 512], F32) for i in range(QG)]
            pM = palloc(P3, "pM", [128, 512], F32)
            pT3 = palloc(P3, "pT3", [128, 2048], BF16)
            b3c = Buf(); bKTc = [Buf(), Buf()]; bVc = [Buf(), Buf()]; bQTg = Buf(); bmixed = Buf()
            bEb = [Buf(), Buf(), Buf()]; bPb = [Buf(), Buf(), Buf()]; brec = Buf(); buvt = Buf()
            bpS = [PSB(), PSB(), PSB()]; bpO = [PSB() for _ in range(QG)]; bpM = PSB(); bpT3 = PSB()
            bvn = Buf(); bvnb = Buf(); bstat = Buf(); bj3 = Buf(); bss3 = Buf(); brs3 = Buf(); bmnb = Buf(); bmnT = Buf()
            s3c = kb.dsem('c3'); sK = kb.dsem('ktg'); sV = kb.dsem('vg'); sQ = kb.dsem('qtg'); sUV = kb.dsem('uvt'); sMT = kb.dsem('mnT')
            load(maskW[:], maskW_d, b3c, s3c)
            load(lng[:], lng_d.to_broadcast([128, 512]), b3c, s3c)
            load(lnb[:], lnb_d.to_broadcast([128, 512]), b3c, s3c)
            load(wsTf[:], sgu_wT_d, b3c, s3c)
            load(tril[:], tril_d, b3c, s3c)
            load(sbT[:], sgu_bT_d, b3c, s3c)
            for hh in range(4):
                kb.op('dve', lambda e, hh=hh: e.tensor_tensor(out=wsT[:, hh, :], in0=wsTf[:, hh, :], in1=tril[:], op=ALU.mult), reads=[b3c], writes=[b3c])
            KTv = KTs.rearrange("h e t -> e h t")
            QTv = QTs.rearrange("h e t -> e h t")
            Vwv = Vs.rearrange("(n p) c -> p n c", p=128)
            UVv = UVs.rearrange("(n p) c -> n p c", p=128)

            uvt2 = [uvt, alloc(P3, "uvtB", [128, 1024], F32)]
            vn2 = [vn, alloc(P3, "vnB", [128, 512], F32)]
            vnb2 = [vnb, alloc(P3, "vnbB", [128, 512], BF16)]
            mnb2 = [mnb, alloc(P3, "mnbB", [128, 2048], BF16)]
            ss32 = [ss3, alloc(P3, "ss3B", [128, 4], F32)]
            bst2 = [bst, alloc(P3, "bstB", [128, 4, 6], F32)]
            mv2 = [mv, alloc(P3, "mvB", [128, 4, 2], F32)]
            lrs2 = [lrs, alloc(P3, "lrsB", [128, 4], F32)]
            buvt2 = [buvt, Buf()]; bvn2 = [bvn, Buf()]; bvnb2 = [bvnb, Buf()]; bmnb2 = [bmnb, Buf()]; bss32 = [bss3, Buf()]; brs32 = [brs3, Buf()]; bstat2 = [bstat, Buf()]

            def tail_part(g, part):
                for j in range(QG):
                    tail_tile(g, part, j)

            def tail_tile(g, part, j):
                mx = mixed2[g % 2]
                bmx = bmixed2[g % 2]
                if True:
                    ot = g * QG + j
                    uvt_, vn_, vnb_, mnb_, ss_, bst_, mv_, lrs_ = uvt2[j], vn2[j], vnb2[j], mnb2[j], ss32[j], bst2[j], mv2[j], lrs2[j]
                    buvt_, bvn_, bvnb_, bmnb_, bss_, brs_, bstat_ = buvt2[j], bvn2[j], bvnb2[j], bmnb2[j], bss32[j], brs32[j], bstat2[j]
                    if part == 1:
                        load(uvt_[:], UVv[ot], buvt_, None, q='sp')
                        for hh in range(4):
                            kb.op('dve', lambda e, hh=hh: e.bn_stats(out=bst_[:, hh, :], in_=uvt_[:, 512 + hh * 128:512 + (hh + 1) * 128]), reads=[buvt_], writes=[bstat_])
                        for hh in range(4):
                            kb.op('dve', lambda e, hh=hh: e.bn_aggr(out=mv_[:, hh, :], in_=bst_[:, hh, :]), reads=[bstat_], writes=[bstat_])
                        kb.op('act', lambda e: e.activation(out=lrs_[:], in_=mv_[:, :, 1], func=AF.Sqrt, bias=epsl[:, 0:1], scale=1.0), reads=[bstat_, bC], writes=[bstat_])
                        kb.op('dve', lambda e: e.reciprocal(out=lrs_[:], in_=lrs_[:]), reads=[bstat_], writes=[bstat_])
                        for hh in range(4):
                            kb.op('dve', lambda e, hh=hh: e.tensor_scalar(out=vn_[:, hh * 128:(hh + 1) * 128], in0=uvt_[:, 512 + hh * 128:512 + (hh + 1) * 128],
                                                                        scalar1=mv_[:, hh, 0:1], scalar2=lrs_[:, hh:hh + 1], op0=ALU.subtract, op1=ALU.mult),
                                  reads=[buvt_, bstat_], writes=[bvn_])
                        kb.op('pool', lambda e: e.tensor_tensor(out=vn_[:], in0=vn_[:], in1=lng[:], op=ALU.mult), reads=[bvn_, b3c], writes=[bvn_])
                        kb.op('pool', lambda e: e.tensor_tensor(out=vnb_[:], in0=vn_[:], in1=lnb[:], op=ALU.add), reads=[bvn_, b3c], writes=[bvnb_])
                    elif part == 2:
                        for hh in range(4):
                            kb.op('pe', lambda e, hh=hh: e.matmul(pM[:, hh * 128:(hh + 1) * 128], lhsT=wsT[:, hh, :], rhs=vnb_[:, hh * 128:(hh + 1) * 128],
                                                                 start=True, stop=True), reads=[b3c, bvnb_], writes=[bpM])
                        for hh in range(4):
                            kb.op('dve', lambda e, hh=hh, j=j: e.scalar_tensor_tensor(
                                out=mx[:, j, 1536 + hh * 128:1536 + (hh + 1) * 128], in0=pM[:, hh * 128:(hh + 1) * 128], scalar=sbT[:, hh:hh + 1],
                                in1=uvt_[:, hh * 128:(hh + 1) * 128], op0=ALU.add, op1=ALU.mult), reads=[bpM, b3c, buvt_], writes=[bmx])
                        kb.op('act', lambda e, j=j: e.activation(out=junk3[:, 0:1536], in_=mx[:, j, 0:1536], func=AF.Square, accum_out=ss_[:, 0:1]),
                              reads=[bmx], writes=[bj3, bss_])
                        kb.op('act', lambda e, j=j: e.activation(out=junk3[:, 0:512], in_=mx[:, j, 1536:2048], func=AF.Square, accum_out=ss_[:, 1:2]),
                              reads=[bmx], writes=[bj3, bss_])
                        rstd_from_ss(ss_[:, 0:1], ss_[:, 2:3], bss_, brs_, 1536.0)
                        rstd_from_ss(ss_[:, 1:2], ss_[:, 3:4], bss_, brs_, 512.0)
                        kb.op('dve', lambda e, j=j: e.tensor_scalar(out=mnb_[:, 0:1536], in0=mx[:, j, 0:1536], scalar1=ss_[:, 2:3], scalar2=None, op0=ALU.mult),
                              reads=[bmx, brs_], writes=[bmnb_])
                        kb.op('pool', lambda e, j=j: e.tensor_scalar(out=mnb_[:, 1536:2048], in0=mx[:, j, 1536:2048], scalar1=ss_[:, 3:4], scalar2=None, op0=ALU.mult),
                              reads=[bmx, brs_], writes=[bmnb_])
                    else:
                        for dc in range(16):
                            kb.op('pe', lambda e, dc=dc: e.transpose(out=pT3[:, dc * 128:(dc + 1) * 128], in_=mnb_[:, dc * 128:(dc + 1) * 128], identity=identb[:]),
                                  reads=[bmnb_, bC], writes=[bpT3])
                        kb.op('act', lambda e: e.copy(out=mnT[:, 0:1024], in_=pT3[:, 0:1024]), reads=[bpT3], writes=[bmnT])
                        kb.op('dve', lambda e: e.tensor_copy(out=mnT[:, 1024:2048], in_=pT3[:, 1024:2048]), reads=[bpT3], writes=[bmnT])
                        store(MTs[ot], mnT[:], bmnT, bMTs, None, q='sp')

            mixed2 = [mixed, alloc(P3, "mixedB", [128, QG, 2048], F32)]
            bmixed2 = [bmixed, Buf()]
            sc_exp = float(128 ** -0.5)
            for g in range(16 // QG):
                ws = g * QG
                def load_chunk(ci):
                    if ci >= 24:
                        return
                    g_, hc_ = divmod(ci, 3)
                    ws_ = g_ * QG
                    bb = ci % 2
                    load(KTc[bb][:], KTv[:, hc_ * 4:(hc_ + 1) * 4, ws_ * 128:(ws_ + NW) * 128], bKTc[bb], None, q='sp')
                    load(Vc[bb][:], Vwv[:, ws_:ws_ + NW, hc_ * 516:(hc_ + 1) * 516], bVc[bb], None, q='act')
                if g == 0:
                    load_chunk(0)
                load(QTg[:], QTv[:, :, ws * 128:(ws + QG) * 128], bQTg, sQ, q='sp', reads=[bQTs])
                for h in range(12):
                    ci = g * 3 + h // 4
                    cb = ci % 2
                    hl = h % 4
                    if h % 4 == 0:
                        load_chunk(ci + 1)
                    def kinfo(kt):
                        jlo = max(0, kt - 16); jhi = min(QG - 1, kt)
                        return jlo, jhi, (jhi - jlo + 1) * 128

                    def qkpair(kp, h=h, cb=cb, hl=hl):
                        c = kp % 3
                        for s2 in range(2):
                            kt = kp * 2 + s2
                            jlo, jhi, n = kinfo(kt)
                            kb.op('pe', lambda e, kt=kt, jlo=jlo, jhi=jhi, n=n, s2=s2, c=c: e.matmul(
                                pS[c][:, s2 * 256:s2 * 256 + n], lhsT=KTc[cb][:, hl, kt * 128:(kt + 1) * 128], rhs=QTg[:, h, jlo * 128:(jhi + 1) * 128],
                                start=True, stop=True), reads=[bKTc[cb], bQTg], writes=[bpS[c]])
                    qkpair(0)
                    qkpair(1)
                    for kp in range(NW // 2):
                        if kp + 2 < NW // 2:
                            qkpair(kp + 2)
                        c = kp % 3
                        n0 = kinfo(kp * 2)[2]; n1 = kinfo(kp * 2 + 1)[2]
                        if n0 == 256 and n1 == 256:
                            kb.op('act', lambda e, c=c: e.activation(out=Eb[c][:], in_=pS[c][:], func=AF.Exp, scale=sc_exp),
                                  reads=[bpS[c]], writes=[bEb[c]])
                        else:
                            for s2, nn in ((0, n0), (1, n1)):
                                kb.op('act', lambda e, c=c, s2=s2, nn=nn: e.activation(out=Eb[c][:, s2 * 256:s2 * 256 + nn], in_=pS[c][:, s2 * 256:s2 * 256 + nn],
                                                                                     func=AF.Exp, scale=sc_exp), reads=[bpS[c]], writes=[bEb[c]])
                        for s2 in range(2):
                            kt = kp * 2 + s2
                            jlo, jhi, n = kinfo(kt)
                            m0 = (16 + jlo - kt) * 128
                            kb.op('dve', lambda e, c=c, n=n, m0=m0, s2=s2: e.tensor_tensor(out=Pb[c][:, s2 * 256:s2 * 256 + n], in0=Eb[c][:, s2 * 256:s2 * 256 + n],
                                                                                         in1=maskW[:, m0:m0 + n], op=ALU.mult),
                                  reads=[bEb[c], b3c], writes=[bPb[c]])
                        for s2 in range(2):
                            kt = kp * 2 + s2
                            jlo, jhi, n = kinfo(kt)
                            for j in range(jlo, jhi + 1):
                                kb.op('pe', lambda e, c=c, j=j, jlo=jlo, kt=kt, h=h, s2=s2, cb=cb, hl=hl: e.matmul(
                                    pO[j][:, 0:129], lhsT=Pb[c][:, s2 * 256 + (j - jlo) * 128:s2 * 256 + (j - jlo + 1) * 128], rhs=Vc[cb][:, kt, hl * 129:(hl + 1) * 129],
                                    start=(kt == j), stop=(kt == 16 + j)), reads=[bPb[c], bVc[cb]], writes=[bpO[j]])
                    for j in range(QG):
                        kb.op('dve', lambda e, j=j: e.reciprocal(out=rec[:, j:j + 1], in_=pO[j][:, 128:129]), reads=[bpO[j]], writes=[brec])
                        kb.op('dve', lambda e, j=j, h=h, mxh=mixed2[g % 2]: e.tensor_scalar(out=mxh[:, j, h * 128:(h + 1) * 128], in0=pO[j][:, 0:128],
                                                                      scalar1=rec[:, j:j + 1], scalar2=None, op0=ALU.mult),
                              reads=[bpO[j], brec], writes=[bmixed2[g % 2]])
                    if g > 0 and h in (0, 1, 2):
                        tail_part(g - 1, h + 1)
                pass
            for part in (1, 2, 3):
                tail_part(16 // QG - 1, part)
            kb.barrier()
        if stop <= 3:
            kb.enabled = False

        bH1s = Buf(); bHN2s = Buf(); bHN2Ts = Buf()

        def load_weight_bf16(st, wdram, wdst, bwdst, scaleT, name):
            stg = [alloc(st, name + "stg%d" % i, [128, 2048], F32) for i in range(2)]
            bstg = [Buf(), Buf()]
            sst = [kb.dsem(name + 's0'), kb.dsem(name + 's1')]
            wv_ = wdram.rearrange("(kc p) f -> p kc f", p=128)
            for kc in range(16):
                i = kc % 2
                load(stg[i][:], wv_[:, kc, :], bstg[i], sst[i], q=ldq())
                if scaleT is None:
                    kb.op('pool', lambda e, i=i, kc=kc: e.tensor_copy(out=wdst[:, kc, :], in_=stg[i][:]), reads=[bstg[i]], writes=[bwdst])
                else:
                    kb.op('pool', lambda e, i=i, kc=kc: e.tensor_scalar(out=wdst[:, kc, :], in0=stg[i][:], scalar1=scaleT[:, kc:kc + 1],
                                                                       scalar2=None, op0=ALU.mult), reads=[bstg[i], bC], writes=[bwdst])

        with ExitStack() as P4:
            wo = alloc(P4, "wo", [128, 16, 2048], BF16)
            gmixT = alloc(P4, "gmixT", [128, 16], F32)
            gt1bc = alloc(P4, "gt1bc", [128, 2048], F32)
            gs2bc = alloc(P4, "gs2bc", [128, 2048], F32)
            sh2bc = alloc(P4, "sh2bc", [128, 2048], F32)
            dtmp = alloc(P4, "dtmp", [128, 128], F32)
            mnT4 = [alloc(P4, "mnT4%d" % i, [128, 16, 128], BF16) for i in range(2)]
            x4 = [alloc(P4, "x4%d" % i, [128, 2048], F32) for i in range(2)]
            tmp4 = alloc(P4, "tmp4", [128, 2048], F32)
            h14 = alloc(P4, "h14", [128, 2048], F32)
            junk4 = alloc(P4, "junk4", [128, 2048], BF16)
            ss4 = alloc(P4, "ss4", [128, 2], F32)
            hn2 = alloc(P4, "hn2", [128, 2048], BF16)
            hn2T = alloc(P4, "hn2T", [128, 2048], BF16)
            pW = [palloc(P4, "pW%d" % i, [128, 512], F32) for i in range(4)]
            pT4 = palloc(P4, "pT4", [128, 2048], BF16)
            bwo = Buf(); b4c = Buf(); bbc = Buf(); bdt = Buf(); bmn4 = [Buf(), Buf()]; bx4 = [Buf(), Buf()]; btmp4 = Buf(); bh14 = Buf()
            bj4 = Buf(); bss4 = Buf(); brs4 = Buf(); bhn2 = Buf(); bhn2T = Buf(); bpW = PSB(); bpT4 = PSB()
            s4c = kb.dsem('c4'); smn = [kb.dsem('mn40'), kb.dsem('mn41')]; sx4 = [kb.dsem('x40'), kb.dsem('x41')]
            sh1 = kb.dsem('h1st'); shn2 = kb.dsem('hn2st'); shn2T = kb.dsem('hn2Tst')
            load(gmixT[:], gmixT_d, bC, s4c)
            with ExitStack() as P4w:
                load_weight_bf16(P4w, w_out, wo, bwo, gmixT, "wo")
                make_bc(P4w, gt1bc, bbc, gt1T, bMod, pW, bpW, dtmp, bdt)
                make_bc(P4w, gs2bc, bbc, gs2T, bMod, pW, bpW, dtmp, bdt)
                make_bc(P4w, sh2bc, bbc, sh2T, bMod, pW, bpW, dtmp, bdt)
                kb.barrier()
            xov = xall.rearrange("(n p) d -> n p d", p=128)
            H1v = H1s.rearrange("(n p) d -> n p d", p=128)
            HN2v = HN2s.rearrange("(n p) d -> n p d", p=128)
            h14b = [h14, alloc(P4, "h14b", [128, 2048], F32)]
            ss4b = [ss4, alloc(P4, "ss4b", [128, 2], F32)]
            tmpB = alloc(P4, "tmpB4", [128, 2048], F32)
            bh14b = [bh14, Buf()]; bss4b = [bss4, Buf()]; brs4b = [brs4, Buf()]; btmpB = Buf()

            def stageA(ot):
                i = ot % 2
                load(mnT4[i][:], MTs[ot].rearrange("p (kc t) -> p kc t", kc=16), bmn4[i], None, q='sp')
                load(x4[i][:], xov[16 + ot], bx4[i], None, q='act')
                for nb in range(4):
                    for kc in range(16):
                        kb.op('pe', lambda e, i=i, nb=nb, kc=kc: e.matmul(pW[nb][:], lhsT=mnT4[i][:, kc, :], rhs=wo[:, kc, nb * 512:(nb + 1) * 512],
                                                                         start=(kc == 0), stop=(kc == 15)), reads=[bmn4[i], bwo], writes=[bpW])
                for nb in range(4):
                    kb.op('dve', lambda e, nb=nb: e.tensor_tensor(out=tmp4[:, nb * 512:(nb + 1) * 512], in0=pW[nb][:], in1=gt1bc[:, nb * 512:(nb + 1) * 512], op=ALU.mult),
                          reads=[bpW, bbc], writes=[btmp4])
                kb.op('pool', lambda e, i=i: e.tensor_tensor(out=h14b[i][:], in0=tmp4[:], in1=x4[i][:], op=ALU.add), reads=[btmp4, bx4[i]], writes=[bh14b[i]])
                store(H1v[ot], h14b[i][:], bh14b[i], bH1s, None, q='sp')
                kb.op('act', lambda e, i=i: e.activation(out=junk4[:], in_=h14b[i][:], func=AF.Square, accum_out=ss4b[i][:, 0:1]), reads=[bh14b[i]], writes=[bj4, bss4b[i]])
                rstd_from_ss(ss4b[i][:, 0:1], ss4b[i][:, 1:2], bss4b[i], brs4b[i], float(D))

            def stageB(ot):
                i = ot % 2
                kb.op('dve', lambda e, i=i: e.scalar_tensor_tensor(out=tmpB[:], in0=h14b[i][:], scalar=ss4b[i][:, 1:2], in1=gs2bc[:], op0=ALU.mult, op1=ALU.mult),
                      reads=[bh14b[i], brs4b[i], bbc], writes=[btmpB])
                kb.op('pool', lambda e: e.tensor_tensor(out=hn2[:], in0=tmpB[:], in1=sh2bc[:], op=ALU.add), reads=[btmpB, bbc], writes=[bhn2])
                store(HN2v[ot], hn2[:], bhn2, bHN2s, None, q='act')
                for dc in range(16):
                    kb.op('pe', lambda e, dc=dc: e.transpose(out=pT4[:, dc * 128:(dc + 1) * 128], in_=hn2[:, dc * 128:(dc + 1) * 128], identity=identb[:]),
                          reads=[bhn2, bC], writes=[bpT4])
                kb.op('act', lambda e: e.copy(out=hn2T[:, 0:1024], in_=pT4[:, 0:1024]), reads=[bpT4], writes=[bhn2T])
                kb.op('dve', lambda e: e.tensor_copy(out=hn2T[:, 1024:2048], in_=pT4[:, 1024:2048]), reads=[bpT4], writes=[bhn2T])
                store(HN2Ts[ot], hn2T[:], bhn2T, bHN2Ts, None, q='sp')

            stageA(0)
            for ot in range(16):
                if ot + 1 < 16:
                    stageA(ot + 1)
                stageB(ot)
            kb.barrier()
        if stop <= 4:
            kb.enabled = False

        with ExitStack() as P56:
            idxT = alloc(P56, "idxT", [128, 2048], I32)
            gT = alloc(P56, "gT", [128, 2048], F32)
            bidxT = Buf(); bgT = Buf()
            with ExitStack() as P5:
                wq = alloc(P5, "wq", [128, 16, 2048], BF16)
                skT = alloc(P5, "skT", [128, 16, 128], BF16)
                iota16 = alloc(P5, "iota16", [128, 16], F32)
                hg = alloc(P5, "hg", [128, 16, 512], BF16)
                qT = alloc(P5, "qT", [128, 16, 512], BF16)
                sc = alloc(P5, "sc", [128, 16, 128], F32)
                wk = alloc(P5, "wk", [128, 16, 128], F32)
                stv = alloc(P5, "stv", [128, 16, 16], F32)
                siv = alloc(P5, "siv", [128, 16, 16], U32)
                sif = alloc(P5, "sif", [128, 16, 16], F32)
                cand = alloc(P5, "cand", [128, 8, 256], F32)
                bs = alloc(P5, "bs", [128, 8, 16], F32)
                bp = alloc(P5, "bp", [128, 8, 16], U32)
                bpf = alloc(P5, "bpf", [128, 8, 16], F32)
                pi_ = alloc(P5, "pi_", [128, 8, 16], F32)
                pj_ = alloc(P5, "pj_", [128, 8, 16], F32)
                pii = alloc(P5, "pii", [128, 8, 16], I32)
                eq = alloc(P5, "eq", [128, 128, 16], F32)
                ea = alloc(P5, "ea", [128, 128], F32)
                ebb = alloc(P5, "ebb", [128, 128], F32)
                idxf = alloc(P5, "idxf", [128, 128], F32)
                gte = alloc(P5, "gte", [128, 8, 16], F32)
                gsum = alloc(P5, "gsum", [128, 8], F32)
                pQ = [palloc(P5, "pQ%d" % i, [128, 512], F32) for i in range(2)]
                pS5 = [palloc(P5, "pS5%d" % i, [128, 512], F32) for i in range(4)]
                pTr = palloc(P5, "pTr", [128, 512], F32)
                bwq = Buf(); b5c = Buf(); bhg = Buf(); bqT = Buf(); bsc = Buf(); bwk = Buf(); bst5 = Buf(); bcand = Buf(); bcwk = Buf()
                bbs = Buf(); bsel = Buf(); bidxf = Buf(); bgte = Buf(); bpQ = [PSB(), PSB()]; bpS5 = PSB(); bpTr = PSB()
                s5c = kb.dsem('c5'); shg = kb.dsem('hg')
                load(iota16[:], iota_d, b5c, s5c)
                with ExitStack() as P5w:
                    skTf = alloc(P5w, "skTf", [128, 16, 128], F32)
                    load(skTf[:], skT_d, b5c, s5c)
                    kb.op('dve', lambda e: e.tensor_copy(out=skT[:], in_=skTf[:]), reads=[b5c], writes=[b5c])
                    load_weight_bf16(P5w, w_q, wq, bwq, None, "wq")
                    kb.barrier()
                hg2 = [hg, alloc(P5, "hgB", [128, 16, 512], BF16)]
                bhg2 = [bhg, Buf()]

                def load_hg(gq):
                    if gq < 4:
                        for jj_ in range(4):
                            load(hg2[gq % 2][:, :, jj_ * 128:(jj_ + 1) * 128], HN2Ts[gq * 4 + jj_].rearrange("p (kc t) -> p kc t", kc=16), bhg2[gq % 2], None, q=ldq())
                load_hg(0)
                for g4 in range(4):
                    load_hg(g4 + 1)
                    hgc = hg2[g4 % 2]
                    bhgc = bhg2[g4 % 2]
                    for hp in range(16):
                        c = hp % 2
                        for kc in range(16):
                            kb.op('pe', lambda e, c=c, kc=kc, hp=hp, hgc=hgc: e.matmul(pQ[c][:], lhsT=wq[:, kc, hp * 128:(hp + 1) * 128], rhs=hgc[:, kc, :],
                                                                             start=(kc == 0), stop=(kc == 15)), reads=[bwq, bhgc], writes=[bpQ[c]])
                        kb.op('act', lambda e, c=c, hp=hp: e.copy(out=qT[:, hp, :], in_=pQ[c][:]), reads=[bpQ[c]], writes=[bqT])
                    emit_cast(8, after=[bqT])
                    for jj in range(4):
                        ot = g4 * 4 + jj
                        for hp in range(16):
                            kb.op('pe', lambda e, hp=hp, jj=jj: e.matmul(pS5[hp // 4][:, (hp % 4) * 128:(hp % 4 + 1) * 128], lhsT=qT[:, hp, jj * 128:(jj + 1) * 128],
                                                                        rhs=skT[:, hp, :], start=True, stop=True), reads=[bqT, b5c], writes=[bpS5])
                        for b4 in range(4):
                            kb.op('act', lambda e, b4=b4: e.copy(out=sc[:, b4 * 4:(b4 + 1) * 4, :], in_=pS5[b4][:].rearrange("p (a k) -> p a k", a=4)),
                                  reads=[bpS5], writes=[bsc])
                        for hp in range(16):
                            kb.op('dve', lambda e, hp=hp: e.max(out=stv[:, hp, 0:8], in_=sc[:, hp, :]), reads=[bsc], writes=[bst5])
                        for hp in range(16):
                            kb.op('dve', lambda e, hp=hp: e.max_index(out=siv[:, hp, 0:8], in_max=stv[:, hp, 0:8], in_values=sc[:, hp, :]), reads=[bsc, bst5], writes=[bst5])
                        for hp in range(16):
                            kb.op('dve', lambda e, hp=hp: e.match_replace(out=wk[:, hp, :], in_to_replace=stv[:, hp, 0:8], in_values=sc[:, hp, :], imm_value=-1e30),
                                  reads=[bsc, bst5], writes=[bwk])
                        for hp in range(16):
                            kb.op('dve', lambda e, hp=hp: e.max(out=stv[:, hp, 8:16], in_=wk[:, hp, :]), reads=[bwk], writes=[bst5])
                        for hp in range(16):
                            kb.op('dve', lambda e, hp=hp: e.max_index(out=siv[:, hp, 8:16], in_max=stv[:, hp, 8:16], in_values=wk[:, hp, :]), reads=[bwk, bst5], writes=[bst5])
                        kb.op('dve', lambda e: e.tensor_copy(out=sif[:], in_=siv[:]), reads=[bst5], writes=[bst5])
                        stv4 = stv[:].rearrange("t (h p) k -> t h p k", p=2)
                        sif4 = sif[:].rearrange("t (h p) k -> t h p k", p=2)
                        for h in range(8):
                            kb.op('dve', lambda e, h=h: e.tensor_tensor(
                                out=cand[:, h, :].rearrange("t (i j) -> t i j", i=16),
                                in0=stv4[:, h, 0, :].unsqueeze(2).to_broadcast([128, 16, 16]),
                                in1=stv4[:, h, 1, :].unsqueeze(1).to_broadcast([128, 16, 16]), op=ALU.add), reads=[bst5], writes=[bcand])
                        for h in range(8):
                            kb.op('dve', lambda e, h=h: e.max(out=bs[:, h, 0:8], in_=cand[:, h, :]), reads=[bcand], writes=[bbs])
                        for h in range(8):
                            kb.op('dve', lambda e, h=h: e.max_index(out=bp[:, h, 0:8], in_max=bs[:, h, 0:8], in_values=cand[:, h, :]), reads=[bcand, bbs], writes=[bbs])
                        for h in range(8):
                            kb.op('dve', lambda e, h=h: e.match_replace(out=wk[:].rearrange("t a k -> t (a k)")[:, h * 256:(h + 1) * 256], in_to_replace=bs[:, h, 0:8], in_values=cand[:, h, :], imm_value=-1e30),
                                  reads=[bcand, bbs], writes=[bwk])
                        for h in range(8):
                            kb.op('dve', lambda e, h=h: e.max(out=bs[:, h, 8:16], in_=wk[:].rearrange("t a k -> t (a k)")[:, h * 256:(h + 1) * 256]), reads=[bwk], writes=[bbs])
                        for h in range(8):
                            kb.op('dve', lambda e, h=h: e.max_index(out=bp[:, h, 8:16], in_max=bs[:, h, 8:16], in_values=wk[:].rearrange("t a k -> t (a k)")[:, h * 256:(h + 1) * 256]), reads=[bwk, bbs], writes=[bbs])
                        kb.op('dve', lambda e: e.tensor_copy(out=bpf[:], in_=bp[:]), reads=[bbs], writes=[bsel])
                        kb.op('dve', lambda e: e.tensor_scalar(out=pj_[:], in0=bpf[:], scalar1=1.0 / 16.0, scalar2=None, op0=ALU.mult), reads=[bsel], writes=[bsel])
                        kb.op('dve', lambda e: e.tensor_copy(out=pii[:], in_=pj_[:]), reads=[bsel], writes=[bsel])
                        kb.op('dve', lambda e: e.tensor_copy(out=pi_[:], in_=pii[:]), reads=[bsel], writes=[bsel])
                        kb.op('dve', lambda e: e.tensor_tensor(out=pj_[:], in0=pi_[:], in1=pj_[:], op=ALU.is_gt), reads=[bsel], writes=[bsel])
                        kb.op('dve', lambda e: e.tensor_tensor(out=pi_[:], in0=pi_[:], in1=pj_[:], op=ALU.subtract), reads=[bsel], writes=[bsel])
                        kb.op('dve', lambda e: e.scalar_tensor_tensor(out=pj_[:], in0=pi_[:], scalar=-16.0, in1=bpf[:], op0=ALU.mult, op1=ALU.add), reads=[bsel], writes=[bsel])
                        for which, pp, dst in ((0, pi_, ea), (1, pj_, ebb)):
                            kb.op('dve', lambda e, pp=pp: e.tensor_tensor(
                                out=eq[:], in0=pp[:].rearrange("t h k -> t (h k)").unsqueeze(2).to_broadcast([128, 128, 16]),
                                in1=iota16[:].unsqueeze(1).to_broadcast([128, 128, 16]), op=ALU.is_equal), reads=[bsel, b5c], writes=[bsel])
                            for h in range(8):
                                kb.op('dve', lambda e, h=h, which=which: e.tensor_tensor(
                                    out=eq[:, h * 16:(h + 1) * 16, :], in0=eq[:, h * 16:(h + 1) * 16, :],
                                    in1=sif4[:, h, which, :].unsqueeze(1).to_broadcast([128, 16, 16]), op=ALU.mult), reads=[bsel, bst5], writes=[bsel])
                            kb.op('dve', lambda e, dst=dst: e.tensor_reduce(out=dst[:], in_=eq[:], axis=AX.X, op=ALU.add), reads=[bsel], writes=[bsel])
                        kb.op('dve', lambda e: e.scalar_tensor_tensor(out=idxf[:], in0=ea[:], scalar=128.0, in1=ebb[:], op0=ALU.mult, op1=ALU.add), reads=[bsel], writes=[bidxf])
                        kb.op('dve', lambda e: e.tensor_tensor(out=gte[:], in0=bs[:], in1=bs[:, :, 0:1].to_broadcast([128, 8, 16]), op=ALU.subtract), reads=[bbs], writes=[bgte])
                        kb.op('act', lambda e: e.activation(out=gte[:], in_=gte[:], func=AF.Exp), reads=[bgte], writes=[bgte])
                        kb.op('dve', lambda e: e.tensor_reduce(out=gsum[:], in_=gte[:], axis=AX.X, op=ALU.add), reads=[bgte], writes=[bgte])
                        kb.op('dve', lambda e: e.reciprocal(out=gsum[:], in_=gsum[:]), reads=[bgte], writes=[bgte])
                        kb.op('dve', lambda e: e.tensor_tensor(out=gte[:], in0=gte[:], in1=gsum[:].unsqueeze(2).to_broadcast([128, 8, 16]), op=ALU.mult), reads=[bgte], writes=[bgte])
                        kb.op('pe', lambda e: e.transpose(out=pTr[:, 0:128], in_=idxf[:], identity=identf[:]), reads=[bidxf, bC], writes=[bpTr])
                        kb.op('pe', lambda e: e.transpose(out=pTr[:, 128:256], in_=gte[:].rearrange("t h k -> t (h k)"), identity=identf[:]), reads=[bgte, bC], writes=[bpTr])
                        kb.op('dve', lambda e, ot=ot: e.tensor_copy(out=idxT[:, ot * 128:(ot + 1) * 128], in_=pTr[:, 0:128]), reads=[bpTr], writes=[bidxT])
                        kb.op('act', lambda e, ot=ot: e.copy(out=gT[:, ot * 128:(ot + 1) * 128], in_=pTr[:, 128:256]), reads=[bpTr], writes=[bgT])
                if dbg:
                    sdb5 = kb.dsem('dbg5')
                    bdb5 = Buf()
                    store(dbgidx, idxT[:], bidxT, bdb5, sdb5)
                    store(dbgg, gT[:], bgT, bdb5, sdb5)
                kb.barrier()
            if stop <= 5:
                kb.enabled = False

            flush_cast()
            kb.barrier()
            with ExitStack() as P6:
                NB = 6
                gb = [alloc(P6, "gb%d" % i, [128, 4096], BF16) for i in range(NB)]
                hn2t = [alloc(P6, "hn2t%d" % i, [128, 2048], BF16) for i in range(2)]
                junk6 = alloc(P6, "junk6", [128, 1024], BF16)
                NA = 4
                acc = [alloc(P6, "acc%d" % i, [128, 8], F32) for i in range(NA)]
                win = [alloc(P6, "win%d" % i, [128, 256], BF16) for i in range(2)]
                gt2bc = alloc(P6, "gt2bc", [128, 2048], F32)
                gfbc = alloc(P6, "gfbc", [128, 2048], F32)
                dtmp6 = alloc(P6, "dtmp6", [128, 128], F32)
                h16 = alloc(P6, "h16", [128, 2048], F32)
                tmp6 = alloc(P6, "tmp6", [128, 2048], F32)
                hf6 = alloc(P6, "hf6", [128, 2048], F32)
                o6 = alloc(P6, "o6", [128, 2048], F32)
                ss6 = alloc(P6, "ss6", [128, 2], F32)
                pXh = [palloc(P6, "pXh%d" % i, [128, 1024], F32) for i in range(2)]
                pOut = [palloc(P6, "pOut%d" % i, [128, 512], F32) for i in range(4)]
                bgb = [Buf() for _ in range(NB)]; bhn2t = [Buf(), Buf()]; bj6 = Buf(); bacc = [Buf() for _ in range(NA)]
                bwin = [Buf(), Buf()]; bbc6 = Buf(); bdt6 = Buf(); bh16 = Buf(); btmp6 = Buf(); bhf6 = Buf(); bo6 = Buf(); bss6 = Buf(); brs6 = Buf()
                bpXh = [PSB(), PSB()]; bpOut = PSB(); bOut = Buf(); bpXall = PSB()
                sgb = [kb.dsem('gb%d' % i) for i in range(NB)]
                ps4 = [pXh[0][:, 0:512], pXh[0][:, 512:1024], pXh[1][:, 0:512], pXh[1][:, 512:1024]]
                make_bc(P6, gt2bc, bbc6, gt2T, bMod, ps4, bpXall, dtmp6, bdt6)
                load(gfbc[:], gfin_d.to_broadcast([128, 2048]), bbc6, None)
                for i in range(2):
                    kb.op('dve', lambda e, i=i: e.memset(win[i][:], 0.0), writes=[bwin[i]])
                kb.barrier()
                HN2v = HN2s.rearrange("(n p) d -> n p d", p=128)
                H1v = H1s.rearrange("(n p) d -> n p d", p=128)
                Ov = out_d.rearrange("(n p) d -> n p d", p=128)
                loaded = set()

                def ensure_tile(ot):
                    if ot not in loaded and ot < 16:
                        loaded.add(ot)
                        load(hn2t[ot % 2][:], HN2v[ot], bhn2t[ot % 2], None, q='sp')

                def emit_xb(t):
                    ot, tt = divmod(t, 128)
                    ensure_tile(ot)
                    hi = ot % 2
                    for hf in range(2):
                        for bb in range(2):
                            b4 = hf * 2 + bb
                            kb.op('pe', lambda e, hf=hf, bb=bb, b4=b4, tt=tt, hi=hi: e.matmul(
                                pXh[hf][:, bb * 512:(bb + 1) * 512], lhsT=identb[:, tt:tt + 1].to_broadcast([128, 128]),
                                rhs=hn2t[hi][:, b4 * 512:(b4 + 1) * 512], start=True, stop=True),
                                reads=[bC, bhn2t[hi]], writes=[bpXh[hf]])

                def emit_gather(t):
                    u = t % NB
                    kb.dma('pool', lambda e, u=u, t=t: e.indirect_dma_start(
                        out=gb[u][:], out_offset=None, in_=UV16, in_offset=bass.IndirectOffsetOnAxis(ap=idxT[:, t:t + 1], axis=0)),
                        sgb[u], reads=[bidxT], writes=[bgb[u]])

                emit_gather(0)
                emit_xb(0)
                for t in range(2048):
                    ot, tt = divmod(t, 128)
                    u = t % NB
                    a4 = t % NA
                    a2 = t % 2
                    if t + 1 < 2048:
                        emit_gather(t + 1)
                    for hf in range(2):
                        kb.op('dve', lambda e, hf=hf, u=u, a4=a4: e.scalar_tensor_tensor(
                            out=junk6[:], in0=gb[u][:, hf * 1024:(hf + 1) * 1024], scalar=1.0, in1=pXh[hf][:], op0=ALU.mult, op1=ALU.mult,
                            accum_out=acc[a4][:, hf:hf + 1]), reads=[bgb[u], bpXh[hf]], writes=[bj6, bacc[a4]])
                    kb.op('act', lambda e, a4=a4: e.activation(out=acc[a4][:, 2:4], in_=acc[a4][:, 0:2], func=AF.Identity, accum_out=acc[a4][:, 4:5]),
                          reads=[bacc[a4]], writes=[bacc[a4]])
                    kb.op('act', lambda e, a4=a4: e.activation(out=acc[a4][:, 5:6], in_=acc[a4][:, 4:5], func=AF.Gelu), reads=[bacc[a4]], writes=[bacc[a4]])
                    kb.op('act', lambda e, a4=a4, a2=a2, t=t: e.activation(out=win[a2][:, 127:128], in_=acc[a4][:, 5:6], func=AF.Identity, scale=gT[:, t:t + 1]),
                          reads=[bacc[a4], bgT], writes=[bwin[a2]])
                    if t + 1 < 2048:
                        emit_xb(t + 1)
                    for b4 in range(4):
                        kb.op('pe', lambda e, b4=b4, a2=a2, tt=tt, u=u: e.matmul(pOut[b4][:], lhsT=win[a2][:, 127 - tt:255 - tt], rhs=gb[u][:, 2048 + b4 * 512:2048 + (b4 + 1) * 512],
                                                                                start=(tt == 0), stop=(tt == 127)), reads=[bwin[a2], bgb[u]], writes=[bpOut])
                    if tt == 127:
                        load(h16[:], H1v[ot], bh16, None, q='act')
                        for b4 in range(4):
                            kb.op('dve', lambda e, b4=b4: e.tensor_tensor(out=tmp6[:, b4 * 512:(b4 + 1) * 512], in0=pOut[b4][:], in1=gt2bc[:, b4 * 512:(b4 + 1) * 512], op=ALU.mult),
                                  reads=[bpOut, bbc6], writes=[btmp6])
                        kb.op('pool', lambda e: e.tensor_tensor(out=hf6[:], in0=tmp6[:], in1=h16[:], op=ALU.add), reads=[btmp6, bh16], writes=[bhf6])
                        kb.op('act', lambda e: e.activation(out=tmp6[:], in_=hf6[:], func=AF.Square, accum_out=ss6[:, 0:1]), reads=[bhf6], writes=[btmp6, bss6])
                        rstd_from_ss(ss6[:, 0:1], ss6[:, 1:2], bss6, brs6, float(D))
                        kb.op('dve', lambda e: e.scalar_tensor_tensor(out=o6[:], in0=hf6[:], scalar=ss6[:, 1:2], in1=gfbc[:], op0=ALU.mult, op1=ALU.mult),
                              reads=[bhf6, brs6, bbc6], writes=[bo6])
                        store(Ov[ot], o6[:], bo6, bOut, None, q='sp')
                kb.barrier()

        blk = es.enter_context(nc.Block())
        kb.emit(blk)
    return nc


_NC = None


def kernel(x, c, positions, w_ada, b_ada, g_norm1, w_in, g_attn_out, g_sgu_out, sgu_w, sgu_b, sgu_ln_g, sgu_ln_b,
           w_out, g_norm2, peer_w_q, peer_sub_keys, peer_u, peer_v, g_final, _prep_only=False):
    global _NC
    f32 = np.float32
    x = np.asarray(x, f32); c = np.asarray(c, f32); positions = np.asarray(positions, np.int32)

    def colT(v, n):
        return np.ascontiguousarray(np.asarray(v, f32).reshape(n, 128).T)

    half = 64
    inv = (10000.0 ** (-np.arange(half, dtype=np.float64) / half))
    inv2 = np.concatenate([inv, inv]) / (2.0 * np.pi)
    inv2 = inv2.astype(f32).reshape(128, 1)
    sgn = np.concatenate([-np.ones(64), np.ones(64)]).astype(f32).reshape(128, 1)
    k = np.arange(128)[:, None]
    cidx = np.arange(17 * 128)[None, :]
    delta = cidx - k
    cm = ((delta >= 0) & (delta <= 128)).astype(f32) + ((delta >= 0) & (delta % 4 == 0) & (delta <= 512)).astype(f32) \
        + ((delta >= 0) & (delta % 16 == 0) & (delta <= 2048)).astype(f32)
    maskW = cm.astype(ml_dtypes.bfloat16)
    perm = np.zeros((128, 128), f32)
    perm[(np.arange(128) + 64) % 128, np.arange(128)] = 1.0
    perm = perm.astype(ml_dtypes.bfloat16)
    identf = np.eye(128, dtype=f32)
    iota16 = np.tile(np.arange(16, dtype=f32)[None, :], (128, 1))
    trilT = (np.arange(128)[:, None] <= np.arange(128)[None, :]).astype(f32)

    shared = {
        "w_ada": np.ascontiguousarray(np.asarray(w_ada, f32)[0]),
        "b_adaT": colT(np.asarray(b_ada)[0], 96),
        "gn1T": colT(np.asarray(g_norm1)[0], 16),
        "gn2T": colT(np.asarray(g_norm2)[0], 16),
        "w_in": np.ascontiguousarray(np.asarray(w_in, f32)[0]),
        "w_out": np.ascontiguousarray(np.asarray(w_out, f32)[0]),
        "gmixT": colT(np.concatenate([np.asarray(g_attn_out)[0], np.asarray(g_sgu_out)[0]]), 16),
        "sgu_wT": np.ascontiguousarray(np.transpose(np.asarray(sgu_w, f32)[0], (2, 0, 1))),
        "sgu_bT": np.ascontiguousarray(np.asarray(sgu_b, f32)[0].T),
        "lng": np.ascontiguousarray(np.asarray(sgu_ln_g, f32)[0].reshape(1, 512)),
        "lnb": np.ascontiguousarray(np.asarray(sgu_ln_b, f32)[0].reshape(1, 512)),
        "w_q": np.ascontiguousarray(np.asarray(peer_w_q, f32)[0]),
        "skT": np.ascontiguousarray(np.transpose(np.asarray(peer_sub_keys, f32)[0].reshape(16, 128, 128), (2, 0, 1))),
        "peer_u": np.ascontiguousarray(np.asarray(peer_u, f32)[0]),
        "peer_v": np.ascontiguousarray(np.asarray(peer_v, f32)[0]),
        "gfin": np.ascontiguousarray(np.asarray(g_final, f32).reshape(1, D)),
        "inv2": inv2, "sgn": sgn, "maskW": maskW, "perm": perm, "identf": identf, "iota16": iota16, "trilT": trilT,
    }
    in_maps = []
    for core in range(8):
        b = core // 2
        hf = core % 2
        xa = np.zeros((NCTX + NT, D), f32)
        pa = np.zeros((1, NCTX + NT), np.int32)
        xa[NCTX:] = x[b, hf * NT:(hf + 1) * NT]
        pa[0, NCTX:] = positions[b, hf * NT:(hf + 1) * NT]
        if hf == 1:
            xa[:NCTX] = x[b, 0:NCTX]
            pa[0, :NCTX] = positions[b, 0:NCTX]
        m = dict(shared)
        m["xall"] = xa
        m["pos"] = pa
        m["condT"] = colT(c[b], 16)
        m["flag"] = np.full((128, 1), float(hf), f32)
        in_maps.append(m)
    if _prep_only:
        return in_maps
    if _NC is None:
        import os
        _NC = build(stop=float(os.environ.get('KSTOP', '99')))
    res = run_bass_kernel_spmd(_NC, in_maps, core_ids=list(range(8)))
    out = np.zeros((4, 4096, D), f32)
    for core in range(8):
        b = core // 2
        hf = core % 2
        out[b, hf * NT:(hf + 1) * NT] = np.asarray(res.results[core]["out"], f32)
    return out
```

```python
import numpy as np
import ml_dtypes
from contextlib import ExitStack
import concourse.bass as bass
import concourse.mybir as mybir
from concourse.bass_utils import run_bass_kernel_spmd

F32 = mybir.dt.float32
BF16 = mybir.dt.bfloat16
U32 = mybir.dt.uint32
I32 = mybir.dt.int32
AF = mybir.ActivationFunctionType
ALU = mybir.AluOpType
AX = mybir.AxisListType

D = 2048
NT = 2048
NCTX = 2048
DIN = 5632
ENG = ('pe', 'act', 'dve', 'pool', 'sp')
QG = 2
NW = 16 + QG


class LazySem:
    def __init__(self, name):
        self.name = name
        self.real = None


class Buf:
    def __init__(self, name=''):
        self.w = None
        self.r = {}
        self.name = name
        self.sems = {}
        self.psum = False


def PSB():
    b = Buf('psum')
    b.psum = True
    return b


class KB:
    EP = 16000

    def __init__(self, nc, es):
        self.nc = nc
        self.es = es
        self.streams = {e: [] for e in ENG}
        self.cnt = {e: 0 for e in ENG}
        self.esem = {e: [] for e in ENG}
        self.known = {e: {} for e in ENG}
        self.dcount = {}
        self.dsems = []
        self.nsem = 0
        self.enabled = True

    def _newsem(self, name):
        self.nsem += 1
        return self.es.enter_context(self.nc.semaphore(name))

    def dsem(self, name):
        return LazySem(name)

    def _real(self, ls):
        if ls.real is None:
            sm = self._newsem('d%d_%s' % (self.nsem, ls.name))
            self.dcount[id(sm)] = 0
            self.dsems.append(sm)
            ls.real = sm
        return ls.real

    def buf_sem(self, buf, kind):
        if kind not in buf.sems:
            buf.sems[kind] = LazySem(kind + '_' + (buf.name or 'b'))
        return buf.sems[kind]

    def _eev(self, e):
        c = self.cnt[e]
        ep = (c - 1) // self.EP
        while len(self.esem[e]) <= ep:
            self.esem[e].append(self._newsem('e_%s_%d' % (e, len(self.esem[e]))))
        return (self.esem[e][ep], (c - 1) % self.EP + 1)

    def _deps(self, e, reads, writes):
        evs = []
        for b in reads:
            if b.w is not None:
                evs.append(b.w)
            if b.psum:
                evs.extend(ev for ev in b.r.values() if ev[3] != e)
        for b in writes:
            if b.w is not None:
                evs.append(b.w)
            evs.extend(b.r.values())
        waits = []
        own = self.esem[e]
        k = self.known[e]
        for (sm, val, isd, _eng) in evs:
            if e == 'pe' and any(sm is x for x in own):
                continue
            if isd:
                val = 16 * self.dcount[id(sm)]
            if k.get(id(sm), 0) < val:
                k[id(sm)] = val
                waits.append((sm, val))
        return waits

    def _book(self, ev, reads, writes):
        for b in reads:
            b.r[id(ev[0])] = ev
        for b in writes:
            b.w = ev
            b.r = {}

    def op(self, e, fn, reads=(), writes=()):
        if not self.enabled:
            return
        waits = self._deps(e, reads, writes)
        self.cnt[e] += 1
        sm, val = self._eev(e)
        self.streams[e].append((waits, fn, (sm, 1)))
        self._book((sm, val, False, e), reads, writes)

    def dma(self, q, fn, sem, reads=(), writes=(), after=()):
        if not self.enabled:
            return
        sem = self._real(sem)
        waits = self._deps(q, list(reads) + list(after), writes)
        self.dcount[id(sem)] += 1
        self.streams[q].append((waits, fn, (sem, 16)))
        self._book((sem, 16 * self.dcount[id(sem)], True, q), reads, writes)

    def barrier(self):
        if not self.enabled:
            return
        for e in ENG:
            waits = []
            k = self.known[e]
            for e2 in ENG:
                if self.cnt[e2] > 0 and not (e == 'pe' and e2 == 'pe'):
                    sm, val = self._eev(e2)
                    if k.get(id(sm), 0) < val:
                        k[id(sm)] = val
                        waits.append((sm, val))
            for sm in self.dsems:
                val = 16 * self.dcount[id(sm)]
                if val > 0 and k.get(id(sm), 0) < val:
                    k[id(sm)] = val
                    waits.append((sm, val))
            if waits:
                self.streams[e].append((waits, None, None))

    def emit(self, blk):
        emap = {'pe': blk.tensor, 'act': blk.scalar, 'dve': blk.vector, 'pool': blk.gpsimd, 'sp': blk.sync}
        for e in ENG:
            def body(eng, e=e):
                for waits, fn, inc in self.streams[e]:
                    for sm, val in waits:
                        eng.wait_ge(sm, val)
                    if fn is not None:
                        fn(eng).then_inc(inc[0], inc[1])
            emap[e](body)


def build(stop=99, dbg=False):
    nc = bass.Bass("TRN2", target_bir_lowering=False)
    SK = "ExternalOutput" if dbg else "Internal"

    def din(name, shape, dt=F32):
        return nc.dram_tensor(name, list(shape), dt, kind="ExternalInput").ap()

    xall = din("xall", [NCTX + NT, D])
    condT_d = din("condT", [128, 16])
    pos_d = din("pos", [1, NCTX + NT], I32)
    flag_d = din("flag", [128, 1])
    w_ada = din("w_ada", [D, 6 * D])
    b_adaT = din("b_adaT", [128, 96])
    gn1T_d = din("gn1T", [128, 16])
    gn2T_d = din("gn2T", [128, 16])
    w_in = din("w_in", [D, DIN])
    w_out = din("w_out", [D, D])
    gmixT_d = din("gmixT", [128, 16])
    sgu_wT_d = din("sgu_wT", [128, 4, 128])
    sgu_bT_d = din("sgu_bT", [128, 4])
    lng_d = din("lng", [1, 512])
    lnb_d = din("lnb", [1, 512])
    w_q = din("w_q", [D, D])
    skT_d = din("skT", [128, 16, 128])
    peer_u = din("peer_u", [16384, D])
    peer_v = din("peer_v", [16384, D])
    gfin_d = din("gfin", [1, D])
    inv2_d = din("inv2", [128, 1])
    sgn_d = din("sgn", [128, 1])
    maskW_d = din("maskW", [128, 17 * 128], BF16)
    perm_d = din("perm", [128, 128], BF16)
    identf_d = din("identf", [128, 128])
    iota_d = din("iota16", [128, 16])
    tril_d = din("trilT", [128, 128])
    out_d = nc.dram_tensor("out", [NT, D], F32, kind="ExternalOutput").ap()

    dbgmod = nc.dram_tensor("dbgmod", [128, 128], F32, kind=SK).ap()
    dbgidx = nc.dram_tensor("dbgidx", [128, 2048], I32, kind=SK).ap()
    dbgg = nc.dram_tensor("dbgg", [128, 2048], F32, kind=SK).ap()
    KTs = nc.dram_tensor("KTs", [12, 128, NCTX + NT], BF16, kind=SK).ap()
    QTs = nc.dram_tensor("QTs", [12, 128, NT], BF16, kind=SK).ap()
    Vs = nc.dram_tensor("Vs", [NCTX + NT, 12 * 129], BF16, kind=SK).ap()
    UVs = nc.dram_tensor("UVs", [NT, 1024], F32, kind=SK).ap()
    MTs = nc.dram_tensor("MTs", [16, 128, 2048], BF16, kind=SK).ap()
    H1s = nc.dram_tensor("H1s", [NT, D], F32, kind=SK).ap()
    HN2s = nc.dram_tensor("HN2s", [NT, D], BF16, kind=SK).ap()
    HN2Ts = nc.dram_tensor("HN2Ts", [16, 128, 2048], BF16, kind=SK).ap()
    UV16 = nc.dram_tensor("UV16", [16384, 4096], BF16).ap()

    with ExitStack() as es:
        kb = KB(nc, es)

        uniq = [0]

        def alloc(st, name, shape, dt):
            uniq[0] += 1
            return st.enter_context(nc.sbuf_tensor("s%d_%s" % (uniq[0], name), list(shape), dt))

        def palloc(st, name, shape, dt):
            uniq[0] += 1
            return st.enter_context(nc.psum_tensor("p%d_%s" % (uniq[0], name), list(shape), dt))

        qrr = [0]

        def ldq():
            qrr[0] += 1
            return 'sp' if qrr[0] % 2 else 'act'

        def load(dst_ap, src_ap, buf, sem, q=None, reads=()):
            kb.dma(q or 'sp', lambda e: e.dma_start(out=dst_ap, in_=src_ap), kb.buf_sem(buf, 'l'), reads=(), writes=[buf])

        def store(dst_ap, src_ap, srcbuf, dstbuf, sem, q=None):
            kb.dma(q or 'sp', lambda e: e.dma_start(out=dst_ap, in_=src_ap), kb.buf_sem(srcbuf, 's'), reads=[srcbuf], writes=())

        cast_sem = kb.dsem('cast')
        cast_todo = []
        for cch in range(16):
            rows = slice(cch * 1024, (cch + 1) * 1024)
            cast_todo.append((UV16[rows, 0:2048], peer_u[rows, :]))
            cast_todo.append((UV16[rows, 2048:4096], peer_v[rows, :]))

        def emit_cast(k, after=()):
            for _ in range(k):
                if cast_todo:
                    o_ap, i_ap = cast_todo.pop(0)
                    kb.dma('pool', lambda e, o_ap=o_ap, i_ap=i_ap: e.dma_start(out=o_ap, in_=i_ap), cast_sem, after=after)

        def flush_cast():
            emit_cast(len(cast_todo))

        G = es
        identf = alloc(G, "identf", [128, 128], F32)
        identb = alloc(G, "identb", [128, 128], BF16)
        onesf = alloc(G, "onesf", [128, 128], F32)
        perm = alloc(G, "perm", [128, 128], BF16)
        flag = alloc(G, "flag", [128, 1], F32)
        onec = alloc(G, "onec", [128, 1], F32)
        modT = alloc(G, "modT", [128, 96], F32)
        gs1T = alloc(G, "gs1T", [128, 16], F32)
        gs2T = alloc(G, "gs2T", [128, 16], F32)
        epsc = alloc(G, "epsc", [128, 1], F32)
        epsl = alloc(G, "epsl", [128, 1], F32)
        bC = Buf('const')
        bMod = Buf('mod')
        sC = kb.dsem('const')
        load(identf[:], identf_d, bC, sC)
        load(perm[:], perm_d, bC, sC)
        load(flag[:], flag_d, bC, sC)
        kb.op('dve', lambda e: e.tensor_copy(out=identb[:], in_=identf[:]), reads=[bC], writes=[bC])
        kb.op('dve', lambda e: e.memset(onesf[:], 1.0), writes=[bC])
        kb.op('dve', lambda e: e.memset(onec[:], 1.0), writes=[bC])
        kb.op('dve', lambda e: e.memset(epsc[:, 0:1], 1e-6), writes=[bC])
        kb.op('dve', lambda e: e.memset(epsl[:, 0:1], 1e-5), writes=[bC])

        def rstd_from_ss(ss, rstd, bss, brs, n, epscol=0):
            kb.op('act', lambda e: e.activation(out=rstd, in_=ss, func=AF.Sqrt, bias=epsc[:, 0:1], scale=1.0 / n),
                  reads=[bss, bC], writes=[brs])
            kb.op('dve', lambda e: e.reciprocal(out=rstd, in_=rstd), reads=[brs], writes=[brs])

        def make_bc(st, dst, dstbuf, srcT, srcbuf, ps4, psbuf, tmpd, tmpbuf):
            for dc in range(16):
                kb.op('dve', lambda e, dc=dc: e.tensor_scalar(out=tmpd[:], in0=identf[:], scalar1=srcT[:, dc:dc + 1], scalar2=None, op0=ALU.mult),
                      reads=[bC, srcbuf], writes=[tmpbuf])
                kb.op('pe', lambda e, dc=dc: e.matmul(ps4[dc // 4][:, (dc % 4) * 128:(dc % 4 + 1) * 128], lhsT=onesf[:], rhs=tmpd[:], start=True, stop=True),
                      reads=[bC, tmpbuf], writes=[psbuf])
            for b4 in range(4):
                kb.op('act', lambda e, b4=b4: e.copy(out=dst[:, b4 * 512:(b4 + 1) * 512], in_=ps4[b4][:]), reads=[psbuf], writes=[dstbuf])

        with ExitStack() as P1:
            condT = alloc(P1, "condT", [128, 16], F32)
            badaT = alloc(P1, "badaT", [128, 96], F32)
            gnT = alloc(P1, "gnT", [128, 32], F32)
            wst = [alloc(P1, "wst%d" % i, [128, 16, 512], F32) for i in range(3)]
            psmod = palloc(P1, "psmod", [128, 512], F32)
            bcond = Buf(); bw = [Buf(), Buf(), Buf()]; bps = PSB()
            sw = [kb.dsem('wada0'), kb.dsem('wada1')]
            load(condT[:], condT_d, bcond, sC)
            load(badaT[:], b_adaT, bcond, sC)
            load(gnT[:, 0:16], gn1T_d, bcond, sC)
            load(gnT[:, 16:32], gn2T_d, bcond, sC)
            kb.op('act', lambda e: e.activation(out=condT[:], in_=condT[:], func=AF.Silu), reads=[bcond], writes=[bcond])
            wsb = [alloc(P1, "wsb%d" % i, [128, 16, 512], BF16) for i in range(2)]
            condb = alloc(P1, "condb", [128, 16], BF16)
            bwsb = [Buf(), Buf()]
            kb.op('dve', lambda e: e.tensor_copy(out=condb[:], in_=condT[:]), reads=[bcond], writes=[bcond])
            wv = w_ada.rearrange("(kc p) f -> p kc f", p=128)
            for blk in range(24):
                i3 = blk % 3
                i = blk % 2
                load(wst[i3][:], wv[:, :, blk * 512:(blk + 1) * 512], bw[i3], None, q=ldq())
                kb.op('dve' if i == 0 else 'pool', lambda e, i=i, i3=i3: e.tensor_copy(out=wsb[i][:], in_=wst[i3][:]), reads=[bw[i3]], writes=[bwsb[i]])
                for j in range(4):
                    col = blk * 4 + j
                    for kc in range(16):
                        kb.op('pe', lambda e, i=i, j=j, kc=kc, col=col: e.matmul(
                            psmod[:, col:col + 1], lhsT=wsb[i][:, kc, j * 128:(j + 1) * 128], rhs=condb[:, kc:kc + 1],
                            start=(kc == 0), stop=(kc == 15)), reads=[bwsb[i], bcond], writes=[bps])
            kb.op('dve', lambda e: e.tensor_tensor(out=modT[:], in0=psmod[:, 0:96], in1=badaT[:], op=ALU.add), reads=[bps, bcond], writes=[bMod])
            kb.op('dve', lambda e: e.scalar_tensor_tensor(out=gs1T[:], in0=modT[:, 16:32], scalar=1.0, in1=gnT[:, 0:16], op0=ALU.add, op1=ALU.mult),
                  reads=[bMod, bcond], writes=[bMod])
            kb.op('dve', lambda e: e.scalar_tensor_tensor(out=gs2T[:], in0=modT[:, 64:80], scalar=1.0, in1=gnT[:, 16:32], op0=ALU.add, op1=ALU.mult),
                  reads=[bMod, bcond], writes=[bMod])
            if dbg:
                sdb = kb.dsem('dbg')
                bdb = Buf()
                store(dbgmod[:, 0:96], modT[:], bMod, bdb, sdb)
                store(dbgmod[:, 96:112], gs1T[:], bMod, bdb, sdb)
                store(dbgmod[:, 112:128], gs2T[:], bMod, bdb, sdb)
            kb.barrier()
        if stop <= 1:
            kb.enabled = False
        sh1T = modT[:, 0:16]
        gt1T = modT[:, 32:48]
        sh2T = modT[:, 48:64]
        gt2T = modT[:, 80:96]

        with ExitStack() as P2:
            ropetab = alloc(P2, "ropetab", [128, 2, 4096], F32)
            sinT = ropetab[:, 0, :]
            cosT = ropetab[:, 1, :]
            bTab = Buf(); bhnT = Buf()
            with ExitStack() as P2r:
                posi = alloc(P2r, "posi", [128, 4096], I32)
                y = alloc(P2r, "ry", [128, 4096], F32)
                y2 = alloc(P2r, "ry2", [128, 2, 4096], F32)
                yi = alloc(P2r, "ryi", [128, 2, 4096], I32)
                m = alloc(P2r, "rm", [128, 2, 4096], F32)
                inv2 = alloc(P2r, "inv2", [128, 1], F32)
                sgn = alloc(P2r, "sgn", [128, 1], F32)
                bR = Buf()
                load(posi[:], pos_d.to_broadcast([128, 4096]), bR, sC)
                load(inv2[:], inv2_d, bR, sC)
                load(sgn[:], sgn_d, bR, sC)
                kb.op('dve', lambda e: e.tensor_copy(out=y[:], in_=posi[:]), reads=[bR], writes=[bR])
                kb.op('dve', lambda e: e.tensor_scalar(out=y2[:, 0, :], in0=y[:], scalar1=inv2[:, 0:1], scalar2=None, op0=ALU.mult), reads=[bR], writes=[bR])
                kb.op('dve', lambda e: e.tensor_scalar(out=y2[:, 1, :], in0=y[:], scalar1=inv2[:, 0:1], scalar2=0.25, op0=ALU.mult, op1=ALU.add), reads=[bR], writes=[bR])
                kb.op('dve', lambda e: e.tensor_copy(out=yi[:], in_=y2[:]), reads=[bR], writes=[bR])
                kb.op('dve', lambda e: e.tensor_copy(out=m[:], in_=yi[:]), reads=[bR], writes=[bR])
                kb.op('dve', lambda e: e.tensor_tensor(out=y2[:], in0=y2[:], in1=m[:], op=ALU.subtract), reads=[bR], writes=[bR])
                kb.op('dve', lambda e: e.tensor_scalar(out=m[:], in0=y2[:], scalar1=0.5, scalar2=None, op0=ALU.is_gt), reads=[bR], writes=[bR])
                kb.op('dve', lambda e: e.tensor_tensor(out=y2[:], in0=y2[:], in1=m[:], op=ALU.subtract), reads=[bR], writes=[bR])
                kb.op('dve', lambda e: e.tensor_scalar(out=m[:], in0=y2[:], scalar1=-0.5, scalar2=None, op0=ALU.is_lt), reads=[bR], writes=[bR])
                kb.op('dve', lambda e: e.tensor_tensor(out=y2[:], in0=y2[:], in1=m[:], op=ALU.add), reads=[bR], writes=[bR])
                kb.op('act', lambda e: e.activation(out=ropetab[:], in_=y2[:], func=AF.Sin, scale=2.0 * np.pi * (1.0 - 1e-6)),
                      reads=[bR], writes=[bTab])
                kb.op('dve', lambda e: e.tensor_scalar(out=ropetab[:, 0, :], in0=ropetab[:, 0, :], scalar1=sgn[:, 0:1], scalar2=None, op0=ALU.mult), reads=[bTab, bR], writes=[bTab])
                kb.barrier()
            if stop <= 1.5:
                kb.enabled = False
            hnT = alloc(P2, "hnT", [128, 16, 2048], BF16)

            xv = xall.rearrange("(n p) d -> n p d", p=128)
            Vsv = Vs.rearrange("(n p) c -> n p c", p=128)
            UVv = UVs.rearrange("(n p) c -> n p c", p=128)
            bKTs = Buf(); bQTs = Buf(); bVs = Buf(); bUVs = Buf()
            wiv = w_in.rearrange("(kc p) f -> p kc f", p=128)
            def do_half(half):
                with ExitStack() as PA:
                    xst = [alloc(PA, "xst%d" % i, [128, 2048], F32) for i in range(2)]
                    xnb = [alloc(PA, "xnb%d" % i, [128, 2048], BF16) for i in range(2)]
                    junk = alloc(PA, "junkA", [128, 2048], BF16)
                    ss = [alloc(PA, "ssA%d" % i, [128, 2], F32) for i in range(2)]
                    pT = [palloc(PA, "pTA%d" % i, [128, 2048], BF16) for i in range(2)]
                    bx = [Buf(), Buf()]; bxn = [Buf(), Buf()]; bj = Buf(); bss = [Buf(), Buf()]; brs = [Buf(), Buf()]; bpT = [PSB(), PSB()]
                    sx = [kb.dsem('xA0_%d' % half), kb.dsem('xA1_%d' % half)]
                    for tt in range(16):
                        i = tt % 2
                        gt = half * 16 + tt
                        load(xst[i][:], xv[gt], bx[i], sx[i], q=ldq())
                        kb.op('act', lambda e, i=i: e.activation(out=junk[:], in_=xst[i][:], func=AF.Square, accum_out=ss[i][:, 0:1]),
                              reads=[bx[i]], writes=[bj, bss[i]])
                        rstd_from_ss(ss[i][:, 0:1], ss[i][:, 1:2], bss[i], brs[i], float(D))
                        kb.op('dve', lambda e, i=i: e.tensor_scalar(out=xnb[i][:], in0=xst[i][:], scalar1=ss[i][:, 1:2], scalar2=None, op0=ALU.mult),
                              reads=[bx[i], brs[i]], writes=[bxn[i]])
                        import os
                        KS = os.environ.get('KSKIP', '')
                        for dc in (range(16) if KS != 'a1' else ()):
                            kb.op('pe', lambda e, i=i, dc=dc: e.transpose(out=pT[i][:, dc * 128:(dc + 1) * 128], in_=xnb[i][:, dc * 128:(dc + 1) * 128], identity=identb[:]),
                                  reads=[bxn[i], bC], writes=[bpT[i]])
                        for dc in (range(16) if KS not in ('a1', 'a2') else ()):
                            if True:
                                kb.op('dve', lambda e, i=i, dc=dc, tt=tt: e.tensor_scalar(
                                    out=hnT[:, dc, tt * 128:(tt + 1) * 128], in0=pT[i][:, dc * 128:(dc + 1) * 128],
                                    scalar1=gs1T[:, dc:dc + 1], scalar2=sh1T[:, dc:dc + 1], op0=ALU.mult, op1=ALU.add),
                                    reads=[bpT[i], bMod], writes=[bhnT])
                            else:
                                kb.op('act', lambda e, i=i, dc=dc, tt=tt: e.activation(
                                    out=hnT[:, dc, tt * 128:(tt + 1) * 128], in_=pT[i][:, dc * 128:(dc + 1) * 128],
                                    func=AF.Identity, scale=(1.0 if KS == 'a4' else gs1T[:, dc:dc + 1]), bias=(0.0 if KS in ('a4', 'a5') else sh1T[:, dc:dc + 1])),
                                    reads=[bpT[i], bMod], writes=[bhnT])
                    kb.barrier()
                if stop <= 1.6 + 0.2 * half:
                    kb.enabled = False
                with ExitStack() as PB:
                    wf = [alloc(PB, "wf%d" % i, [128, 16, 256], F32) for i in range(2)]
                    wb = [alloc(PB, "wb%d" % i, [128, 16, 512], BF16) for i in range(2)]
                    qb = [alloc(PB, "qb%d" % i, [128, 512], BF16) for i in range(2)]
                    t1 = [alloc(PB, "t1%d" % i, [128, 512], F32) for i in range(2)]
                    t2 = [alloc(PB, "t2%d" % i, [128, 512], F32) for i in range(2)]
                    ro = [alloc(PB, "ro%d" % i, [128, 512], BF16) for i in range(2)]
                    vst = [alloc(PB, "vst%d" % i, [128, 4, 129], BF16) for i in range(2)]
                    uvst = [alloc(PB, "uvst%d" % i, [128, 512], F32) for i in range(2)]
                    pq = [palloc(PB, "pq%d" % i, [128, 512], F32) for i in range(2)]
                    psw = [palloc(PB, "psw%d" % i, [128, 512], F32) for i in range(2)]
                    pv = [palloc(PB, "pv%d" % i, [128, 512], F32) for i in range(2)]
                    bwf = [Buf(), Buf()]; bwb = [Buf(), Buf()]; bqb = [Buf(), Buf()]; bt1 = [Buf(), Buf()]; bt2 = [Buf(), Buf()]
                    bro = [Buf(), Buf()]; bvst = [Buf(), Buf()]; buv = [Buf(), Buf()]; bpq = [PSB(), PSB()]; bpsw = [PSB(), PSB()]; bpv = [PSB(), PSB()]
                    swf = [kb.dsem('wf0_%d' % half), kb.dsem('wf1_%d' % half)]
                    sro = [kb.dsem('ro0_%d' % half), kb.dsem('ro1_%d' % half)]
                    svs = [kb.dsem('vs0_%d' % half), kb.dsem('vs1_%d' % half)]
                    suv = [kb.dsem('uv0_%d' % half), kb.dsem('uv1_%d' % half)]
                    fl = flag if half == 0 else onec
                    for i in range(2):
                        kb.op('dve', lambda e, i=i: e.tensor_copy(out=vst[i][:, :, 128:129], in_=fl[:, 0:1].unsqueeze(1).to_broadcast([128, 4, 1])),
                              reads=[bC], writes=[bvst[i]])
                    blocks = [3, 4, 5, 6, 7, 8] if half == 0 else list(range(11))
                    cnt_fm = 0
                    cnt_tm = 0
                    for bi, blk in enumerate(blocks):
                        i = bi % 2
                        for hc in range(2):
                            load(wf[hc][:], wiv[:, :, blk * 512 + hc * 256:blk * 512 + (hc + 1) * 256], bwf[hc], swf[hc], q=ldq())
                            kb.op('pool', lambda e, i=i, hc=hc: e.tensor_copy(out=wb[i][:, :, hc * 256:(hc + 1) * 256], in_=wf[hc][:]),
                                  reads=[bwf[hc]], writes=[bwb[i]])
                        import os
                        KS = os.environ.get('KSKIP', '')
                        if KS == 'b1' or (KS == 'b2' and blk >= 6) or (KS == 'b3' and blk < 6):
                            continue
                        if blk < 6:
                            isq = blk < 3
                            for hh in range(4):
                                head = (blk % 3) * 4 + hh
                                for tg in range(4):
                                    c = cnt_fm % 2
                                    cnt_fm += 1
                                    for kc in range(16):
                                        kb.op('pe', lambda e, i=i, c=c, kc=kc, hh=hh, tg=tg: e.matmul(
                                            pq[c][:], lhsT=wb[i][:, kc, hh * 128:(hh + 1) * 128], rhs=hnT[:, kc, tg * 512:(tg + 1) * 512],
                                            start=(kc == 0), stop=(kc == 15)), reads=[bwb[i], bhnT], writes=[bpq[c]])
                                    tok0 = half * 2048 + tg * 512
                                    kb.op('act', lambda e, c=c: e.copy(out=qb[c][:], in_=pq[c][:]), reads=[bpq[c]], writes=[bqb[c]])
                                    kb.op('dve', lambda e, c=c, tok0=tok0: e.tensor_tensor(out=t1[c][:], in0=pq[c][:], in1=cosT[:, tok0:tok0 + 512], op=ALU.mult),
                                          reads=[bpq[c], bTab], writes=[bt1[c]])
                                    kb.op('pe', lambda e, c=c: e.matmul(psw[c][:], lhsT=perm[:], rhs=qb[c][:], start=True, stop=True),
                                          reads=[bC, bqb[c]], writes=[bpsw[c]])
                                    kb.op('dve', lambda e, c=c, tok0=tok0: e.tensor_tensor(out=t2[c][:], in0=psw[c][:], in1=sinT[:, tok0:tok0 + 512], op=ALU.mult),
                                          reads=[bpsw[c], bTab], writes=[bt2[c]])
                                    kb.op('pool', lambda e, c=c: e.tensor_tensor(out=ro[c][:], in0=t1[c][:], in1=t2[c][:], op=ALU.add),
                                          reads=[bt1[c], bt2[c]], writes=[bro[c]])
                                    if isq:
                                        store(QTs[head, :, tg * 512:(tg + 1) * 512], ro[c][:], bro[c], bQTs, sro[c], q='sp')
                                    else:
                                        store(KTs[head, :, tok0:tok0 + 512], ro[c][:], bro[c], bKTs, sro[c], q='sp')
                        else:
                            for tt in range(16):
                                c = cnt_tm % 2
                                cnt_tm += 1
                                for kc in range(16):
                                    kb.op('pe', lambda e, i=i, c=c, kc=kc, tt=tt: e.matmul(
                                        pv[c][:], lhsT=hnT[:, kc, tt * 128:(tt + 1) * 128], rhs=wb[i][:, kc, :],
                                        start=(kc == 0), stop=(kc == 15)), reads=[bwb[i], bhnT], writes=[bpv[c]])
                                gt = half * 16 + tt
                                if blk < 9:
                                    hb = blk - 6
                                    kb.op('act', lambda e, c=c: e.activation(out=vst[c][:, :, 0:128], in_=pv[c][:].rearrange("p (h e) -> p h e", h=4),
                                                                            func=AF.Identity, scale=fl[:, 0:1]),
                                          reads=[bpv[c], bC], writes=[bvst[c]])
                                    store(Vsv[gt][:, hb * 516:(hb + 1) * 516], vst[c][:].rearrange("p h e -> p (h e)"), bvst[c], bVs, svs[c], q='act')
                                else:
                                    kb.op('act', lambda e, c=c: e.activation(out=uvst[c][:], in_=pv[c][:], func=AF.Gelu), reads=[bpv[c]], writes=[buv[c]])
                                    store(UVv[tt][:, (blk - 9) * 512:(blk - 8) * 512], uvst[c][:], buv[c], bUVs, suv[c], q='act')
                    kb.barrier()

            do_half(0)
            do_half(1)
        if stop <= 2:
            kb.enabled = False

        bMTs = Buf()
        with ExitStack() as P3:
            KTc = [alloc(P3, "KTc%d" % i, [128, 4, NW * 128], BF16) for i in range(2)]
            Vc = [alloc(P3, "Vc%d" % i, [128, NW, 4 * 129], BF16) for i in range(2)]
            QTg = alloc(P3, "QTg", [128, 12, QG * 128], BF16)
            mixed = alloc(P3, "mixed", [128, QG, 2048], F32)
            maskW = alloc(P3, "maskW", [128, 17 * 128], BF16)
            Eb = [alloc(P3, "Eb%d" % i, [128, 512], BF16) for i in range(3)]
            Pb = [alloc(P3, "Pb%d" % i, [128, 512], BF16) for i in range(3)]
            rec = alloc(P3, "rec", [128, QG], F32)
            uvt = alloc(P3, "uvt", [128, 1024], F32)
            lng = alloc(P3, "lng", [128, 512], F32)
            lnb = alloc(P3, "lnb", [128, 512], F32)
            wsTf = alloc(P3, "wsTf", [128, 4, 128], F32)
            wsT = alloc(P3, "wsT", [128, 4, 128], BF16)
            tril = alloc(P3, "tril", [128, 128], F32)
            sbT = alloc(P3, "sbT", [128, 4], F32)
            bst = alloc(P3, "bst", [128, 4, 6], F32)
            mv = alloc(P3, "mv", [128, 4, 2], F32)
            lrs = alloc(P3, "lrs", [128, 4], F32)
            vn = alloc(P3, "vn", [128, 512], F32)
            vnb = alloc(P3, "vnb", [128, 512], BF16)
            junk3 = alloc(P3, "junk3", [128, 1536], BF16)
            ss3 = alloc(P3, "ss3", [128, 4], F32)
            mnb = alloc(P3, "mnb", [128, 2048], BF16)
            mnT = alloc(P3, "mnT", [128, 2048], BF16)
            pS = [palloc(P3, "pS%d" % i, [128, 512], F32) for i in range(3)]
            pO = [palloc(P3, "pO%d" % i, [128, 512], F32) for i in range(QG)]
            pM = palloc(P3, "pM", [128, 512], F32)
            pT3 = palloc(P3, "pT3", [128, 2048], BF16)
            b3c = Buf(); bKTc = [Buf(), Buf()]; bVc = [Buf(), Buf()]; bQTg = Buf(); bmixed = Buf()
            bEb = [Buf(), Buf(), Buf()]; bPb = [Buf(), Buf(), Buf()]; brec = Buf(); buvt = Buf()
            bpS = [PSB(), PSB(), PSB()]; bpO = [PSB() for _ in range(QG)]; bpM = PSB(); bpT3 = PSB()
            bvn = Buf(); bvnb = Buf(); bstat = Buf(); bj3 = Buf(); bss3 = Buf(); brs3 = Buf(); bmnb = Buf(); bmnT = Buf()
            s3c = kb.dsem('c3'); sK = kb.dsem('ktg'); sV = kb.dsem('vg'); sQ = kb.dsem('qtg'); sUV = kb.dsem('uvt'); sMT = kb.dsem('mnT')
            load(maskW[:], maskW_d, b3c, s3c)
            load(lng[:], lng_d.to_broadcast([128, 512]), b3c, s3c)
            load(lnb[:], lnb_d.to_broadcast([128, 512]), b3c, s3c)
            load(wsTf[:], sgu_wT_d, b3c, s3c)
            load(tril[:], tril_d, b3c, s3c)
            load(sbT[:], sgu_bT_d, b3c, s3c)
            for hh in range(4):
                kb.op('dve', lambda e, hh=hh: e.tensor_tensor(out=wsT[:, hh, :], in0=wsTf[:, hh, :], in1=tril[:], op=ALU.mult), reads=[b3c], writes=[b3c])
            KTv = KTs.rearrange("h e t -> e h t")
            QTv = QTs.rearrange("h e t -> e h t")
            Vwv = Vs.rearrange("(n p) c -> p n c", p=128)
            UVv = UVs.rearrange("(n p) c -> n p c", p=128)

            uvt2 = [uvt, alloc(P3, "uvtB", [128, 1024], F32)]
            vn2 = [vn, alloc(P3, "vnB", [128, 512], F32)]
            vnb2 = [vnb, alloc(P3, "vnbB", [128, 512], BF16)]
            mnb2 = [mnb, alloc(P3, "mnbB", [128, 2048], BF16)]
            ss32 = [ss3, alloc(P3, "ss3B", [128, 4], F32)]
            bst2 = [bst, alloc(P3, "bstB", [128, 4, 6], F32)]
            mv2 = [mv, alloc(P3, "mvB", [128, 4, 2], F32)]
            lrs2 = [lrs, alloc(P3, "lrsB", [128, 4], F32)]
            buvt2 = [buvt, Buf()]; bvn2 = [bvn, Buf()]; bvnb2 = [bvnb, Buf()]; bmnb2 = [bmnb, Buf()]; bss32 = [bss3, Buf()]; brs32 = [brs3, Buf()]; bstat2 = [bstat, Buf()]

            def tail_part(g, part):
                for j in range(QG):
                    tail_tile(g, part, j)

            def tail_tile(g, part, j):
                mx = mixed2[g % 2]
                bmx = bmixed2[g % 2]
                if True:
                    ot = g * QG + j
                    uvt_, vn_, vnb_, mnb_, ss_, bst_, mv_, lrs_ = uvt2[j], vn2[j], vnb2[j], mnb2[j], ss32[j], bst2[j], mv2[j], lrs2[j]
                    buvt_, bvn_, bvnb_, bmnb_, bss_, brs_, bstat_ = buvt2[j], bvn2[j], bvnb2[j], bmnb2[j], bss32[j], brs32[j], bstat2[j]
                    if part == 1:
                        load(uvt_[:], UVv[ot], buvt_, None, q='sp')
                        for hh in range(4):
                            kb.op('dve', lambda e, hh=hh: e.bn_stats(out=bst_[:, hh, :], in_=uvt_[:, 512 + hh * 128:512 + (hh + 1) * 128]), reads=[buvt_], writes=[bstat_])
                        for hh in range(4):
                            kb.op('dve', lambda e, hh=hh: e.bn_aggr(out=mv_[:, hh, :], in_=bst_[:, hh, :]), reads=[bstat_], writes=[bstat_])
                        kb.op('act', lambda e: e.activation(out=lrs_[:], in_=mv_[:, :, 1], func=AF.Sqrt, bias=epsl[:, 0:1], scale=1.0), reads=[bstat_, bC], writes=[bstat_])
                        kb.op('dve', lambda e: e.reciprocal(out=lrs_[:], in_=lrs_[:]), reads=[bstat_], writes=[bstat_])
                        for hh in range(4):
                            kb.op('dve', lambda e, hh=hh: e.tensor_scalar(out=vn_[:, hh * 128:(hh + 1) * 128], in0=uvt_[:, 512 + hh * 128:512 + (hh + 1) * 128],
                                                                        scalar1=mv_[:, hh, 0:1], scalar2=lrs_[:, hh:hh + 1], op0=ALU.subtract, op1=ALU.mult),
                                  reads=[buvt_, bstat_], writes=[bvn_])
                        kb.op('pool', lambda e: e.tensor_tensor(out=vn_[:], in0=vn_[:], in1=lng[:], op=ALU.mult), reads=[bvn_, b3c], writes=[bvn_])
                        kb.op('pool', lambda e: e.tensor_tensor(out=vnb_[:], in0=vn_[:], in1=lnb[:], op=ALU.add), reads=[bvn_, b3c], writes=[bvnb_])
                    elif part == 2:
                        for hh in range(4):
                            kb.op('pe', lambda e, hh=hh: e.matmul(pM[:, hh * 128:(hh + 1) * 128], lhsT=wsT[:, hh, :], rhs=vnb_[:, hh * 128:(hh + 1) * 128],
                                                                 start=True, stop=True), reads=[b3c, bvnb_], writes=[bpM])
                        for hh in range(4):
                            kb.op('dve', lambda e, hh=hh, j=j: e.scalar_tensor_tensor(
                                out=mx[:, j, 1536 + hh * 128:1536 + (hh + 1) * 128], in0=pM[:, hh * 128:(hh + 1) * 128], scalar=sbT[:, hh:hh + 1],
                                in1=uvt_[:, hh * 128:(hh + 1) * 128], op0=ALU.add, op1=ALU.mult), reads=[bpM, b3c, buvt_], writes=[bmx])
                        kb.op('act', lambda e, j=j: e.activation(out=junk3[:, 0:1536], in_=mx[:, j, 0:1536], func=AF.Square, accum_out=ss_[:, 0:1]),
                              reads=[bmx], writes=[bj3, bss_])
                        kb.op('act', lambda e, j=j: e.activation(out=junk3[:, 0:512], in_=mx[:, j, 1536:2048], func=AF.Square, accum_out=ss_[:, 1:2]),
                              reads=[bmx], writes=[bj3, bss_])
                        rstd_from_ss(ss_[:, 0:1], ss_[:, 2:3], bss_, brs_, 1536.0)
                        rstd_from_ss(ss_[:, 1:2], ss_[:, 3:4], bss_, brs_, 512.0)
                        kb.op('dve', lambda e, j=j: e.tensor_scalar(out=mnb_[:, 0:1536], in0=mx[:, j, 0:1536], scalar1=ss_[:, 2:3], scalar2=None, op0=ALU.mult),
                              reads=[bmx, brs_], writes=[bmnb_])
                        kb.op('pool', lambda e, j=j: e.tensor_scalar(out=mnb_[:, 1536:2048], in0=mx[:, j, 1536:2048], scalar1=ss_[:, 3:4], scalar2=None, op0=ALU.mult),
                              reads=[bmx, brs_], writes=[bmnb_])
                    else:
                        for dc in range(16):
                            kb.op('pe', lambda e, dc=dc: e.transpose(out=pT3[:, dc * 128:(dc + 1) * 128], in_=mnb_[:, dc * 128:(dc + 1) * 128], identity=identb[:]),
                                  reads=[bmnb_, bC], writes=[bpT3])
                        kb.op('act', lambda e: e.copy(out=mnT[:, 0:1024], in_=pT3[:, 0:1024]), reads=[bpT3], writes=[bmnT])
                        kb.op('dve', lambda e: e.tensor_copy(out=mnT[:, 1024:2048], in_=pT3[:, 1024:2048]), reads=[bpT3], writes=[bmnT])
                        store(MTs[ot], mnT[:], bmnT, bMTs, None, q='sp')

            mixed2 = [mixed, alloc(P3, "mixedB", [128, QG, 2048], F32)]
            bmixed2 = [bmixed, Buf()]
            sc_exp = float(128 ** -0.5)
            for g in range(16 // QG):
                ws = g * QG
                def load_chunk(ci):
                    if ci >= 24:
                        return
                    g_, hc_ = divmod(ci, 3)
                    ws_ = g_ * QG
                    bb = ci % 2
                    load(KTc[bb][:], KTv[:, hc_ * 4:(hc_ + 1) * 4, ws_ * 128:(ws_ + NW) * 128], bKTc[bb], None, q='sp')
                    load(Vc[bb][:], Vwv[:, ws_:ws_ + NW, hc_ * 516:(hc_ + 1) * 516], bVc[bb], None, q='act')
                if g == 0:
                    load_chunk(0)
                load(QTg[:], QTv[:, :, ws * 128:(ws + QG) * 128], bQTg, sQ, q='sp', reads=[bQTs])
                for h in range(12):
                    ci = g * 3 + h // 4
                    cb = ci % 2
                    hl = h % 4
                    if h % 4 == 0:
                        load_chunk(ci + 1)
                    def kinfo(kt):
                        jlo = max(0, kt - 16); jhi = min(QG - 1, kt)
                        return jlo, jhi, (jhi - jlo + 1) * 128

                    def qkpair(kp, h=h, cb=cb, hl=hl):
                        c = kp % 3
                        for s2 in range(2):
                            kt = kp * 2 + s2
                            jlo, jhi, n = kinfo(kt)
                            kb.op('pe', lambda e, kt=kt, jlo=jlo, jhi=jhi, n=n, s2=s2, c=c: e.matmul(
                                pS[c][:, s2 * 256:s2 * 256 + n], lhsT=KTc[cb][:, hl, kt * 128:(kt + 1) * 128], rhs=QTg[:, h, jlo * 128:(jhi + 1) * 128],
                                start=True, stop=True), reads=[bKTc[cb], bQTg], writes=[bpS[c]])
                    qkpair(0)
                    qkpair(1)
                    for kp in range(NW // 2):
                        if kp + 2 < NW // 2:
                            qkpair(kp + 2)
                        c = kp % 3
                        n0 = kinfo(kp * 2)[2]; n1 = kinfo(kp * 2 + 1)[2]
                        if n0 == 256 and n1 == 256:
                            kb.op('act', lambda e, c=c: e.activation(out=Eb[c][:], in_=pS[c][:], func=AF.Exp, scale=sc_exp),
                                  reads=[bpS[c]], writes=[bEb[c]])
                        else:
                            for s2, nn in ((0, n0), (1, n1)):
                                kb.op('act', lambda e, c=c, s2=s2, nn=nn: e.activation(out=Eb[c][:, s2 * 256:s2 * 256 + nn], in_=pS[c][:, s2 * 256:s2 * 256 + nn],
                                                                                     func=AF.Exp, scale=sc_exp), reads=[bpS[c]], writes=[bEb[c]])
                        for s2 in range(2):
                            kt = kp * 2 + s2
                            jlo, jhi, n = kinfo(kt)
                            m0 = (16 + jlo - kt) * 128
                            kb.op('dve', lambda e, c=c, n=n, m0=m0, s2=s2: e.tensor_tensor(out=Pb[c][:, s2 * 256:s2 * 256 + n], in0=Eb[c][:, s2 * 256:s2 * 256 + n],
                                                                                         in1=maskW[:, m0:m0 + n], op=ALU.mult),
                                  reads=[bEb[c], b3c], writes=[bPb[c]])
                        for s2 in range(2):
                            kt = kp * 2 + s2
                            jlo, jhi, n = kinfo(kt)
                            for j in range(jlo, jhi + 1):
                                kb.op('pe', lambda e, c=c, j=j, jlo=jlo, kt=kt, h=h, s2=s2, cb=cb, hl=hl: e.matmul(
                                    pO[j][:, 0:129], lhsT=Pb[c][:, s2 * 256 + (j - jlo) * 128:s2 * 256 + (j - jlo + 1) * 128], rhs=Vc[cb][:, kt, hl * 129:(hl + 1) * 129],
                                    start=(kt == j), stop=(kt == 16 + j)), reads=[bPb[c], bVc[cb]], writes=[bpO[j]])
                    for j in range(QG):
                        kb.op('dve', lambda e, j=j: e.reciprocal(out=rec[:, j:j + 1], in_=pO[j][:, 128:129]), reads=[bpO[j]], writes=[brec])
                        kb.op('dve', lambda e, j=j, h=h, mxh=mixed2[g % 2]: e.tensor_scalar(out=mxh[:, j, h * 128:(h + 1) * 128], in0=pO[j][:, 0:128],
                                                                      scalar1=rec[:, j:j + 1], scalar2=None, op0=ALU.mult),
                              reads=[bpO[j], brec], writes=[bmixed2[g % 2]])
                    if g > 0 and h in (0, 1, 2):
                        tail_part(g - 1, h + 1)
                pass
            for part in (1, 2, 3):
                tail_part(16 // QG - 1, part)
            kb.barrier()
        if stop <= 3:
            kb.enabled = False

        bH1s = Buf(); bHN2s = Buf(); bHN2Ts = Buf()

        def load_weight_bf16(st, wdram, wdst, bwdst, scaleT, name):
            stg = [alloc(st, name + "stg%d" % i, [128, 2048], F32) for i in range(2)]
            bstg = [Buf(), Buf()]
            sst = [kb.dsem(name + 's0'), kb.dsem(name + 's1')]
            wv_ = wdram.rearrange("(kc p) f -> p kc f", p=128)
            for kc in range(16):
                i = kc % 2
                load(stg[i][:], wv_[:, kc, :], bstg[i], sst[i], q=ldq())
                if scaleT is None:
                    kb.op('pool', lambda e, i=i, kc=kc: e.tensor_copy(out=wdst[:, kc, :], in_=stg[i][:]), reads=[bstg[i]], writes=[bwdst])
                else:
                    kb.op('pool', lambda e, i=i, kc=kc: e.tensor_scalar(out=wdst[:, kc, :], in0=stg[i][:], scalar1=scaleT[:, kc:kc + 1],
                                                                       scalar2=None, op0=ALU.mult), reads=[bstg[i], bC], writes=[bwdst])

        with ExitStack() as P4:
            wo = alloc(P4, "wo", [128, 16, 2048], BF16)
            gmixT = alloc(P4, "gmixT", [128, 16], F32)
            gt1bc = alloc(P4, "gt1bc", [128, 2048], F32)
            gs2bc = alloc(P4, "gs2bc", [128, 2048], F32)
            sh2bc = alloc(P4, "sh2bc", [128, 2048], F32)
            dtmp = alloc(P4, "dtmp", [128, 128], F32)
            mnT4 = [alloc(P4, "mnT4%d" % i, [128, 16, 128], BF16) for i in range(2)]
            x4 = [alloc(P4, "x4%d" % i, [128, 2048], F32) for i in range(2)]
            tmp4 = alloc(P4, "tmp4", [128, 2048], F32)
            h14 = alloc(P4, "h14", [128, 2048], F32)
            junk4 = alloc(P4, "junk4", [128, 2048], BF16)
            ss4 = alloc(P4, "ss4", [128, 2], F32)
            hn2 = alloc(P4, "hn2", [128, 2048], BF16)
            hn2T = alloc(P4, "hn2T", [128, 2048], BF16)
            pW = [palloc(P4, "pW%d" % i, [128, 512], F32) for i in range(4)]
            pT4 = palloc(P4, "pT4", [128, 2048], BF16)
            bwo = Buf(); b4c = Buf(); bbc = Buf(); bdt = Buf(); bmn4 = [Buf(), Buf()]; bx4 = [Buf(), Buf()]; btmp4 = Buf(); bh14 = Buf()
            bj4 = Buf(); bss4 = Buf(); brs4 = Buf(); bhn2 = Buf(); bhn2T = Buf(); bpW = PSB(); bpT4 = PSB()
            s4c = kb.dsem('c4'); smn = [kb.dsem('mn40'), kb.dsem('mn41')]; sx4 = [kb.dsem('x40'), kb.dsem('x41')]
            sh1 = kb.dsem('h1st'); shn2 = kb.dsem('hn2st'); shn2T = kb.dsem('hn2Tst')
            load(gmixT[:], gmixT_d, bC, s4c)
            with ExitStack() as P4w:
                load_weight_bf16(P4w, w_out, wo, bwo, gmixT, "wo")
                make_bc(P4w, gt1bc, bbc, gt1T, bMod, pW, bpW, dtmp, bdt)
                make_bc(P4w, gs2bc, bbc, gs2T, bMod, pW, bpW, dtmp, bdt)
                make_bc(P4w, sh2bc, bbc, sh2T, bMod, pW, bpW, dtmp, bdt)
                kb.barrier()
            xov = xall.rearrange("(n p) d -> n p d", p=128)
            H1v = H1s.rearrange("(n p) d -> n p d", p=128)
            HN2v = HN2s.rearrange("(n p) d -> n p d", p=128)
            h14b = [h14, alloc(P4, "h14b", [128, 2048], F32)]
            ss4b = [ss4, alloc(P4, "ss4b", [128, 2], F32)]
            tmpB = alloc(P4, "tmpB4", [128, 2048], F32)
            bh14b = [bh14, Buf()]; bss4b = [bss4, Buf()]; brs4b = [brs4, Buf()]; btmpB = Buf()

            def stageA1(ot):
                i = ot % 2
                load(mnT4[i][:], MTs[ot].rearrange("p (kc t) -> p kc t", kc=16), bmn4[i], None, q='sp')
                load(x4[i][:], xov[16 + ot], bx4[i], None, q='act')
                for nb in range(4):
                    for kc in range(16):
                        kb.op('pe', lambda e, i=i, nb=nb, kc=kc: e.matmul(pW[nb][:], lhsT=mnT4[i][:, kc, :], rhs=wo[:, kc, nb * 512:(nb + 1) * 512],
                                                                         start=(kc == 0), stop=(kc == 15)), reads=[bmn4[i], bwo], writes=[bpW])

            def stageA2(ot):
                i = ot % 2
                for nb in range(4):
                    kb.op('dve', lambda e, nb=nb: e.tensor_tensor(out=tmp4[:, nb * 512:(nb + 1) * 512], in0=pW[nb][:], in1=gt1bc[:, nb * 512:(nb + 1) * 512], op=ALU.mult),
                          reads=[bpW, bbc], writes=[btmp4])
                kb.op('pool', lambda e, i=i: e.tensor_tensor(out=h14b[i][:], in0=tmp4[:], in1=x4[i][:], op=ALU.add), reads=[btmp4, bx4[i]], writes=[bh14b[i]])
                store(H1v[ot], h14b[i][:], bh14b[i], bH1s, None, q='sp')
                kb.op('act', lambda e, i=i: e.activation(out=junk4[:], in_=h14b[i][:], func=AF.Square, accum_out=ss4b[i][:, 0:1]), reads=[bh14b[i]], writes=[bj4, bss4b[i]])
                rstd_from_ss(ss4b[i][:, 0:1], ss4b[i][:, 1:2], bss4b[i], brs4b[i], float(D))

            def stageB1(ot):
                i = ot % 2
                kb.op('dve', lambda e, i=i: e.scalar_tensor_tensor(out=tmpB[:], in0=h14b[i][:], scalar=ss4b[i][:, 1:2], in1=gs2bc[:], op0=ALU.mult, op1=ALU.mult),
                      reads=[bh14b[i], brs4b[i], bbc], writes=[btmpB])
                kb.op('pool', lambda e: e.tensor_tensor(out=hn2[:], in0=tmpB[:], in1=sh2bc[:], op=ALU.add), reads=[btmpB, bbc], writes=[bhn2])
                store(HN2v[ot], hn2[:], bhn2, bHN2s, None, q='act')

            def stageB2(ot):
                for dc in range(16):
                    kb.op('pe', lambda e, dc=dc: e.transpose(out=pT4[:, dc * 128:(dc + 1) * 128], in_=hn2[:, dc * 128:(dc + 1) * 128], identity=identb[:]),
                          reads=[bhn2, bC], writes=[bpT4])
                kb.op('act', lambda e: e.copy(out=hn2T[:, 0:1024], in_=pT4[:, 0:1024]), reads=[bpT4], writes=[bhn2T])
                kb.op('dve', lambda e: e.tensor_copy(out=hn2T[:, 1024:2048], in_=pT4[:, 1024:2048]), reads=[bpT4], writes=[bhn2T])
                store(HN2Ts[ot], hn2T[:], bhn2T, bHN2Ts, None, q='sp')

            stageA1(0)
            stageA2(0)
            for ot in range(16):
                if ot + 1 < 16:
                    stageA1(ot + 1)
                stageB1(ot)
                if ot + 1 < 16:
                    stageA2(ot + 1)
                stageB2(ot)
            kb.barrier()
        if stop <= 4:
            kb.enabled = False

        with ExitStack() as P56:
            idxT = alloc(P56, "idxT", [128, 2048], I32)
            gT = alloc(P56, "gT", [128, 2048], F32)
            bidxT = Buf(); bgT = Buf()
            with ExitStack() as P5:
                wq = alloc(P5, "wq", [128, 16, 2048], BF16)
                skT = alloc(P5, "skT", [128, 16, 128], BF16)
                iota16 = alloc(P5, "iota16", [128, 16], F32)
                hg = alloc(P5, "hg", [128, 16, 512], BF16)
                qT = alloc(P5, "qT", [128, 16, 512], BF16)
                sc = alloc(P5, "sc", [128, 16, 128], F32)
                wk = alloc(P5, "wk", [128, 16, 128], F32)
                stv = alloc(P5, "stv", [128, 16, 16], F32)
                siv = alloc(P5, "siv", [128, 16, 16], U32)
                sif = alloc(P5, "sif", [128, 16, 16], F32)
                cand = alloc(P5, "cand", [128, 8, 256], F32)
                bs = alloc(P5, "bs", [128, 8, 16], F32)
                bp = alloc(P5, "bp", [128, 8, 16], U32)
                bpf = alloc(P5, "bpf", [128, 8, 16], F32)
                pi_ = alloc(P5, "pi_", [128, 8, 16], F32)
                pj_ = alloc(P5, "pj_", [128, 8, 16], F32)
                pii = alloc(P5, "pii", [128, 8, 16], I32)
                eq = alloc(P5, "eq", [128, 128, 16], F32)
                ea = alloc(P5, "ea", [128, 128], F32)
                ebb = alloc(P5, "ebb", [128, 128], F32)
                idxf = alloc(P5, "idxf", [128, 128], F32)
                gte = alloc(P5, "gte", [128, 8, 16], F32)
                gsum = alloc(P5, "gsum", [128, 8], F32)
                pQ = [palloc(P5, "pQ%d" % i, [128, 512], F32) for i in range(2)]
                pS5 = [palloc(P5, "pS5%d" % i, [128, 512], F32) for i in range(4)]
                pTr = palloc(P5, "pTr", [128, 512], F32)
                bwq = Buf(); b5c = Buf(); bhg = Buf(); bqT = Buf(); bsc = Buf(); bwk = Buf(); bst5 = Buf(); bcand = Buf(); bcwk = Buf()
                bbs = Buf(); bsel = Buf(); bidxf = Buf(); bgte = Buf(); bpQ = [PSB(), PSB()]; bpS5 = PSB(); bpTr = PSB()
                s5c = kb.dsem('c5'); shg = kb.dsem('hg')
                load(iota16[:], iota_d, b5c, s5c)
                with ExitStack() as P5w:
                    skTf = alloc(P5w, "skTf", [128, 16, 128], F32)
                    load(skTf[:], skT_d, b5c, s5c)
                    kb.op('dve', lambda e: e.tensor_copy(out=skT[:], in_=skTf[:]), reads=[b5c], writes=[b5c])
                    load_weight_bf16(P5w, w_q, wq, bwq, None, "wq")
                    kb.barrier()
                hg2 = [hg, alloc(P5, "hgB", [128, 16, 512], BF16)]
                bhg2 = [bhg, Buf()]

                def load_hg(gq):
                    if gq < 4:
                        for jj_ in range(4):
                            load(hg2[gq % 2][:, :, jj_ * 128:(jj_ + 1) * 128], HN2Ts[gq * 4 + jj_].rearrange("p (kc t) -> p kc t", kc=16), bhg2[gq % 2], None, q=ldq())
                load_hg(0)
                for g4 in range(4):
                    load_hg(g4 + 1)
                    hgc = hg2[g4 % 2]
                    bhgc = bhg2[g4 % 2]
                    for hp in range(16):
                        c = hp % 2
                        for kc in range(16):
                            kb.op('pe', lambda e, c=c, kc=kc, hp=hp, hgc=hgc: e.matmul(pQ[c][:], lhsT=wq[:, kc, hp * 128:(hp + 1) * 128], rhs=hgc[:, kc, :],
                                                                             start=(kc == 0), stop=(kc == 15)), reads=[bwq, bhgc], writes=[bpQ[c]])
                        kb.op('act', lambda e, c=c, hp=hp: e.copy(out=qT[:, hp, :], in_=pQ[c][:]), reads=[bpQ[c]], writes=[bqT])
                    emit_cast(8, after=[bqT])
                    for jj in range(4):
                        ot = g4 * 4 + jj
                        for hp in range(16):
                            kb.op('pe', lambda e, hp=hp, jj=jj: e.matmul(pS5[hp // 4][:, (hp % 4) * 128:(hp % 4 + 1) * 128], lhsT=qT[:, hp, jj * 128:(jj + 1) * 128],
                                                                        rhs=skT[:, hp, :], start=True, stop=True), reads=[bqT, b5c], writes=[bpS5])
                        for b4 in range(4):
                            kb.op('act', lambda e, b4=b4: e.copy(out=sc[:, b4 * 4:(b4 + 1) * 4, :], in_=pS5[b4][:].rearrange("p (a k) -> p a k", a=4)),
                                  reads=[bpS5], writes=[bsc])
                        for hp in range(16):
                            kb.op('dve', lambda e, hp=hp: e.max(out=stv[:, hp, 0:8], in_=sc[:, hp, :]), reads=[bsc], writes=[bst5])
                        for hp in range(16):
                            kb.op('dve', lambda e, hp=hp: e.max_index(out=siv[:, hp, 0:8], in_max=stv[:, hp, 0:8], in_values=sc[:, hp, :]), reads=[bsc, bst5], writes=[bst5])
                        for hp in range(16):
                            kb.op('dve', lambda e, hp=hp: e.match_replace(out=wk[:, hp, :], in_to_replace=stv[:, hp, 0:8], in_values=sc[:, hp, :], imm_value=-1e30),
                                  reads=[bsc, bst5], writes=[bwk])
                        for hp in range(16):
                            kb.op('dve', lambda e, hp=hp: e.max(out=stv[:, hp, 8:16], in_=wk[:, hp, :]), reads=[bwk], writes=[bst5])
                        for hp in range(16):
                            kb.op('dve', lambda e, hp=hp: e.max_index(out=siv[:, hp, 8:16], in_max=stv[:, hp, 8:16], in_values=wk[:, hp, :]), reads=[bwk, bst5], writes=[bst5])
                        kb.op('dve', lambda e: e.tensor_copy(out=sif[:], in_=siv[:]), reads=[bst5], writes=[bst5])
                        stv4 = stv[:].rearrange("t (h p) k -> t h p k", p=2)
                        sif4 = sif[:].rearrange("t (h p) k -> t h p k", p=2)
                        for h in range(8):
                            kb.op('dve', lambda e, h=h: e.tensor_tensor(
                                out=cand[:, h, :].rearrange("t (i j) -> t i j", i=16),
                                in0=stv4[:, h, 0, :].unsqueeze(2).to_broadcast([128, 16, 16]),
                                in1=stv4[:, h, 1, :].unsqueeze(1).to_broadcast([128, 16, 16]), op=ALU.add), reads=[bst5], writes=[bcand])
                        for h in range(8):
                            kb.op('dve', lambda e, h=h: e.max(out=bs[:, h, 0:8], in_=cand[:, h, :]), reads=[bcand], writes=[bbs])
                        for h in range(8):
                            kb.op('dve', lambda e, h=h: e.max_index(out=bp[:, h, 0:8], in_max=bs[:, h, 0:8], in_values=cand[:, h, :]), reads=[bcand, bbs], writes=[bbs])
                        for h in range(8):
                            kb.op('dve', lambda e, h=h: e.match_replace(out=wk[:].rearrange("t a k -> t (a k)")[:, h * 256:(h + 1) * 256], in_to_replace=bs[:, h, 0:8], in_values=cand[:, h, :], imm_value=-1e30),
                                  reads=[bcand, bbs], writes=[bwk])
                        for h in range(8):
                            kb.op('dve', lambda e, h=h: e.max(out=bs[:, h, 8:16], in_=wk[:].rearrange("t a k -> t (a k)")[:, h * 256:(h + 1) * 256]), reads=[bwk], writes=[bbs])
                        for h in range(8):
                            kb.op('dve', lambda e, h=h: e.max_index(out=bp[:, h, 8:16], in_max=bs[:, h, 8:16], in_values=wk[:].rearrange("t a k -> t (a k)")[:, h * 256:(h + 1) * 256]), reads=[bwk, bbs], writes=[bbs])
                        kb.op('dve', lambda e: e.tensor_copy(out=bpf[:], in_=bp[:]), reads=[bbs], writes=[bsel])
                        kb.op('dve', lambda e: e.tensor_scalar(out=pj_[:], in0=bpf[:], scalar1=1.0 / 16.0, scalar2=None, op0=ALU.mult), reads=[bsel], writes=[bsel])
                        kb.op('dve', lambda e: e.tensor_copy(out=pii[:], in_=pj_[:]), reads=[bsel], writes=[bsel])
                        kb.op('dve', lambda e: e.tensor_copy(out=pi_[:], in_=pii[:]), reads=[bsel], writes=[bsel])
                        kb.op('dve', lambda e: e.tensor_tensor(out=pj_[:], in0=pi_[:], in1=pj_[:], op=ALU.is_gt), reads=[bsel], writes=[bsel])
                        kb.op('dve', lambda e: e.tensor_tensor(out=pi_[:], in0=pi_[:], in1=pj_[:], op=ALU.subtract), reads=[bsel], writes=[bsel])
                        kb.op('dve', lambda e: e.scalar_tensor_tensor(out=pj_[:], in0=pi_[:], scalar=-16.0, in1=bpf[:], op0=ALU.mult, op1=ALU.add), reads=[bsel], writes=[bsel])
                        for which, pp, dst in ((0, pi_, ea), (1, pj_, ebb)):
                            kb.op('dve', lambda e, pp=pp: e.tensor_tensor(
                                out=eq[:], in0=pp[:].rearrange("t h k -> t (h k)").unsqueeze(2).to_broadcast([128, 128, 16]),
                                in1=iota16[:].unsqueeze(1).to_broadcast([128, 128, 16]), op=ALU.is_equal), reads=[bsel, b5c], writes=[bsel])
                            for h in range(8):
                                kb.op('dve', lambda e, h=h, which=which: e.tensor_tensor(
                                    out=eq[:, h * 16:(h + 1) * 16, :], in0=eq[:, h * 16:(h + 1) * 16, :],
                                    in1=sif4[:, h, which, :].unsqueeze(1).to_broadcast([128, 16, 16]), op=ALU.mult), reads=[bsel, bst5], writes=[bsel])
                            kb.op('dve', lambda e, dst=dst: e.tensor_reduce(out=dst[:], in_=eq[:], axis=AX.X, op=ALU.add), reads=[bsel], writes=[bsel])
                        kb.op('dve', lambda e: e.scalar_tensor_tensor(out=idxf[:], in0=ea[:], scalar=128.0, in1=ebb[:], op0=ALU.mult, op1=ALU.add), reads=[bsel], writes=[bidxf])
                        kb.op('dve', lambda e: e.tensor_tensor(out=gte[:], in0=bs[:], in1=bs[:, :, 0:1].to_broadcast([128, 8, 16]), op=ALU.subtract), reads=[bbs], writes=[bgte])
                        kb.op('act', lambda e: e.activation(out=gte[:], in_=gte[:], func=AF.Exp), reads=[bgte], writes=[bgte])
                        kb.op('dve', lambda e: e.tensor_reduce(out=gsum[:], in_=gte[:], axis=AX.X, op=ALU.add), reads=[bgte], writes=[bgte])
                        kb.op('dve', lambda e: e.reciprocal(out=gsum[:], in_=gsum[:]), reads=[bgte], writes=[bgte])
                        kb.op('dve', lambda e: e.tensor_tensor(out=gte[:], in0=gte[:], in1=gsum[:].unsqueeze(2).to_broadcast([128, 8, 16]), op=ALU.mult), reads=[bgte], writes=[bgte])
                        kb.op('pe', lambda e: e.transpose(out=pTr[:, 0:128], in_=idxf[:], identity=identf[:]), reads=[bidxf, bC], writes=[bpTr])
                        kb.op('pe', lambda e: e.transpose(out=pTr[:, 128:256], in_=gte[:].rearrange("t h k -> t (h k)"), identity=identf[:]), reads=[bgte, bC], writes=[bpTr])
                        kb.op('dve', lambda e, ot=ot: e.tensor_copy(out=idxT[:, ot * 128:(ot + 1) * 128], in_=pTr[:, 0:128]), reads=[bpTr], writes=[bidxT])
                        kb.op('act', lambda e, ot=ot: e.copy(out=gT[:, ot * 128:(ot + 1) * 128], in_=pTr[:, 128:256]), reads=[bpTr], writes=[bgT])
                if dbg:
                    sdb5 = kb.dsem('dbg5')
                    bdb5 = Buf()
                    store(dbgidx, idxT[:], bidxT, bdb5, sdb5)
                    store(dbgg, gT[:], bgT, bdb5, sdb5)
                kb.barrier()
            if stop <= 5:
                kb.enabled = False

            flush_cast()
            kb.barrier()
            with ExitStack() as P6:
                NB = 6
                gb = [alloc(P6, "gb%d" % i, [128, 4096], BF16) for i in range(NB)]
                hn2t = [alloc(P6, "hn2t%d" % i, [128, 2048], BF16) for i in range(2)]
                junk6 = alloc(P6, "junk6", [128, 1024], BF16)
                NA = 4
                acc = [alloc(P6, "acc%d" % i, [128, 8], F32) for i in range(NA)]
                win = [alloc(P6, "win%d" % i, [128, 256], BF16) for i in range(2)]
                gt2bc = alloc(P6, "gt2bc", [128, 2048], F32)
                gfbc = alloc(P6, "gfbc", [128, 2048], F32)
                dtmp6 = alloc(P6, "dtmp6", [128, 128], F32)
                h16 = alloc(P6, "h16", [128, 2048], F32)
                tmp6 = alloc(P6, "tmp6", [128, 2048], F32)
                hf6 = alloc(P6, "hf6", [128, 2048], F32)
                o6 = alloc(P6, "o6", [128, 2048], F32)
                ss6 = alloc(P6, "ss6", [128, 2], F32)
                pXh = [palloc(P6, "pXh%d" % i, [128, 1024], F32) for i in range(2)]
                pOut = [palloc(P6, "pOut%d" % i, [128, 512], F32) for i in range(4)]
                bgb = [Buf() for _ in range(NB)]; bhn2t = [Buf(), Buf()]; bj6 = Buf(); bacc = [Buf() for _ in range(NA)]
                bwin = [Buf(), Buf()]; bbc6 = Buf(); bdt6 = Buf(); bh16 = Buf(); btmp6 = Buf(); bhf6 = Buf(); bo6 = Buf(); bss6 = Buf(); brs6 = Buf()
                bpXh = [PSB(), PSB()]; bpOut = PSB(); bOut = Buf(); bpXall = PSB()
                sgb = [kb.dsem('gb%d' % i) for i in range(NB)]
                ps4 = [pXh[0][:, 0:512], pXh[0][:, 512:1024], pXh[1][:, 0:512], pXh[1][:, 512:1024]]
                make_bc(P6, gt2bc, bbc6, gt2T, bMod, ps4, bpXall, dtmp6, bdt6)
                load(gfbc[:], gfin_d.to_broadcast([128, 2048]), bbc6, None)
                for i in range(2):
                    kb.op('dve', lambda e, i=i: e.memset(win[i][:], 0.0), writes=[bwin[i]])
                kb.barrier()
                HN2v = HN2s.rearrange("(n p) d -> n p d", p=128)
                H1v = H1s.rearrange("(n p) d -> n p d", p=128)
                Ov = out_d.rearrange("(n p) d -> n p d", p=128)
                loaded = set()

                def ensure_tile(ot):
                    if ot not in loaded and ot < 16:
                        loaded.add(ot)
                        load(hn2t[ot % 2][:], HN2v[ot], bhn2t[ot % 2], None, q='sp')

                def emit_xb(t):
                    ot, tt = divmod(t, 128)
                    ensure_tile(ot)
                    hi = ot % 2
                    for hf in range(2):
                        for bb in range(2):
                            b4 = hf * 2 + bb
                            kb.op('pe', lambda e, hf=hf, bb=bb, b4=b4, tt=tt, hi=hi: e.matmul(
                                pXh[hf][:, bb * 512:(bb + 1) * 512], lhsT=identb[:, tt:tt + 1].to_broadcast([128, 128]),
                                rhs=hn2t[hi][:, b4 * 512:(b4 + 1) * 512], start=True, stop=True),
                                reads=[bC, bhn2t[hi]], writes=[bpXh[hf]])

                def emit_gather(t):
                    u = t % NB
                    kb.dma('pool', lambda e, u=u, t=t: e.indirect_dma_start(
                        out=gb[u][:], out_offset=None, in_=UV16, in_offset=bass.IndirectOffsetOnAxis(ap=idxT[:, t:t + 1], axis=0)),
                        sgb[u], reads=[bidxT], writes=[bgb[u]])

                emit_gather(0)
                emit_xb(0)
                for t in range(2048):
                    ot, tt = divmod(t, 128)
                    u = t % NB
                    a4 = t % NA
                    a2 = t % 2
                    if t + 1 < 2048:
                        emit_gather(t + 1)
                    for hf in range(2):
                        kb.op('dve', lambda e, hf=hf, u=u, a4=a4: e.scalar_tensor_tensor(
                            out=junk6[:], in0=gb[u][:, hf * 1024:(hf + 1) * 1024], scalar=1.0, in1=pXh[hf][:], op0=ALU.mult, op1=ALU.mult,
                            accum_out=acc[a4][:, hf:hf + 1]), reads=[bgb[u], bpXh[hf]], writes=[bj6, bacc[a4]])
                    kb.op('act', lambda e, a4=a4: e.activation(out=acc[a4][:, 2:4], in_=acc[a4][:, 0:2], func=AF.Identity, accum_out=acc[a4][:, 4:5]),
                          reads=[bacc[a4]], writes=[bacc[a4]])
                    kb.op('act', lambda e, a4=a4: e.activation(out=acc[a4][:, 5:6], in_=acc[a4][:, 4:5], func=AF.Gelu), reads=[bacc[a4]], writes=[bacc[a4]])
                    kb.op('act', lambda e, a4=a4, a2=a2, t=t: e.activation(out=win[a2][:, 127:128], in_=acc[a4][:, 5:6], func=AF.Identity, scale=gT[:, t:t + 1]),
                          reads=[bacc[a4], bgT], writes=[bwin[a2]])
                    if t + 1 < 2048:
                        emit_xb(t + 1)
                    for b4 in range(4):
                        kb.op('pe', lambda e, b4=b4, a2=a2, tt=tt, u=u: e.matmul(pOut[b4][:], lhsT=win[a2][:, 127 - tt:255 - tt], rhs=gb[u][:, 2048 + b4 * 512:2048 + (b4 + 1) * 512],
                                                                                start=(tt == 0), stop=(tt == 127)), reads=[bwin[a2], bgb[u]], writes=[bpOut])
                    if tt == 127:
                        load(h16[:], H1v[ot], bh16, None, q='act')
                        for b4 in range(4):
                            kb.op('dve', lambda e, b4=b4: e.tensor_tensor(out=tmp6[:, b4 * 512:(b4 + 1) * 512], in0=pOut[b4][:], in1=gt2bc[:, b4 * 512:(b4 + 1) * 512], op=ALU.mult),
                                  reads=[bpOut, bbc6], writes=[btmp6])
                        kb.op('pool', lambda e: e.tensor_tensor(out=hf6[:], in0=tmp6[:], in1=h16[:], op=ALU.add), reads=[btmp6, bh16], writes=[bhf6])
                        kb.op('act', lambda e: e.activation(out=tmp6[:], in_=hf6[:], func=AF.Square, accum_out=ss6[:, 0:1]), reads=[bhf6], writes=[btmp6, bss6])
                        rstd_from_ss(ss6[:, 0:1], ss6[:, 1:2], bss6, brs6, float(D))
                        kb.op('dve', lambda e: e.scalar_tensor_tensor(out=o6[:], in0=hf6[:], scalar=ss6[:, 1:2], in1=gfbc[:], op0=ALU.mult, op1=ALU.mult),
                              reads=[bhf6, brs6, bbc6], writes=[bo6])
                        store(Ov[ot], o6[:], bo6, bOut, None, q='sp')
                kb.barrier()

        blk = es.enter_context(nc.Block())
        kb.emit(blk)
    return nc


_NC = None


def kernel(x, c, positions, w_ada, b_ada, g_norm1, w_in, g_attn_out, g_sgu_out, sgu_w, sgu_b, sgu_ln_g, sgu_ln_b,
           w_out, g_norm2, peer_w_q, peer_sub_keys, peer_u, peer_v, g_final, _prep_only=False):
    global _NC
    f32 = np.float32
    x = np.asarray(x, f32); c = np.asarray(c, f32); positions = np.asarray(positions, np.int32)

    def colT(v, n):
        return np.ascontiguousarray(np.asarray(v, f32).reshape(n, 128).T)

    half = 64
    inv = (10000.0 ** (-np.arange(half, dtype=np.float64) / half))
    inv2 = np.concatenate([inv, inv]) / (2.0 * np.pi)
    inv2 = inv2.astype(f32).reshape(128, 1)
    sgn = np.concatenate([-np.ones(64), np.ones(64)]).astype(f32).reshape(128, 1)
    k = np.arange(128)[:, None]
    cidx = np.arange(17 * 128)[None, :]
    delta = cidx - k
    cm = ((delta >= 0) & (delta <= 128)).astype(f32) + ((delta >= 0) & (delta % 4 == 0) & (delta <= 512)).astype(f32) \
        + ((delta >= 0) & (delta % 16 == 0) & (delta <= 2048)).astype(f32)
    maskW = cm.astype(ml_dtypes.bfloat16)
    perm = np.zeros((128, 128), f32)
    perm[(np.arange(128) + 64) % 128, np.arange(128)] = 1.0
    perm = perm.astype(ml_dtypes.bfloat16)
    identf = np.eye(128, dtype=f32)
    iota16 = np.tile(np.arange(16, dtype=f32)[None, :], (128, 1))
    trilT = (np.arange(128)[:, None] <= np.arange(128)[None, :]).astype(f32)

    shared = {
        "w_ada": np.ascontiguousarray(np.asarray(w_ada, f32)[0]),
        "b_adaT": colT(np.asarray(b_ada)[0], 96),
        "gn1T": colT(np.asarray(g_norm1)[0], 16),
        "gn2T": colT(np.asarray(g_norm2)[0], 16),
        "w_in": np.ascontiguousarray(np.asarray(w_in, f32)[0]),
        "w_out": np.ascontiguousarray(np.asarray(w_out, f32)[0]),
        "gmixT": colT(np.concatenate([np.asarray(g_attn_out)[0], np.asarray(g_sgu_out)[0]]), 16),
        "sgu_wT": np.ascontiguousarray(np.transpose(np.asarray(sgu_w, f32)[0], (2, 0, 1))),
        "sgu_bT": np.ascontiguousarray(np.asarray(sgu_b, f32)[0].T),
        "lng": np.ascontiguousarray(np.asarray(sgu_ln_g, f32)[0].reshape(1, 512)),
        "lnb": np.ascontiguousarray(np.asarray(sgu_ln_b, f32)[0].reshape(1, 512)),
        "w_q": np.ascontiguousarray(np.asarray(peer_w_q, f32)[0]),
        "skT": np.ascontiguousarray(np.transpose(np.asarray(peer_sub_keys, f32)[0].reshape(16, 128, 128), (2, 0, 1))),
        "peer_u": np.ascontiguousarray(np.asarray(peer_u, f32)[0]),
        "peer_v": np.ascontiguousarray(np.asarray(peer_v, f32)[0]),
        "gfin": np.ascontiguousarray(np.asarray(g_final, f32).reshape(1, D)),
        "inv2": inv2, "sgn": sgn, "maskW": maskW, "perm": perm, "identf": identf, "iota16": iota16, "trilT": trilT,
    }
    in_maps = []
    for core in range(8):
        b = core // 2
        hf = core % 2
        xa = np.zeros((NCTX + NT, D), f32)
        pa = np.zeros((1, NCTX + NT), np.int32)
        xa[NCTX:] = x[b, hf * NT:(hf + 1) * NT]
        pa[0, NCTX:] = positions[b, hf * NT:(hf + 1) * NT]
        if hf == 1:
            xa[:NCTX] = x[b, 0:NCTX]
            pa[0, :NCTX] = positions[b, 0:NCTX]
        m = dict(shared)
        m["xall"] = xa
        m["pos"] = pa
        m["condT"] = colT(c[b], 16)
        m["flag"] = np.full((128, 1), float(hf), f32)
        in_maps.append(m)
    if _prep_only:
        return in_maps
    if _NC is None:
        import os
        _NC = build(stop=float(os.environ.get('KSTOP', '99')))
    res = run_bass_kernel_spmd(_NC, in_maps, core_ids=list(range(8)))
    out = np.zeros((4, 4096, D), f32)
    for core in range(8):
        b = core // 2
        hf = core % 2
        out[b, hf * NT:(hf + 1) * NT] = np.asarray(res.results[core]["out"], f32)
    return out
```

```python
import numpy as np
import ml_dtypes
from contextlib import ExitStack
import concourse.bass as bass
import concourse.mybir as mybir
from concourse.bass_utils import run_bass_kernel_spmd

F32 = mybir.dt.float32
BF16 = mybir.dt.bfloat16
U32 = mybir.dt.uint32
I32 = mybir.dt.int32
AF = mybir.ActivationFunctionType
ALU = mybir.AluOpType
AX = mybir.AxisListType

D = 2048
NT = 2048
NCTX = 2048
DIN = 5632
ENG = ('pe', 'act', 'dve', 'pool', 'sp')
QG = 2
NW = 16 + QG


class LazySem:
    def __init__(self, name):
        self.name = name
        self.real = None


class Buf:
    def __init__(self, name=''):
        self.w = None
        self.r = {}
        self.name = name
        self.sems = {}
        self.psum = False


def PSB():
    b = Buf('psum')
    b.psum = True
    return b


class KB:
    EP = 16000

    def __init__(self, nc, es):
        self.nc = nc
        self.es = es
        self.streams = {e: [] for e in ENG}
        self.cnt = {e: 0 for e in ENG}
        self.esem = {e: [] for e in ENG}
        self.known = {e: {} for e in ENG}
        self.dcount = {}
        self.dsems = []
        self.nsem = 0
        self.enabled = True

    def _newsem(self, name):
        self.nsem += 1
        return self.es.enter_context(self.nc.semaphore(name))

    def dsem(self, name):
        return LazySem(name)

    def _real(self, ls):
        if ls.real is None:
            sm = self._newsem('d%d_%s' % (self.nsem, ls.name))
            self.dcount[id(sm)] = 0
            self.dsems.append(sm)
            ls.real = sm
        return ls.real

    def buf_sem(self, buf, kind):
        if kind not in buf.sems:
            buf.sems[kind] = LazySem(kind + '_' + (buf.name or 'b'))
        return buf.sems[kind]

    def _eev(self, e):
        c = self.cnt[e]
        ep = (c - 1) // self.EP
        while len(self.esem[e]) <= ep:
            self.esem[e].append(self._newsem('e_%s_%d' % (e, len(self.esem[e]))))
        return (self.esem[e][ep], (c - 1) % self.EP + 1)

    def _deps(self, e, reads, writes):
        evs = []
        for b in reads:
            if b.w is not None:
                evs.append(b.w)
            if b.psum:
                evs.extend(ev for ev in b.r.values() if ev[3] != e)
        for b in writes:
            if b.w is not None:
                evs.append(b.w)
            evs.extend(b.r.values())
        waits = []
        own = self.esem[e]
        k = self.known[e]
        for (sm, val, isd, _eng) in evs:
            if e == 'pe' and any(sm is x for x in own):
                continue
            if isd:
                val = 16 * self.dcount[id(sm)]
            if k.get(id(sm), 0) < val:
                k[id(sm)] = val
                waits.append((sm, val))
        return waits

    def _book(self, ev, reads, writes):
        for b in reads:
            b.r[id(ev[0])] = ev
        for b in writes:
            b.w = ev
            b.r = {}

    def op(self, e, fn, reads=(), writes=()):
        if not self.enabled:
            return
        waits = self._deps(e, reads, writes)
        self.cnt[e] += 1
        sm, val = self._eev(e)
        self.streams[e].append((waits, fn, (sm, 1)))
        self._book((sm, val, False, e), reads, writes)

    def dma(self, q, fn, sem, reads=(), writes=(), after=()):
        if not self.enabled:
            return
        sem = self._real(sem)
        waits = self._deps(q, list(reads) + list(after), writes)
        self.dcount[id(sem)] += 1
        self.streams[q].append((waits, fn, (sem, 16)))
        self._book((sem, 16 * self.dcount[id(sem)], True, q), reads, writes)

    def barrier(self):
        if not self.enabled:
            return
        for e in ENG:
            waits = []
            k = self.known[e]
            for e2 in ENG:
                if self.cnt[e2] > 0 and not (e == 'pe' and e2 == 'pe'):
                    sm, val = self._eev(e2)
                    if k.get(id(sm), 0) < val:
                        k[id(sm)] = val
                        waits.append((sm, val))
            for sm in self.dsems:
                val = 16 * self.dcount[id(sm)]
                if val > 0 and k.get(id(sm), 0) < val:
                    k[id(sm)] = val
                    waits.append((sm, val))
            if waits:
                self.streams[e].append((waits, None, None))

    def emit(self, blk):
        emap = {'pe': blk.tensor, 'act': blk.scalar, 'dve': blk.vector, 'pool': blk.gpsimd, 'sp': blk.sync}
        for e in ENG:
            def body(eng, e=e):
                for waits, fn, inc in self.streams[e]:
                    for sm, val in waits:
                        eng.wait_ge(sm, val)
                    if fn is not None:
                        fn(eng).then_inc(inc[0], inc[1])
            emap[e](body)


def build(stop=99, dbg=False):
    nc = bass.Bass("TRN2", target_bir_lowering=False)
    SK = "ExternalOutput" if dbg else "Internal"

    def din(name, shape, dt=F32):
        return nc.dram_tensor(name, list(shape), dt, kind="ExternalInput").ap()

    xall = din("xall", [NCTX + NT, D])
    condT_d = din("condT", [128, 16])
    pos_d = din("pos", [1, NCTX + NT], I32)
    flag_d = din("flag", [128, 1])
    w_ada = din("w_ada", [D, 6 * D])
    b_adaT = din("b_adaT", [128, 96])
    gn1T_d = din("gn1T", [128, 16])
    gn2T_d = din("gn2T", [128, 16])
    w_in = din("w_in", [D, DIN])
    w_out = din("w_out", [D, D])
    gmixT_d = din("gmixT", [128, 16])
    sgu_wT_d = din("sgu_wT", [128, 4, 128])
    sgu_bT_d = din("sgu_bT", [128, 4])
    lng_d = din("lng", [1, 512])
    lnb_d = din("lnb", [1, 512])
    w_q = din("w_q", [D, D])
    skT_d = din("skT", [128, 16, 128])
    peer_u = din("peer_u", [16384, D])
    peer_v = din("peer_v", [16384, D])
    gfin_d = din("gfin", [1, D])
    inv2_d = din("inv2", [128, 1])
    sgn_d = din("sgn", [128, 1])
    maskW_d = din("maskW", [128, 17 * 128], BF16)
    perm_d = din("perm", [128, 128], BF16)
    identf_d = din("identf", [128, 128])
    iota_d = din("iota16", [128, 16])
    tril_d = din("trilT", [128, 128])
    out_d = nc.dram_tensor("out", [NT, D], F32, kind="ExternalOutput").ap()

    dbgmod = nc.dram_tensor("dbgmod", [128, 128], F32, kind=SK).ap()
    dbgidx = nc.dram_tensor("dbgidx", [128, 2048], I32, kind=SK).ap()
    dbgg = nc.dram_tensor("dbgg", [128, 2048], F32, kind=SK).ap()
    KTs = nc.dram_tensor("KTs", [12, 128, NCTX + NT], BF16, kind=SK).ap()
    QTs = nc.dram_tensor("QTs", [12, 128, NT], BF16, kind=SK).ap()
    Vs = nc.dram_tensor("Vs", [NCTX + NT, 12 * 129], BF16, kind=SK).ap()
    UVs = nc.dram_tensor("UVs", [NT, 1024], F32, kind=SK).ap()
    MTs = nc.dram_tensor("MTs", [16, 128, 2048], BF16, kind=SK).ap()
    H1s = nc.dram_tensor("H1s", [NT, D], F32, kind=SK).ap()
    HN2s = nc.dram_tensor("HN2s", [NT, D], BF16, kind=SK).ap()
    HN2Ts = nc.dram_tensor("HN2Ts", [16, 128, 2048], BF16, kind=SK).ap()
    UV16 = nc.dram_tensor("UV16", [16384, 4096], BF16).ap()

    with ExitStack() as es:
        kb = KB(nc, es)

        uniq = [0]

        def alloc(st, name, shape, dt):
            uniq[0] += 1
            return st.enter_context(nc.sbuf_tensor("s%d_%s" % (uniq[0], name), list(shape), dt))

        def palloc(st, name, shape, dt):
            uniq[0] += 1
            return st.enter_context(nc.psum_tensor("p%d_%s" % (uniq[0], name), list(shape), dt))

        qrr = [0]

        def ldq():
            qrr[0] += 1
            return 'sp' if qrr[0] % 2 else 'act'

        def load(dst_ap, src_ap, buf, sem, q=None, reads=()):
            kb.dma(q or 'sp', lambda e: e.dma_start(out=dst_ap, in_=src_ap), kb.buf_sem(buf, 'l'), reads=(), writes=[buf])

        def store(dst_ap, src_ap, srcbuf, dstbuf, sem, q=None):
            kb.dma(q or 'sp', lambda e: e.dma_start(out=dst_ap, in_=src_ap), kb.buf_sem(srcbuf, 's'), reads=[srcbuf], writes=())

        cast_sem = kb.dsem('cast')
        cast_todo = []
        for cch in range(16):
            rows = slice(cch * 1024, (cch + 1) * 1024)
            cast_todo.append((UV16[rows, 0:2048], peer_u[rows, :]))
            cast_todo.append((UV16[rows, 2048:4096], peer_v[rows, :]))

        def emit_cast(k, after=()):
            for _ in range(k):
                if cast_todo:
                    o_ap, i_ap = cast_todo.pop(0)
                    kb.dma('pool', lambda e, o_ap=o_ap, i_ap=i_ap: e.dma_start(out=o_ap, in_=i_ap), cast_sem, after=after)

        def flush_cast():
            emit_cast(len(cast_todo))

        G = es
        identf = alloc(G, "identf", [128, 128], F32)
        identb = alloc(G, "identb", [128, 128], BF16)
        onesf = alloc(G, "onesf", [128, 128], F32)
        perm = alloc(G, "perm", [128, 128], BF16)
        flag = alloc(G, "flag", [128, 1], F32)
        onec = alloc(G, "onec", [128, 1], F32)
        modT = alloc(G, "modT", [128, 96], F32)
        gs1T = alloc(G, "gs1T", [128, 16], F32)
        gs2T = alloc(G, "gs2T", [128, 16], F32)
        epsc = alloc(G, "epsc", [128, 1], F32)
        epsl = alloc(G, "epsl", [128, 1], F32)
        bC = Buf('const')
        bMod = Buf('mod')
        sC = kb.dsem('const')
        load(identf[:], identf_d, bC, sC)
        load(perm[:], perm_d, bC, sC)
        load(flag[:], flag_d, bC, sC)
        kb.op('dve', lambda e: e.tensor_copy(out=identb[:], in_=identf[:]), reads=[bC], writes=[bC])
        kb.op('dve', lambda e: e.memset(onesf[:], 1.0), writes=[bC])
        kb.op('dve', lambda e: e.memset(onec[:], 1.0), writes=[bC])
        kb.op('dve', lambda e: e.memset(epsc[:, 0:1], 1e-6), writes=[bC])
        kb.op('dve', lambda e: e.memset(epsl[:, 0:1], 1e-5), writes=[bC])

        def rstd_from_ss(ss, rstd, bss, brs, n, epscol=0):
            kb.op('act', lambda e: e.activation(out=rstd, in_=ss, func=AF.Sqrt, bias=epsc[:, 0:1], scale=1.0 / n),
                  reads=[bss, bC], writes=[brs])
            kb.op('dve', lambda e: e.reciprocal(out=rstd, in_=rstd), reads=[brs], writes=[brs])

        def make_bc(st, dst, dstbuf, srcT, srcbuf, ps4, psbuf, tmpd, tmpbuf):
            for dc in range(16):
                kb.op('dve', lambda e, dc=dc: e.tensor_scalar(out=tmpd[:], in0=identf[:], scalar1=srcT[:, dc:dc + 1], scalar2=None, op0=ALU.mult),
                      reads=[bC, srcbuf], writes=[tmpbuf])
                kb.op('pe', lambda e, dc=dc: e.matmul(ps4[dc // 4][:, (dc % 4) * 128:(dc % 4 + 1) * 128], lhsT=onesf[:], rhs=tmpd[:], start=True, stop=True),
                      reads=[bC, tmpbuf], writes=[psbuf])
            for b4 in range(4):
                kb.op('act', lambda e, b4=b4: e.copy(out=dst[:, b4 * 512:(b4 + 1) * 512], in_=ps4[b4][:]), reads=[psbuf], writes=[dstbuf])

        with ExitStack() as P1:
            condT = alloc(P1, "condT", [128, 16], F32)
            badaT = alloc(P1, "badaT", [128, 96], F32)
            gnT = alloc(P1, "gnT", [128, 32], F32)
            wst = [alloc(P1, "wst%d" % i, [128, 16, 512], F32) for i in range(3)]
            psmod = palloc(P1, "psmod", [128, 512], F32)
            bcond = Buf(); bw = [Buf(), Buf(), Buf()]; bps = PSB()
            sw = [kb.dsem('wada0'), kb.dsem('wada1')]
            load(condT[:], condT_d, bcond, sC)
            load(badaT[:], b_adaT, bcond, sC)
            load(gnT[:, 0:16], gn1T_d, bcond, sC)
            load(gnT[:, 16:32], gn2T_d, bcond, sC)
            kb.op('act', lambda e: e.activation(out=condT[:], in_=condT[:], func=AF.Silu), reads=[bcond], writes=[bcond])
            wsb = [alloc(P1, "wsb%d" % i, [128, 16, 512], BF16) for i in range(2)]
            condb = alloc(P1, "condb", [128, 16], BF16)
            bwsb = [Buf(), Buf()]
            kb.op('dve', lambda e: e.tensor_copy(out=condb[:], in_=condT[:]), reads=[bcond], writes=[bcond])
            wv = w_ada.rearrange("(kc p) f -> p kc f", p=128)
            for blk in range(24):
                i3 = blk % 3
                i = blk % 2
                load(wst[i3][:], wv[:, :, blk * 512:(blk + 1) * 512], bw[i3], None, q=ldq())
                kb.op('dve' if i == 0 else 'pool', lambda e, i=i, i3=i3: e.tensor_copy(out=wsb[i][:], in_=wst[i3][:]), reads=[bw[i3]], writes=[bwsb[i]])
                for j in range(4):
                    col = blk * 4 + j
                    for kc in range(16):
                        kb.op('pe', lambda e, i=i, j=j, kc=kc, col=col: e.matmul(
                            psmod[:, col:col + 1], lhsT=wsb[i][:, kc, j * 128:(j + 1) * 128], rhs=condb[:, kc:kc + 1],
                            start=(kc == 0), stop=(kc == 15)), reads=[bwsb[i], bcond], writes=[bps])
            kb.op('dve', lambda e: e.tensor_tensor(out=modT[:], in0=psmod[:, 0:96], in1=badaT[:], op=ALU.add), reads=[bps, bcond], writes=[bMod])
            kb.op('dve', lambda e: e.scalar_tensor_tensor(out=gs1T[:], in0=modT[:, 16:32], scalar=1.0, in1=gnT[:, 0:16], op0=ALU.add, op1=ALU.mult),
                  reads=[bMod, bcond], writes=[bMod])
            kb.op('dve', lambda e: e.scalar_tensor_tensor(out=gs2T[:], in0=modT[:, 64:80], scalar=1.0, in1=gnT[:, 16:32], op0=ALU.add, op1=ALU.mult),
                  reads=[bMod, bcond], writes=[bMod])
            if dbg:
                sdb = kb.dsem('dbg')
                bdb = Buf()
                store(dbgmod[:, 0:96], modT[:], bMod, bdb, sdb)
                store(dbgmod[:, 96:112], gs1T[:], bMod, bdb, sdb)
                store(dbgmod[:, 112:128], gs2T[:], bMod, bdb, sdb)
            kb.barrier()
        if stop <= 1:
            kb.enabled = False
        sh1T = modT[:, 0:16]
        gt1T = modT[:, 32:48]
        sh2T = modT[:, 48:64]
        gt2T = modT[:, 80:96]

        with ExitStack() as P2:
            ropetab = alloc(P2, "ropetab", [128, 2, 4096], F32)
            sinT = ropetab[:, 0, :]
            cosT = ropetab[:, 1, :]
            bTab = Buf(); bhnT = Buf()
            with ExitStack() as P2r:
                posi = alloc(P2r, "posi", [128, 4096], I32)
                y = alloc(P2r, "ry", [128, 4096], F32)
                y2 = alloc(P2r, "ry2", [128, 2, 4096], F32)
                yi = alloc(P2r, "ryi", [128, 2, 4096], I32)
                m = alloc(P2r, "rm", [128, 2, 4096], F32)
                inv2 = alloc(P2r, "inv2", [128, 1], F32)
                sgn = alloc(P2r, "sgn", [128, 1], F32)
                bR = Buf()
                load(posi[:], pos_d.to_broadcast([128, 4096]), bR, sC)
                load(inv2[:], inv2_d, bR, sC)
                load(sgn[:], sgn_d, bR, sC)
                kb.op('dve', lambda e: e.tensor_copy(out=y[:], in_=posi[:]), reads=[bR], writes=[bR])
                kb.op('dve', lambda e: e.tensor_scalar(out=y2[:, 0, :], in0=y[:], scalar1=inv2[:, 0:1], scalar2=None, op0=ALU.mult), reads=[bR], writes=[bR])
                kb.op('dve', lambda e: e.tensor_scalar(out=y2[:, 1, :], in0=y[:], scalar1=inv2[:, 0:1], scalar2=0.25, op0=ALU.mult, op1=ALU.add), reads=[bR], writes=[bR])
                kb.op('dve', lambda e: e.tensor_copy(out=yi[:], in_=y2[:]), reads=[bR], writes=[bR])
                kb.op('dve', lambda e: e.tensor_copy(out=m[:], in_=yi[:]), reads=[bR], writes=[bR])
                kb.op('dve', lambda e: e.tensor_tensor(out=y2[:], in0=y2[:], in1=m[:], op=ALU.subtract), reads=[bR], writes=[bR])
                kb.op('dve', lambda e: e.tensor_scalar(out=m[:], in0=y2[:], scalar1=0.5, scalar2=None, op0=ALU.is_gt), reads=[bR], writes=[bR])
                kb.op('dve', lambda e: e.tensor_tensor(out=y2[:], in0=y2[:], in1=m[:], op=ALU.subtract), reads=[bR], writes=[bR])
                kb.op('dve', lambda e: e.tensor_scalar(out=m[:], in0=y2[:], scalar1=-0.5, scalar2=None, op0=ALU.is_lt), reads=[bR], writes=[bR])
                kb.op('dve', lambda e: e.tensor_tensor(out=y2[:], in0=y2[:], in1=m[:], op=ALU.add), reads=[bR], writes=[bR])
                kb.op('act', lambda e: e.activation(out=ropetab[:], in_=y2[:], func=AF.Sin, scale=2.0 * np.pi * (1.0 - 1e-6)),
                      reads=[bR], writes=[bTab])
                kb.op('dve', lambda e: e.tensor_scalar(out=ropetab[:, 0, :], in0=ropetab[:, 0, :], scalar1=sgn[:, 0:1], scalar2=None, op0=ALU.mult), reads=[bTab, bR], writes=[bTab])
                kb.barrier()
            if stop <= 1.5:
                kb.enabled = False
            hnT = alloc(P2, "hnT", [128, 16, 2048], BF16)

            xv = xall.rearrange("(n p) d -> n p d", p=128)
            Vsv = Vs.rearrange("(n p) c -> n p c", p=128)
            UVv = UVs.rearrange("(n p) c -> n p c", p=128)
            bKTs = Buf(); bQTs = Buf(); bVs = Buf(); bUVs = Buf()
            wiv = w_in.rearrange("(kc p) f -> p kc f", p=128)
            def do_half(half):
                with ExitStack() as PA:
                    xst = [alloc(PA, "xst%d" % i, [128, 2048], F32) for i in range(2)]
                    xnb = [alloc(PA, "xnb%d" % i, [128, 2048], BF16) for i in range(2)]
                    junk = alloc(PA, "junkA", [128, 2048], BF16)
                    ss = [alloc(PA, "ssA%d" % i, [128, 2], F32) for i in range(2)]
                    pT = [palloc(PA, "pTA%d" % i, [128, 2048], BF16) for i in range(2)]
                    bx = [Buf(), Buf()]; bxn = [Buf(), Buf()]; bj = Buf(); bss = [Buf(), Buf()]; brs = [Buf(), Buf()]; bpT = [PSB(), PSB()]
                    sx = [kb.dsem('xA0_%d' % half), kb.dsem('xA1_%d' % half)]
                    def S1(tt):
                        i = tt % 2
                        gt = half * 16 + tt
                        load(xst[i][:], xv[gt], bx[i], None, q=ldq())
                        kb.op('act', lambda e, i=i: e.activation(out=junk[:], in_=xst[i][:], func=AF.Square, accum_out=ss[i][:, 0:1]),
                              reads=[bx[i]], writes=[bj, bss[i]])
                        rstd_from_ss(ss[i][:, 0:1], ss[i][:, 1:2], bss[i], brs[i], float(D))
                        kb.op('dve', lambda e, i=i: e.tensor_scalar(out=xnb[i][:], in0=xst[i][:], scalar1=ss[i][:, 1:2], scalar2=None, op0=ALU.mult),
                              reads=[bx[i], brs[i]], writes=[bxn[i]])

                    def S23(tt):
                        i = tt % 2
                        for dc in range(16):
                            kb.op('pe', lambda e, i=i, dc=dc: e.transpose(out=pT[i][:, dc * 128:(dc + 1) * 128], in_=xnb[i][:, dc * 128:(dc + 1) * 128], identity=identb[:]),
                                  reads=[bxn[i], bC], writes=[bpT[i]])
                        for dc in range(16):
                            kb.op('dve', lambda e, i=i, dc=dc, tt=tt: e.tensor_scalar(
                                out=hnT[:, dc, tt * 128:(tt + 1) * 128], in0=pT[i][:, dc * 128:(dc + 1) * 128],
                                scalar1=gs1T[:, dc:dc + 1], scalar2=sh1T[:, dc:dc + 1], op0=ALU.mult, op1=ALU.add),
                                reads=[bpT[i], bMod], writes=[bhnT])

                    S1(0)
                    for tt in range(16):
                        if tt + 1 < 16:
                            S1(tt + 1)
                        S23(tt)
                    kb.barrier()
                if stop <= 1.6 + 0.2 * half:
                    kb.enabled = False
                with ExitStack() as PB:
                    wf = [alloc(PB, "wf%d" % i, [128, 16, 256], F32) for i in range(2)]
                    wb = [alloc(PB, "wb%d" % i, [128, 16, 512], BF16) for i in range(2)]
                    qb = [alloc(PB, "qb%d" % i, [128, 512], BF16) for i in range(2)]
                    t1 = [alloc(PB, "t1%d" % i, [128, 512], F32) for i in range(2)]
                    t2 = [alloc(PB, "t2%d" % i, [128, 512], F32) for i in range(2)]
                    ro = [alloc(PB, "ro%d" % i, [128, 512], BF16) for i in range(2)]
                    vst = [alloc(PB, "vst%d" % i, [128, 4, 129], BF16) for i in range(2)]
                    uvst = [alloc(PB, "uvst%d" % i, [128, 512], F32) for i in range(2)]
                    pq = [palloc(PB, "pq%d" % i, [128, 512], F32) for i in range(2)]
                    psw = [palloc(PB, "psw%d" % i, [128, 512], F32) for i in range(2)]
                    pv = [palloc(PB, "pv%d" % i, [128, 512], F32) for i in range(2)]
                    bwf = [Buf(), Buf()]; bwb = [Buf(), Buf()]; bqb = [Buf(), Buf()]; bt1 = [Buf(), Buf()]; bt2 = [Buf(), Buf()]
                    bro = [Buf(), Buf()]; bvst = [Buf(), Buf()]; buv = [Buf(), Buf()]; bpq = [PSB(), PSB()]; bpsw = [PSB(), PSB()]; bpv = [PSB(), PSB()]
                    swf = [kb.dsem('wf0_%d' % half), kb.dsem('wf1_%d' % half)]
                    sro = [kb.dsem('ro0_%d' % half), kb.dsem('ro1_%d' % half)]
                    svs = [kb.dsem('vs0_%d' % half), kb.dsem('vs1_%d' % half)]
                    suv = [kb.dsem('uv0_%d' % half), kb.dsem('uv1_%d' % half)]
                    fl = flag if half == 0 else onec
                    for i in range(2):
                        kb.op('dve', lambda e, i=i: e.tensor_copy(out=vst[i][:, :, 128:129], in_=fl[:, 0:1].unsqueeze(1).to_broadcast([128, 4, 1])),
                              reads=[bC], writes=[bvst[i]])
                    blocks = [3, 4, 5, 6, 7, 8] if half == 0 else list(range(11))
                    cnt_fm = 0
                    cnt_tm = 0
                    pending = []
                    for bi, blk in enumerate(blocks):
                        i = bi % 2
                        for hc in range(2):
                            load(wf[hc][:], wiv[:, :, blk * 512 + hc * 256:blk * 512 + (hc + 1) * 256], bwf[hc], swf[hc], q=ldq())
                            kb.op('pool', lambda e, i=i, hc=hc: e.tensor_copy(out=wb[i][:, :, hc * 256:(hc + 1) * 256], in_=wf[hc][:]),
                                  reads=[bwf[hc]], writes=[bwb[i]])
                        import os
                        KS = os.environ.get('KSKIP', '')
                        if KS == 'b1' or (KS == 'b2' and blk >= 6) or (KS == 'b3' and blk < 6):
                            continue
                        if blk < 6:
                            isq = blk < 3
                            for hh in range(4):
                                head = (blk % 3) * 4 + hh
                                for tg in range(4):
                                    c = cnt_fm % 2
                                    cnt_fm += 1
                                    for kc in range(16):
                                        kb.op('pe', lambda e, i=i, c=c, kc=kc, hh=hh, tg=tg: e.matmul(
                                            pq[c][:], lhsT=wb[i][:, kc, hh * 128:(hh + 1) * 128], rhs=hnT[:, kc, tg * 512:(tg + 1) * 512],
                                            start=(kc == 0), stop=(kc == 15)), reads=[bwb[i], bhnT], writes=[bpq[c]])
                                    tok0 = half * 2048 + tg * 512
                                    kb.op('act', lambda e, c=c: e.copy(out=qb[c][:], in_=pq[c][:]), reads=[bpq[c]], writes=[bqb[c]])
                                    kb.op('dve', lambda e, c=c, tok0=tok0: e.tensor_tensor(out=t1[c][:], in0=pq[c][:], in1=cosT[:, tok0:tok0 + 512], op=ALU.mult),
                                          reads=[bpq[c], bTab], writes=[bt1[c]])
                                    def post(c=c, tok0=tok0, isq=isq, head=head, tg=tg):
                                        kb.op('pe', lambda e: e.matmul(psw[c][:], lhsT=perm[:], rhs=qb[c][:], start=True, stop=True),
                                              reads=[bC, bqb[c]], writes=[bpsw[c]])
                                        kb.op('dve', lambda e: e.tensor_tensor(out=t2[c][:], in0=psw[c][:], in1=sinT[:, tok0:tok0 + 512], op=ALU.mult),
                                              reads=[bpsw[c], bTab], writes=[bt2[c]])
                                        kb.op('pool', lambda e: e.tensor_tensor(out=ro[c][:], in0=t1[c][:], in1=t2[c][:], op=ALU.add),
                                              reads=[bt1[c], bt2[c]], writes=[bro[c]])
                                        if isq:
                                            store(QTs[head, :, tg * 512:(tg + 1) * 512], ro[c][:], bro[c], bQTs, None, q='sp')
                                        else:
                                            store(KTs[head, :, tok0:tok0 + 512], ro[c][:], bro[c], bKTs, None, q='sp')
                                    if pending:
                                        pending.pop()()
                                    pending.append(post)
                            if pending:
                                pending.pop()()
                        else:
                            for tt in range(16):
                                c = cnt_tm % 2
                                cnt_tm += 1
                                for kc in range(16):
                                    kb.op('pe', lambda e, i=i, c=c, kc=kc, tt=tt: e.matmul(
                                        pv[c][:], lhsT=hnT[:, kc, tt * 128:(tt + 1) * 128], rhs=wb[i][:, kc, :],
                                        start=(kc == 0), stop=(kc == 15)), reads=[bwb[i], bhnT], writes=[bpv[c]])
                                gt = half * 16 + tt
                                if blk < 9:
                                    hb = blk - 6
                                    kb.op('act', lambda e, c=c: e.activation(out=vst[c][:, :, 0:128], in_=pv[c][:].rearrange("p (h e) -> p h e", h=4),
                                                                            func=AF.Identity, scale=fl[:, 0:1]),
                                          reads=[bpv[c], bC], writes=[bvst[c]])
                                    store(Vsv[gt][:, hb * 516:(hb + 1) * 516], vst[c][:].rearrange("p h e -> p (h e)"), bvst[c], bVs, svs[c], q='act')
                                else:
                                    kb.op('act', lambda e, c=c: e.activation(out=uvst[c][:], in_=pv[c][:], func=AF.Gelu), reads=[bpv[c]], writes=[buv[c]])
                                    store(UVv[tt][:, (blk - 9) * 512:(blk - 8) * 512], uvst[c][:], buv[c], bUVs, suv[c], q='act')
                    kb.barrier()

            do_half(0)
            do_half(1)
        if stop <= 2:
            kb.enabled = False

        bMTs = Buf()
        with ExitStack() as P3:
            KTc = [alloc(P3, "KTc%d" % i, [128, 4, NW * 128], BF16) for i in range(2)]
            Vc = [alloc(P3, "Vc%d" % i, [128, NW, 4 * 129], BF16) for i in range(2)]
            QTg = alloc(P3, "QTg", [128, 12, QG * 128], BF16)
            mixed = alloc(P3, "mixed", [128, QG, 2048], F32)
            maskW = alloc(P3, "maskW", [128, 17 * 128], BF16)
            Eb = [alloc(P3, "Eb%d" % i, [128, 512], BF16) for i in range(3)]
            Pb = [alloc(P3, "Pb%d" % i, [128, 512], BF16) for i in range(3)]
            rec = alloc(P3, "rec", [128, QG], F32)
            uvt = alloc(P3, "uvt", [128, 1024], F32)
            lng = alloc(P3, "lng", [128, 512], F32)
            lnb = alloc(P3, "lnb", [128, 512], F32)
            wsTf = alloc(P3, "wsTf", [128, 4, 128], F32)
            wsT = alloc(P3, "wsT", [128, 4, 128], BF16)
            tril = alloc(P3, "tril", [128, 128], F32)
            sbT = alloc(P3, "sbT", [128, 4], F32)
            bst = alloc(P3, "bst", [128, 4, 6], F32)
            mv = alloc(P3, "mv", [128, 4, 2], F32)
            lrs = alloc(P3, "lrs", [128, 4], F32)
            vn = alloc(P3, "vn", [128, 512], F32)
            vnb = alloc(P3, "vnb", [128, 512], BF16)
            junk3 = alloc(P3, "junk3", [128, 1536], BF16)
            ss3 = alloc(P3, "ss3", [128, 4], F32)
            mnb = alloc(P3, "mnb", [128, 2048], BF16)
            mnT = alloc(P3, "mnT", [128, 2048], BF16)
            pS = [palloc(P3, "pS%d" % i, [128, 512], F32) for i in range(3)]
            pO = [palloc(P3, "pO%d" % i, [128, 512], F32) for i in range(QG)]
            pM = palloc(P3, "pM", [128, 512], F32)
            pT3 = palloc(P3, "pT3", [128, 2048], BF16)
            b3c = Buf(); bKTc = [Buf(), Buf()]; bVc = [Buf(), Buf()]; bQTg = Buf(); bmixed = Buf()
            bEb = [Buf(), Buf(), Buf()]; bPb = [Buf(), Buf(), Buf()]; brec = Buf(); buvt = Buf()
            bpS = [PSB(), PSB(), PSB()]; bpO = [PSB() for _ in range(QG)]; bpM = PSB(); bpT3 = PSB()
            bvn = Buf(); bvnb = Buf(); bstat = Buf(); bj3 = Buf(); bss3 = Buf(); brs3 = Buf(); bmnb = Buf(); bmnT = Buf()
            s3c = kb.dsem('c3'); sK = kb.dsem('ktg'); sV = kb.dsem('vg'); sQ = kb.dsem('qtg'); sUV = kb.dsem('uvt'); sMT = kb.dsem('mnT')
            load(maskW[:], maskW_d, b3c, s3c)
            load(lng[:], lng_d.to_broadcast([128, 512]), b3c, s3c)
            load(lnb[:], lnb_d.to_broadcast([128, 512]), b3c, s3c)
            load(wsTf[:], sgu_wT_d, b3c, s3c)
            load(tril[:], tril_d, b3c, s3c)
            load(sbT[:], sgu_bT_d, b3c, s3c)
            for hh in range(4):
                kb.op('dve', lambda e, hh=hh: e.tensor_tensor(out=wsT[:, hh, :], in0=wsTf[:, hh, :], in1=tril[:], op=ALU.mult), reads=[b3c], writes=[b3c])
            KTv = KTs.rearrange("h e t -> e h t")
            QTv = QTs.rearrange("h e t -> e h t")
            Vwv = Vs.rearrange("(n p) c -> p n c", p=128)
            UVv = UVs.rearrange("(n p) c -> n p c", p=128)

            uvt2 = [uvt, alloc(P3, "uvtB", [128, 1024], F32)]
            vn2 = [vn, alloc(P3, "vnB", [128, 512], F32)]
            vnb2 = [vnb, alloc(P3, "vnbB", [128, 512], BF16)]
            mnb2 = [mnb, alloc(P3, "mnbB", [128, 2048], BF16)]
            ss32 = [ss3, alloc(P3, "ss3B", [128, 4], F32)]
            bst2 = [bst, alloc(P3, "bstB", [128, 4, 6], F32)]
            mv2 = [mv, alloc(P3, "mvB", [128, 4, 2], F32)]
            lrs2 = [lrs, alloc(P3, "lrsB", [128, 4], F32)]
            buvt2 = [buvt, Buf()]; bvn2 = [bvn, Buf()]; bvnb2 = [bvnb, Buf()]; bmnb2 = [bmnb, Buf()]; bss32 = [bss3, Buf()]; brs32 = [brs3, Buf()]; bstat2 = [bstat, Buf()]

            def tail_part(g, part):
                for j in range(QG):
                    tail_tile(g, part, j)

            def tail_tile(g, part, j):
                mx = mixed2[g % 2]
                bmx = bmixed2[g % 2]
                if True:
                    ot = g * QG + j
                    uvt_, vn_, vnb_, mnb_, ss_, bst_, mv_, lrs_ = uvt2[j], vn2[j], vnb2[j], mnb2[j], ss32[j], bst2[j], mv2[j], lrs2[j]
                    buvt_, bvn_, bvnb_, bmnb_, bss_, brs_, bstat_ = buvt2[j], bvn2[j], bvnb2[j], bmnb2[j], bss32[j], brs32[j], bstat2[j]
                    if part == 1:
                        load(uvt_[:], UVv[ot], buvt_, None, q='sp')
                        for hh in range(4):
                            kb.op('dve', lambda e, hh=hh: e.bn_stats(out=bst_[:, hh, :], in_=uvt_[:, 512 + hh * 128:512 + (hh + 1) * 128]), reads=[buvt_], writes=[bstat_])
                        for hh in range(4):
                            kb.op('dve', lambda e, hh=hh: e.bn_aggr(out=mv_[:, hh, :], in_=bst_[:, hh, :]), reads=[bstat_], writes=[bstat_])
                        kb.op('act', lambda e: e.activation(out=lrs_[:], in_=mv_[:, :, 1], func=AF.Sqrt, bias=epsl[:, 0:1], scale=1.0), reads=[bstat_, bC], writes=[bstat_])
                        kb.op('dve', lambda e: e.reciprocal(out=lrs_[:], in_=lrs_[:]), reads=[bstat_], writes=[bstat_])
                        for hh in range(4):
                            kb.op('dve', lambda e, hh=hh: e.tensor_scalar(out=vn_[:, hh * 128:(hh + 1) * 128], in0=uvt_[:, 512 + hh * 128:512 + (hh + 1) * 128],
                                                                        scalar1=mv_[:, hh, 0:1], scalar2=lrs_[:, hh:hh + 1], op0=ALU.subtract, op1=ALU.mult),
                                  reads=[buvt_, bstat_], writes=[bvn_])
                        kb.op('pool', lambda e: e.tensor_tensor(out=vn_[:], in0=vn_[:], in1=lng[:], op=ALU.mult), reads=[bvn_, b3c], writes=[bvn_])
                        kb.op('pool', lambda e: e.tensor_tensor(out=vnb_[:], in0=vn_[:], in1=lnb[:], op=ALU.add), reads=[bvn_, b3c], writes=[bvnb_])
                    elif part == 2:
                        for hh in range(4):
                            kb.op('pe', lambda e, hh=hh: e.matmul(pM[:, hh * 128:(hh + 1) * 128], lhsT=wsT[:, hh, :], rhs=vnb_[:, hh * 128:(hh + 1) * 128],
                                                                 start=True, stop=True), reads=[b3c, bvnb_], writes=[bpM])
                        for hh in range(4):
                            kb.op('dve', lambda e, hh=hh, j=j: e.scalar_tensor_tensor(
                                out=mx[:, j, 1536 + hh * 128:1536 + (hh + 1) * 128], in0=pM[:, hh * 128:(hh + 1) * 128], scalar=sbT[:, hh:hh + 1],
                                in1=uvt_[:, hh * 128:(hh + 1) * 128], op0=ALU.add, op1=ALU.mult), reads=[bpM, b3c, buvt_], writes=[bmx])
                        kb.op('act', lambda e, j=j: e.activation(out=junk3[:, 0:1536], in_=mx[:, j, 0:1536], func=AF.Square, accum_out=ss_[:, 0:1]),
                              reads=[bmx], writes=[bj3, bss_])
                        kb.op('act', lambda e, j=j: e.activation(out=junk3[:, 0:512], in_=mx[:, j, 1536:2048], func=AF.Square, accum_out=ss_[:, 1:2]),
                              reads=[bmx], writes=[bj3, bss_])
                        rstd_from_ss(ss_[:, 0:1], ss_[:, 2:3], bss_, brs_, 1536.0)
                        rstd_from_ss(ss_[:, 1:2], ss_[:, 3:4], bss_, brs_, 512.0)
                        kb.op('dve', lambda e, j=j: e.tensor_scalar(out=mnb_[:, 0:1536], in0=mx[:, j, 0:1536], scalar1=ss_[:, 2:3], scalar2=None, op0=ALU.mult),
                              reads=[bmx, brs_], writes=[bmnb_])
                        kb.op('pool', lambda e, j=j: e.tensor_scalar(out=mnb_[:, 1536:2048], in0=mx[:, j, 1536:2048], scalar1=ss_[:, 3:4], scalar2=None, op0=ALU.mult),
                              reads=[bmx, brs_], writes=[bmnb_])
                    else:
                        for dc in range(16):
                            kb.op('pe', lambda e, dc=dc: e.transpose(out=pT3[:, dc * 128:(dc + 1) * 128], in_=mnb_[:, dc * 128:(dc + 1) * 128], identity=identb[:]),
                                  reads=[bmnb_, bC], writes=[bpT3])
                        kb.op('act', lambda e: e.copy(out=mnT[:, 0:1024], in_=pT3[:, 0:1024]), reads=[bpT3], writes=[bmnT])
                        kb.op('dve', lambda e: e.tensor_copy(out=mnT[:, 1024:2048], in_=pT3[:, 1024:2048]), reads=[bpT3], writes=[bmnT])
                        store(MTs[ot], mnT[:], bmnT, bMTs, None, q='sp')

            mixed2 = [mixed, alloc(P3, "mixedB", [128, QG, 2048], F32)]
            bmixed2 = [bmixed, Buf()]
            sc_exp = float(128 ** -0.5)
            for g in range(16 // QG):
                ws = g * QG
                def load_chunk(ci):
                    if ci >= 24:
                        return
                    g_, hc_ = divmod(ci, 3)
                    ws_ = g_ * QG
                    bb = ci % 2
                    load(KTc[bb][:], KTv[:, hc_ * 4:(hc_ + 1) * 4, ws_ * 128:(ws_ + NW) * 128], bKTc[bb], None, q='sp')
                    load(Vc[bb][:], Vwv[:, ws_:ws_ + NW, hc_ * 516:(hc_ + 1) * 516], bVc[bb], None, q='act')
                if g == 0:
                    load_chunk(0)
                load(QTg[:], QTv[:, :, ws * 128:(ws + QG) * 128], bQTg, sQ, q='sp', reads=[bQTs])
                for h in range(12):
                    ci = g * 3 + h // 4
                    cb = ci % 2
                    hl = h % 4
                    if h % 4 == 0:
                        load_chunk(ci + 1)
                    def kinfo(kt):
                        jlo = max(0, kt - 16); jhi = min(QG - 1, kt)
                        return jlo, jhi, (jhi - jlo + 1) * 128

                    def qkpair(kp, h=h, cb=cb, hl=hl):
                        c = kp % 3
                        for s2 in range(2):
                            kt = kp * 2 + s2
                            jlo, jhi, n = kinfo(kt)
                            kb.op('pe', lambda e, kt=kt, jlo=jlo, jhi=jhi, n=n, s2=s2, c=c: e.matmul(
                                pS[c][:, s2 * 256:s2 * 256 + n], lhsT=KTc[cb][:, hl, kt * 128:(kt + 1) * 128], rhs=QTg[:, h, jlo * 128:(jhi + 1) * 128],
                                start=True, stop=True), reads=[bKTc[cb], bQTg], writes=[bpS[c]])
                    qkpair(0)
                    qkpair(1)
                    for kp in range(NW // 2):
                        if kp + 2 < NW // 2:
                            qkpair(kp + 2)
                        c = kp % 3
                        n0 = kinfo(kp * 2)[2]; n1 = kinfo(kp * 2 + 1)[2]
                        if n0 == 256 and n1 == 256:
                            kb.op('act', lambda e, c=c: e.activation(out=Eb[c][:], in_=pS[c][:], func=AF.Exp, scale=sc_exp),
                                  reads=[bpS[c]], writes=[bEb[c]])
                        else:
                            for s2, nn in ((0, n0), (1, n1)):
                                kb.op('act', lambda e, c=c, s2=s2, nn=nn: e.activation(out=Eb[c][:, s2 * 256:s2 * 256 + nn], in_=pS[c][:, s2 * 256:s2 * 256 + nn],
                                                                                     func=AF.Exp, scale=sc_exp), reads=[bpS[c]], writes=[bEb[c]])
                        for s2 in range(2):
                            kt = kp * 2 + s2
                            jlo, jhi, n = kinfo(kt)
                            m0 = (16 + jlo - kt) * 128
                            kb.op('dve', lambda e, c=c, n=n, m0=m0, s2=s2: e.tensor_tensor(out=Pb[c][:, s2 * 256:s2 * 256 + n], in0=Eb[c][:, s2 * 256:s2 * 256 + n],
                                                                                         in1=maskW[:, m0:m0 + n], op=ALU.mult),
                                  reads=[bEb[c], b3c], writes=[bPb[c]])
                        for s2 in range(2):
                            kt = kp * 2 + s2
                            jlo, jhi, n = kinfo(kt)
                            for j in range(jlo, jhi + 1):
                                kb.op('pe', lambda e, c=c, j=j, jlo=jlo, kt=kt, h=h, s2=s2, cb=cb, hl=hl: e.matmul(
                                    pO[j][:, 0:129], lhsT=Pb[c][:, s2 * 256 + (j - jlo) * 128:s2 * 256 + (j - jlo + 1) * 128], rhs=Vc[cb][:, kt, hl * 129:(hl + 1) * 129],
                                    start=(kt == j), stop=(kt == 16 + j)), reads=[bPb[c], bVc[cb]], writes=[bpO[j]])
                    for j in range(QG):
                        kb.op('dve', lambda e, j=j: e.reciprocal(out=rec[:, j:j + 1], in_=pO[j][:, 128:129]), reads=[bpO[j]], writes=[brec])
                        kb.op('dve', lambda e, j=j, h=h, mxh=mixed2[g % 2]: e.tensor_scalar(out=mxh[:, j, h * 128:(h + 1) * 128], in0=pO[j][:, 0:128],
                                                                      scalar1=rec[:, j:j + 1], scalar2=None, op0=ALU.mult),
                              reads=[bpO[j], brec], writes=[bmixed2[g % 2]])
                    if g > 0 and h in (0, 1, 2):
                        tail_part(g - 1, h + 1)
                pass
            for part in (1, 2, 3):
                tail_part(16 // QG - 1, part)
            kb.barrier()
        if stop <= 3:
            kb.enabled = False

        bH1s = Buf(); bHN2s = Buf(); bHN2Ts = Buf()

        def load_weight_bf16(st, wdram, wdst, bwdst, scaleT, name):
            stg = [alloc(st, name + "stg%d" % i, [128, 2048], F32) for i in range(2)]
            bstg = [Buf(), Buf()]
            sst = [kb.dsem(name + 's0'), kb.dsem(name + 's1')]
            wv_ = wdram.rearrange("(kc p) f -> p kc f", p=128)
            for kc in range(16):
                i = kc % 2
                load(stg[i][:], wv_[:, kc, :], bstg[i], sst[i], q=ldq())
                if scaleT is None:
                    kb.op('pool', lambda e, i=i, kc=kc: e.tensor_copy(out=wdst[:, kc, :], in_=stg[i][:]), reads=[bstg[i]], writes=[bwdst])
                else:
                    kb.op('pool', lambda e, i=i, kc=kc: e.tensor_scalar(out=wdst[:, kc, :], in0=stg[i][:], scalar1=scaleT[:, kc:kc + 1],
                                                                       scalar2=None, op0=ALU.mult), reads=[bstg[i], bC], writes=[bwdst])

        with ExitStack() as P4:
            wo = alloc(P4, "wo", [128, 16, 2048], BF16)
            gmixT = alloc(P4, "gmixT", [128, 16], F32)
            gt1bc = alloc(P4, "gt1bc", [128, 2048], F32)
            gs2bc = alloc(P4, "gs2bc", [128, 2048], F32)
            sh2bc = alloc(P4, "sh2bc", [128, 2048], F32)
            dtmp = alloc(P4, "dtmp", [128, 128], F32)
            mnT4 = [alloc(P4, "mnT4%d" % i, [128, 16, 128], BF16) for i in range(2)]
            x4 = [alloc(P4, "x4%d" % i, [128, 2048], F32) for i in range(2)]
            tmp4 = alloc(P4, "tmp4", [128, 2048], F32)
            h14 = alloc(P4, "h14", [128, 2048], F32)
            junk4 = alloc(P4, "junk4", [128, 2048], BF16)
            ss4 = alloc(P4, "ss4", [128, 2], F32)
            hn2 = alloc(P4, "hn2", [128, 2048], BF16)
            hn2T = alloc(P4, "hn2T", [128, 2048], BF16)
            pW = [palloc(P4, "pW%d" % i, [128, 512], F32) for i in range(4)]
            pT4 = palloc(P4, "pT4", [128, 2048], BF16)
            bwo = Buf(); b4c = Buf(); bbc = Buf(); bdt = Buf(); bmn4 = [Buf(), Buf()]; bx4 = [Buf(), Buf()]; btmp4 = Buf(); bh14 = Buf()
            bj4 = Buf(); bss4 = Buf(); brs4 = Buf(); bhn2 = Buf(); bhn2T = Buf(); bpW = PSB(); bpT4 = PSB()
            s4c = kb.dsem('c4'); smn = [kb.dsem('mn40'), kb.dsem('mn41')]; sx4 = [kb.dsem('x40'), kb.dsem('x41')]
            sh1 = kb.dsem('h1st'); shn2 = kb.dsem('hn2st'); shn2T = kb.dsem('hn2Tst')
            load(gmixT[:], gmixT_d, bC, s4c)
            with ExitStack() as P4w:
                load_weight_bf16(P4w, w_out, wo, bwo, gmixT, "wo")
                make_bc(P4w, gt1bc, bbc, gt1T, bMod, pW, bpW, dtmp, bdt)
                make_bc(P4w, gs2bc, bbc, gs2T, bMod, pW, bpW, dtmp, bdt)
                make_bc(P4w, sh2bc, bbc, sh2T, bMod, pW, bpW, dtmp, bdt)
                kb.barrier()
            xov = xall.rearrange("(n p) d -> n p d", p=128)
            H1v = H1s.rearrange("(n p) d -> n p d", p=128)
            HN2v = HN2s.rearrange("(n p) d -> n p d", p=128)
            h14b = [h14, alloc(P4, "h14b", [128, 2048], F32)]
            ss4b = [ss4, alloc(P4, "ss4b", [128, 2], F32)]
            tmpB = alloc(P4, "tmpB4", [128, 2048], F32)
            bh14b = [bh14, Buf()]; bss4b = [bss4, Buf()]; brs4b = [brs4, Buf()]; btmpB = Buf()

            def stageA1(ot):
                i = ot % 2
                load(mnT4[i][:], MTs[ot].rearrange("p (kc t) -> p kc t", kc=16), bmn4[i], None, q='sp')
                load(x4[i][:], xov[16 + ot], bx4[i], None, q='act')
                for nb in range(4):
                    for kc in range(16):
                        kb.op('pe', lambda e, i=i, nb=nb, kc=kc: e.matmul(pW[nb][:], lhsT=mnT4[i][:, kc, :], rhs=wo[:, kc, nb * 512:(nb + 1) * 512],
                                                                         start=(kc == 0), stop=(kc == 15)), reads=[bmn4[i], bwo], writes=[bpW])

            def stageA2(ot):
                i = ot % 2
                for nb in range(4):
                    kb.op('dve', lambda e, nb=nb: e.tensor_tensor(out=tmp4[:, nb * 512:(nb + 1) * 512], in0=pW[nb][:], in1=gt1bc[:, nb * 512:(nb + 1) * 512], op=ALU.mult),
                          reads=[bpW, bbc], writes=[btmp4])
                kb.op('pool', lambda e, i=i: e.tensor_tensor(out=h14b[i][:], in0=tmp4[:], in1=x4[i][:], op=ALU.add), reads=[btmp4, bx4[i]], writes=[bh14b[i]])
                store(H1v[ot], h14b[i][:], bh14b[i], bH1s, None, q='sp')
                kb.op('act', lambda e, i=i: e.activation(out=junk4[:], in_=h14b[i][:], func=AF.Square, accum_out=ss4b[i][:, 0:1]), reads=[bh14b[i]], writes=[bj4, bss4b[i]])
                rstd_from_ss(ss4b[i][:, 0:1], ss4b[i][:, 1:2], bss4b[i], brs4b[i], float(D))

            def stageB1(ot):
                i = ot % 2
                kb.op('dve', lambda e, i=i: e.scalar_tensor_tensor(out=tmpB[:], in0=h14b[i][:], scalar=ss4b[i][:, 1:2], in1=gs2bc[:], op0=ALU.mult, op1=ALU.mult),
                      reads=[bh14b[i], brs4b[i], bbc], writes=[btmpB])
                kb.op('pool', lambda e: e.tensor_tensor(out=hn2[:], in0=tmpB[:], in1=sh2bc[:], op=ALU.add), reads=[btmpB, bbc], writes=[bhn2])
                store(HN2v[ot], hn2[:], bhn2, bHN2s, None, q='act')

            def stageB2(ot):
                for dc in range(16):
                    kb.op('pe', lambda e, dc=dc: e.transpose(out=pT4[:, dc * 128:(dc + 1) * 128], in_=hn2[:, dc * 128:(dc + 1) * 128], identity=identb[:]),
                          reads=[bhn2, bC], writes=[bpT4])
                kb.op('act', lambda e: e.copy(out=hn2T[:, 0:1024], in_=pT4[:, 0:1024]), reads=[bpT4], writes=[bhn2T])
                kb.op('dve', lambda e: e.tensor_copy(out=hn2T[:, 1024:2048], in_=pT4[:, 1024:2048]), reads=[bpT4], writes=[bhn2T])
                store(HN2Ts[ot], hn2T[:], bhn2T, bHN2Ts, None, q='sp')

            stageA1(0)
            stageA2(0)
            for ot in range(16):
                if ot + 1 < 16:
                    stageA1(ot + 1)
                stageB1(ot)
                if ot + 1 < 16:
                    stageA2(ot + 1)
                stageB2(ot)
            kb.barrier()
        if stop <= 4:
            kb.enabled = False

        with ExitStack() as P56:
            idxT = alloc(P56, "idxT", [128, 2048], I32)
            gT = alloc(P56, "gT", [128, 2048], F32)
            bidxT = Buf(); bgT = Buf()
            with ExitStack() as P5:
                wq = alloc(P5, "wq", [128, 16, 2048], BF16)
                skT = alloc(P5, "skT", [128, 16, 128], BF16)
                iota16 = alloc(P5, "iota16", [128, 16], F32)
                hg = alloc(P5, "hg", [128, 16, 512], BF16)
                qT = alloc(P5, "qT", [128, 16, 512], BF16)
                sc = alloc(P5, "sc", [128, 16, 128], F32)
                wk = alloc(P5, "wk", [128, 16, 128], F32)
                stv = alloc(P5, "stv", [128, 16, 16], F32)
                siv = alloc(P5, "siv", [128, 16, 16], U32)
                sif = alloc(P5, "sif", [128, 16, 16], F32)
                cand = alloc(P5, "cand", [128, 8, 256], F32)
                bs = alloc(P5, "bs", [128, 8, 16], F32)
                bp = alloc(P5, "bp", [128, 8, 16], U32)
                bpf = alloc(P5, "bpf", [128, 8, 16], F32)
                pi_ = alloc(P5, "pi_", [128, 8, 16], F32)
                pj_ = alloc(P5, "pj_", [128, 8, 16], F32)
                pii = alloc(P5, "pii", [128, 8, 16], I32)
                eq = alloc(P5, "eq", [128, 128, 16], F32)
                ea = alloc(P5, "ea", [128, 128], F32)
                ebb = alloc(P5, "ebb", [128, 128], F32)
                idxf = alloc(P5, "idxf", [128, 128], F32)
                gte = alloc(P5, "gte", [128, 8, 16], F32)
                gsum = alloc(P5, "gsum", [128, 8], F32)
                pQ = [palloc(P5, "pQ%d" % i, [128, 512], F32) for i in range(2)]
                pS5 = [palloc(P5, "pS5%d" % i, [128, 512], F32) for i in range(4)]
                pTr = palloc(P5, "pTr", [128, 512], F32)
                bwq = Buf(); b5c = Buf(); bhg = Buf(); bqT = Buf(); bsc = Buf(); bwk = Buf(); bst5 = Buf(); bcand = Buf(); bcwk = Buf()
                bbs = Buf(); bsel = Buf(); bidxf = Buf(); bgte = Buf(); bpQ = [PSB(), PSB()]; bpS5 = PSB(); bpTr = PSB()
                s5c = kb.dsem('c5'); shg = kb.dsem('hg')
                load(iota16[:], iota_d, b5c, s5c)
                with ExitStack() as P5w:
                    skTf = alloc(P5w, "skTf", [128, 16, 128], F32)
                    load(skTf[:], skT_d, b5c, s5c)
                    kb.op('dve', lambda e: e.tensor_copy(out=skT[:], in_=skTf[:]), reads=[b5c], writes=[b5c])
                    load_weight_bf16(P5w, w_q, wq, bwq, None, "wq")
                    kb.barrier()
                hg2 = [hg, alloc(P5, "hgB", [128, 16, 512], BF16)]
                bhg2 = [bhg, Buf()]

                def load_hg(gq):
                    if gq < 4:
                        for jj_ in range(4):
                            load(hg2[gq % 2][:, :, jj_ * 128:(jj_ + 1) * 128], HN2Ts[gq * 4 + jj_].rearrange("p (kc t) -> p kc t", kc=16), bhg2[gq % 2], None, q=ldq())
                load_hg(0)
                for g4 in range(4):
                    load_hg(g4 + 1)
                    hgc = hg2[g4 % 2]
                    bhgc = bhg2[g4 % 2]
                    for hp in range(16):
                        c = hp % 2
                        for kc in range(16):
                            kb.op('pe', lambda e, c=c, kc=kc, hp=hp, hgc=hgc: e.matmul(pQ[c][:], lhsT=wq[:, kc, hp * 128:(hp + 1) * 128], rhs=hgc[:, kc, :],
                                                                             start=(kc == 0), stop=(kc == 15)), reads=[bwq, bhgc], writes=[bpQ[c]])
                        kb.op('act', lambda e, c=c, hp=hp: e.copy(out=qT[:, hp, :], in_=pQ[c][:]), reads=[bpQ[c]], writes=[bqT])
                    emit_cast(8, after=[bqT])
                    for jj in range(4):
                        ot = g4 * 4 + jj
                        for hp in range(16):
                            kb.op('pe', lambda e, hp=hp, jj=jj: e.matmul(pS5[hp // 4][:, (hp % 4) * 128:(hp % 4 + 1) * 128], lhsT=qT[:, hp, jj * 128:(jj + 1) * 128],
                                                                        rhs=skT[:, hp, :], start=True, stop=True), reads=[bqT, b5c], writes=[bpS5])
                        for b4 in range(4):
                            kb.op('act', lambda e, b4=b4: e.copy(out=sc[:, b4 * 4:(b4 + 1) * 4, :], in_=pS5[b4][:].rearrange("p (a k) -> p a k", a=4)),
                                  reads=[bpS5], writes=[bsc])
                        for hp in range(16):
                            kb.op('dve', lambda e, hp=hp: e.max(out=stv[:, hp, 0:8], in_=sc[:, hp, :]), reads=[bsc], writes=[bst5])
                        for hp in range(16):
                            kb.op('dve', lambda e, hp=hp: e.max_index(out=siv[:, hp, 0:8], in_max=stv[:, hp, 0:8], in_values=sc[:, hp, :]), reads=[bsc, bst5], writes=[bst5])
                        for hp in range(16):
                            kb.op('dve', lambda e, hp=hp: e.match_replace(out=wk[:, hp, :], in_to_replace=stv[:, hp, 0:8], in_values=sc[:, hp, :], imm_value=-1e30),
                                  reads=[bsc, bst5], writes=[bwk])
                        for hp in range(16):
                            kb.op('dve', lambda e, hp=hp: e.max(out=stv[:, hp, 8:16], in_=wk[:, hp, :]), reads=[bwk], writes=[bst5])
                        for hp in range(16):
                            kb.op('dve', lambda e, hp=hp: e.max_index(out=siv[:, hp, 8:16], in_max=stv[:, hp, 8:16], in_values=wk[:, hp, :]), reads=[bwk, bst5], writes=[bst5])
                        kb.op('dve', lambda e: e.tensor_copy(out=sif[:], in_=siv[:]), reads=[bst5], writes=[bst5])
                        stv4 = stv[:].rearrange("t (h p) k -> t h p k", p=2)
                        sif4 = sif[:].rearrange("t (h p) k -> t h p k", p=2)
                        for h in range(8):
                            kb.op('dve', lambda e, h=h: e.tensor_tensor(
                                out=cand[:, h, :].rearrange("t (i j) -> t i j", i=16),
                                in0=stv4[:, h, 0, :].unsqueeze(2).to_broadcast([128, 16, 16]),
                                in1=stv4[:, h, 1, :].unsqueeze(1).to_broadcast([128, 16, 16]), op=ALU.add), reads=[bst5], writes=[bcand])
                        for h in range(8):
                            kb.op('dve', lambda e, h=h: e.max(out=bs[:, h, 0:8], in_=cand[:, h, :]), reads=[bcand], writes=[bbs])
                        for h in range(8):
                            kb.op('dve', lambda e, h=h: e.max_index(out=bp[:, h, 0:8], in_max=bs[:, h, 0:8], in_values=cand[:, h, :]), reads=[bcand, bbs], writes=[bbs])
                        for h in range(8):
                            kb.op('dve', lambda e, h=h: e.match_replace(out=wk[:].rearrange("t a k -> t (a k)")[:, h * 256:(h + 1) * 256], in_to_replace=bs[:, h, 0:8], in_values=cand[:, h, :], imm_value=-1e30),
                                  reads=[bcand, bbs], writes=[bwk])
                        for h in range(8):
                            kb.op('dve', lambda e, h=h: e.max(out=bs[:, h, 8:16], in_=wk[:].rearrange("t a k -> t (a k)")[:, h * 256:(h + 1) * 256]), reads=[bwk], writes=[bbs])
                        for h in range(8):
                            kb.op('dve', lambda e, h=h: e.max_index(out=bp[:, h, 8:16], in_max=bs[:, h, 8:16], in_values=wk[:].rearrange("t a k -> t (a k)")[:, h * 256:(h + 1) * 256]), reads=[bwk, bbs], writes=[bbs])
                        kb.op('dve', lambda e: e.tensor_copy(out=bpf[:], in_=bp[:]), reads=[bbs], writes=[bsel])
                        kb.op('dve', lambda e: e.tensor_scalar(out=pj_[:], in0=bpf[:], scalar1=1.0 / 16.0, scalar2=None, op0=ALU.mult), reads=[bsel], writes=[bsel])
                        kb.op('dve', lambda e: e.tensor_copy(out=pii[:], in_=pj_[:]), reads=[bsel], writes=[bsel])
                        kb.op('dve', lambda e: e.tensor_copy(out=pi_[:], in_=pii[:]), reads=[bsel], writes=[bsel])
                        kb.op('dve', lambda e: e.tensor_tensor(out=pj_[:], in0=pi_[:], in1=pj_[:], op=ALU.is_gt), reads=[bsel], writes=[bsel])
                        kb.op('dve', lambda e: e.tensor_tensor(out=pi_[:], in0=pi_[:], in1=pj_[:], op=ALU.subtract), reads=[bsel], writes=[bsel])
                        kb.op('dve', lambda e: e.scalar_tensor_tensor(out=pj_[:], in0=pi_[:], scalar=-16.0, in1=bpf[:], op0=ALU.mult, op1=ALU.add), reads=[bsel], writes=[bsel])
                        for which, pp, dst in ((0, pi_, ea), (1, pj_, ebb)):
                            kb.op('dve', lambda e, pp=pp: e.tensor_tensor(
                                out=eq[:], in0=pp[:].rearrange("t h k -> t (h k)").unsqueeze(2).to_broadcast([128, 128, 16]),
                                in1=iota16[:].unsqueeze(1).to_broadcast([128, 128, 16]), op=ALU.is_equal), reads=[bsel, b5c], writes=[bsel])
                            for h in range(8):
                                kb.op('dve', lambda e, h=h, which=which: e.tensor_tensor(
                                    out=eq[:, h * 16:(h + 1) * 16, :], in0=eq[:, h * 16:(h + 1) * 16, :],
                                    in1=sif4[:, h, which, :].unsqueeze(1).to_broadcast([128, 16, 16]), op=ALU.mult), reads=[bsel, bst5], writes=[bsel])
                            kb.op('dve', lambda e, dst=dst: e.tensor_reduce(out=dst[:], in_=eq[:], axis=AX.X, op=ALU.add), reads=[bsel], writes=[bsel])
                        kb.op('dve', lambda e: e.scalar_tensor_tensor(out=idxf[:], in0=ea[:], scalar=128.0, in1=ebb[:], op0=ALU.mult, op1=ALU.add), reads=[bsel], writes=[bidxf])
                        kb.op('dve', lambda e: e.tensor_tensor(out=gte[:], in0=bs[:], in1=bs[:, :, 0:1].to_broadcast([128, 8, 16]), op=ALU.subtract), reads=[bbs], writes=[bgte])
                        kb.op('act', lambda e: e.activation(out=gte[:], in_=gte[:], func=AF.Exp), reads=[bgte], writes=[bgte])
                        kb.op('dve', lambda e: e.tensor_reduce(out=gsum[:], in_=gte[:], axis=AX.X, op=ALU.add), reads=[bgte], writes=[bgte])
                        kb.op('dve', lambda e: e.reciprocal(out=gsum[:], in_=gsum[:]), reads=[bgte], writes=[bgte])
                        kb.op('dve', lambda e: e.tensor_tensor(out=gte[:], in0=gte[:], in1=gsum[:].unsqueeze(2).to_broadcast([128, 8, 16]), op=ALU.mult), reads=[bgte], writes=[bgte])
                        kb.op('pe', lambda e: e.transpose(out=pTr[:, 0:128], in_=idxf[:], identity=identf[:]), reads=[bidxf, bC], writes=[bpTr])
                        kb.op('pe', lambda e: e.transpose(out=pTr[:, 128:256], in_=gte[:].rearrange("t h k -> t (h k)"), identity=identf[:]), reads=[bgte, bC], writes=[bpTr])
                        kb.op('dve', lambda e, ot=ot: e.tensor_copy(out=idxT[:, ot * 128:(ot + 1) * 128], in_=pTr[:, 0:128]), reads=[bpTr], writes=[bidxT])
                        kb.op('act', lambda e, ot=ot: e.copy(out=gT[:, ot * 128:(ot + 1) * 128], in_=pTr[:, 128:256]), reads=[bpTr], writes=[bgT])
                if dbg:
                    sdb5 = kb.dsem('dbg5')
                    bdb5 = Buf()
                    store(dbgidx, idxT[:], bidxT, bdb5, sdb5)
                    store(dbgg, gT[:], bgT, bdb5, sdb5)
                kb.barrier()
            if stop <= 5:
                kb.enabled = False

            flush_cast()
            kb.barrier()
            with ExitStack() as P6:
                NB = 6
                gb = [alloc(P6, "gb%d" % i, [128, 4096], BF16) for i in range(NB)]
                hn2t = [alloc(P6, "hn2t%d" % i, [128, 2048], BF16) for i in range(2)]
                junk6 = alloc(P6, "junk6", [128, 1024], BF16)
                NA = 4
                acc = [alloc(P6, "acc%d" % i, [128, 8], F32) for i in range(NA)]
                win = [alloc(P6, "win%d" % i, [128, 256], BF16) for i in range(2)]
                gt2bc = alloc(P6, "gt2bc", [128, 2048], F32)
                gfbc = alloc(P6, "gfbc", [128, 2048], F32)
                dtmp6 = alloc(P6, "dtmp6", [128, 128], F32)
                h16 = alloc(P6, "h16", [128, 2048], F32)
                tmp6 = alloc(P6, "tmp6", [128, 2048], F32)
                hf6 = alloc(P6, "hf6", [128, 2048], F32)
                o6 = alloc(P6, "o6", [128, 2048], F32)
                ss6 = alloc(P6, "ss6", [128, 2], F32)
                pXh = [palloc(P6, "pXh%d" % i, [128, 1024], F32) for i in range(2)]
                pOut = [palloc(P6, "pOut%d" % i, [128, 512], F32) for i in range(4)]
                bgb = [Buf() for _ in range(NB)]; bhn2t = [Buf(), Buf()]; bj6 = Buf(); bacc = [Buf() for _ in range(NA)]
                bwin = [Buf(), Buf()]; bbc6 = Buf(); bdt6 = Buf(); bh16 = Buf(); btmp6 = Buf(); bhf6 = Buf(); bo6 = Buf(); bss6 = Buf(); brs6 = Buf()
                bpXh = [PSB(), PSB()]; bpOut = PSB(); bOut = Buf(); bpXall = PSB()
                sgb = [kb.dsem('gb%d' % i) for i in range(NB)]
                ps4 = [pXh[0][:, 0:512], pXh[0][:, 512:1024], pXh[1][:, 0:512], pXh[1][:, 512:1024]]
                make_bc(P6, gt2bc, bbc6, gt2T, bMod, ps4, bpXall, dtmp6, bdt6)
                load(gfbc[:], gfin_d.to_broadcast([128, 2048]), bbc6, None)
                for i in range(2):
                    kb.op('dve', lambda e, i=i: e.memset(win[i][:], 0.0), writes=[bwin[i]])
                kb.barrier()
                HN2v = HN2s.rearrange("(n p) d -> n p d", p=128)
                H1v = H1s.rearrange("(n p) d -> n p d", p=128)
                Ov = out_d.rearrange("(n p) d -> n p d", p=128)
                loaded = set()

                def ensure_tile(ot):
                    if ot not in loaded and ot < 16:
                        loaded.add(ot)
                        load(hn2t[ot % 2][:], HN2v[ot], bhn2t[ot % 2], None, q='sp')

                def emit_xb(t):
                    ot, tt = divmod(t, 128)
                    ensure_tile(ot)
                    hi = ot % 2
                    for hf in range(2):
                        for bb in range(2):
                            b4 = hf * 2 + bb
                            kb.op('pe', lambda e, hf=hf, bb=bb, b4=b4, tt=tt, hi=hi: e.matmul(
                                pXh[hf][:, bb * 512:(bb + 1) * 512], lhsT=identb[:, tt:tt + 1].to_broadcast([128, 128]),
                                rhs=hn2t[hi][:, b4 * 512:(b4 + 1) * 512], start=True, stop=True),
                                reads=[bC, bhn2t[hi]], writes=[bpXh[hf]])

                def emit_gather(t):
                    u = t % NB
                    kb.dma('pool', lambda e, u=u, t=t: e.indirect_dma_start(
                        out=gb[u][:], out_offset=None, in_=UV16, in_offset=bass.IndirectOffsetOnAxis(ap=idxT[:, t:t + 1], axis=0)),
                        sgb[u], reads=[bidxT], writes=[bgb[u]])

                emit_gather(0)
                emit_xb(0)
                for t in range(2048):
                    ot, tt = divmod(t, 128)
                    u = t % NB
                    a4 = t % NA
                    a2 = t % 2
                    if t + 1 < 2048:
                        emit_gather(t + 1)
                    for hf in range(2):
                        kb.op('dve', lambda e, hf=hf, u=u, a4=a4: e.scalar_tensor_tensor(
                            out=junk6[:], in0=gb[u][:, hf * 1024:(hf + 1) * 1024], scalar=1.0, in1=pXh[hf][:], op0=ALU.mult, op1=ALU.mult,
                            accum_out=acc[a4][:, hf:hf + 1]), reads=[bgb[u], bpXh[hf]], writes=[bj6, bacc[a4]])
                    kb.op('act', lambda e, a4=a4: e.activation(out=acc[a4][:, 2:4], in_=acc[a4][:, 0:2], func=AF.Identity, accum_out=acc[a4][:, 4:5]),
                          reads=[bacc[a4]], writes=[bacc[a4]])
                    kb.op('act', lambda e, a4=a4: e.activation(out=acc[a4][:, 5:6], in_=acc[a4][:, 4:5], func=AF.Gelu), reads=[bacc[a4]], writes=[bacc[a4]])
                    kb.op('act', lambda e, a4=a4, a2=a2, t=t: e.activation(out=win[a2][:, 127:128], in_=acc[a4][:, 5:6], func=AF.Identity, scale=gT[:, t:t + 1]),
                          reads=[bacc[a4], bgT], writes=[bwin[a2]])
                    if t + 1 < 2048:
                        emit_xb(t + 1)
                    for b4 in range(4):
                        kb.op('pe', lambda e, b4=b4, a2=a2, tt=tt, u=u: e.matmul(pOut[b4][:], lhsT=win[a2][:, 127 - tt:255 - tt], rhs=gb[u][:, 2048 + b4 * 512:2048 + (b4 + 1) * 512],
                                                                                start=(tt == 0), stop=(tt == 127)), reads=[bwin[a2], bgb[u]], writes=[bpOut])
                    if tt == 127:
                        load(h16[:], H1v[ot], bh16, None, q='act')
                        for b4 in range(4):
                            kb.op('dve', lambda e, b4=b4: e.tensor_tensor(out=tmp6[:, b4 * 512:(b4 + 1) * 512], in0=pOut[b4][:], in1=gt2bc[:, b4 * 512:(b4 + 1) * 512], op=ALU.mult),
                                  reads=[bpOut, bbc6], writes=[btmp6])
                        kb.op('pool', lambda e: e.tensor_tensor(out=hf6[:], in0=tmp6[:], in1=h16[:], op=ALU.add), reads=[btmp6, bh16], writes=[bhf6])
                        kb.op('act', lambda e: e.activation(out=tmp6[:], in_=hf6[:], func=AF.Square, accum_out=ss6[:, 0:1]), reads=[bhf6], writes=[btmp6, bss6])
                        rstd_from_ss(ss6[:, 0:1], ss6[:, 1:2], bss6, brs6, float(D))
                        kb.op('dve', lambda e: e.scalar_tensor_tensor(out=o6[:], in0=hf6[:], scalar=ss6[:, 1:2], in1=gfbc[:], op0=ALU.mult, op1=ALU.mult),
                              reads=[bhf6, brs6, bbc6], writes=[bo6])
                        store(Ov[ot], o6[:], bo6, bOut, None, q='sp')
                kb.barrier()

        blk = es.enter_context(nc.Block())
        kb.emit(blk)
    return nc


_NC = None


def kernel(x, c, positions, w_ada, b_ada, g_norm1, w_in, g_attn_out, g_sgu_out, sgu_w, sgu_b, sgu_ln_g, sgu_ln_b,
           w_out, g_norm2, peer_w_q, peer_sub_keys, peer_u, peer_v, g_final, _prep_only=False):
    global _NC
    f32 = np.float32
    x = np.asarray(x, f32); c = np.asarray(c, f32); positions = np.asarray(positions, np.int32)

    def colT(v, n):
        return np.ascontiguousarray(np.asarray(v, f32).reshape(n, 128).T)

    half = 64
    inv = (10000.0 ** (-np.arange(half, dtype=np.float64) / half))
    inv2 = np.concatenate([inv, inv]) / (2.0 * np.pi)
    inv2 = inv2.astype(f32).reshape(128, 1)
    sgn = np.concatenate([-np.ones(64), np.ones(64)]).astype(f32).reshape(128, 1)
    k = np.arange(128)[:, None]
    cidx = np.arange(17 * 128)[None, :]
    delta = cidx - k
    cm = ((delta >= 0) & (delta <= 128)).astype(f32) + ((delta >= 0) & (delta % 4 == 0) & (delta <= 512)).astype(f32) \
        + ((delta >= 0) & (delta % 16 == 0) & (delta <= 2048)).astype(f32)
    maskW = cm.astype(ml_dtypes.bfloat16)
    perm = np.zeros((128, 128), f32)
    perm[(np.arange(128) + 64) % 128, np.arange(128)] = 1.0
    perm = perm.astype(ml_dtypes.bfloat16)
    identf = np.eye(128, dtype=f32)
    iota16 = np.tile(np.arange(16, dtype=f32)[None, :], (128, 1))
    trilT = (np.arange(128)[:, None] <= np.arange(128)[None, :]).astype(f32)

    shared = {
        "w_ada": np.ascontiguousarray(np.asarray(w_ada, f32)[0]),
        "b_adaT": colT(np.asarray(b_ada)[0], 96),
        "gn1T": colT(np.asarray(g_norm1)[0], 16),
        "gn2T": colT(np.asarray(g_norm2)[0], 16),
        "w_in": np.ascontiguousarray(np.asarray(w_in, f32)[0]),
        "w_out": np.ascontiguousarray(np.asarray(w_out, f32)[0]),
        "gmixT": colT(np.concatenate([np.asarray(g_attn_out)[0], np.asarray(g_sgu_out)[0]]), 16),
        "sgu_wT": np.ascontiguousarray(np.transpose(np.asarray(sgu_w, f32)[0], (2, 0, 1))),
        "sgu_bT": np.ascontiguousarray(np.asarray(sgu_b, f32)[0].T),
        "lng": np.ascontiguousarray(np.asarray(sgu_ln_g, f32)[0].reshape(1, 512)),
        "lnb": np.ascontiguousarray(np.asarray(sgu_ln_b, f32)[0].reshape(1, 512)),
        "w_q": np.ascontiguousarray(np.asarray(peer_w_q, f32)[0]),
        "skT": np.ascontiguousarray(np.transpose(np.asarray(peer_sub_keys, f32)[0].reshape(16, 128, 128), (2, 0, 1))),
        "peer_u": np.ascontiguousarray(np.asarray(peer_u, f32)[0]),
        "peer_v": np.ascontiguousarray(np.asarray(peer_v, f32)[0]),
        "gfin": np.ascontiguousarray(np.asarray(g_final, f32).reshape(1, D)),
        "inv2": inv2, "sgn": sgn, "maskW": maskW, "perm": perm, "identf": identf, "iota16": iota16, "trilT": trilT,
    }
    in_maps = []
    for core in range(8):
        b = core // 2
        hf = core % 2
        xa = np.zeros((NCTX + NT, D), f32)
        pa = np.zeros((1, NCTX + NT), np.int32)
        xa[NCTX:] = x[b, hf * NT:(hf + 1) * NT]
        pa[0, NCTX:] = positions[b, hf * NT:(hf + 1) * NT]
        if hf == 1:
            xa[:NCTX] = x[b, 0:NCTX]
            pa[0, :NCTX] = positions[b, 0:NCTX]
        m = dict(shared)
        m["xall"] = xa
        m["pos"] = pa
        m["condT"] = colT(c[b], 16)
        m["flag"] = np.full((128, 1), float(hf), f32)
        in_maps.append(m)
    if _prep_only:
        return in_maps
    if _NC is None:
        import os
        _NC = build(stop=float(os.environ.get('KSTOP', '99')))
    res = run_bass_kernel_spmd(_NC, in_maps, core_ids=list(range(8)))
    out = np.zeros((4, 4096, D), f32)
    for core in range(8):
        b = core // 2
        hf = core % 2
        out[b, hf * NT:(hf + 1) * NT] = np.asarray(res.results[core]["out"], f32)
    return out
```
